# Optimizing a Trainium2 kernel written in Bass

```python
import numpy as np
import jax
import jax.numpy as jnp
from jax import lax

D_MODEL = 1024
BATCH = 8
SEQ = 8192
DEPTH = 2

CTX_LEN = 256
GRID_W = 64
N_MOD = 9
D_FF = 2816
NA_HEADS = 6
NA_HEAD_DIM = 64
NA_WIDTH = NA_HEADS * NA_HEAD_DIM
NA_WIN_R = 8
NA_WIN_C = 16
SG_GROUPS = 4
SG_GROUP_DIM = 64
SG_WIDTH = SG_GROUPS * SG_GROUP_DIM
SG_CHUNK = 128
RET_HEADS = 6
RET_HEAD_DIM = 64
RET_WIDTH = RET_HEADS * RET_HEAD_DIM
RET_CHUNK = 128
D_MIX = NA_WIDTH + SG_WIDTH + RET_WIDTH
IN_COLS = 3 * NA_WIDTH + 2 * SG_WIDTH + 5 * RET_WIDTH
ROPE_BASE = 10000.0
ROPE_FREQS = RET_HEAD_DIM // 4
RMS_EPS = 1e-6
LN_EPS = 1e-5

kernel_name = 'hybrid_na_gmlp_retention_dit_block'


def rms_norm(h, w):
    hf = h.astype(jnp.float32)
    hf = hf * lax.rsqrt(jnp.mean(hf * hf, axis=-1, keepdims=True) + RMS_EPS)
    return hf.astype(h.dtype) * w


def ada_rms(h, w, shift, scale):
    return rms_norm(h, w) * (1.0 + scale) + shift


def swiglu(h, w1, w2):
    g, u = jnp.split(h @ w1, 2, axis=-1)
    return (jax.nn.silu(g) * u) @ w2


def ffn_branch(h, mod, i, nw, w1, w2):
    return h + 0.5 * mod[:, i + 2] * swiglu(ada_rms(h, nw, mod[:, i], mod[:, i + 1]), w1, w2)


def split_heads(t, n):
    return t.reshape(*t.shape[:-1], n, t.shape[-1] // n)


def qk_norm(t, w):
    tf = t.astype(jnp.float32)
    tf = tf * lax.rsqrt(jnp.mean(tf * tf, axis=-1, keepdims=True) + RMS_EPS)
    return tf.astype(t.dtype) * w


def rope_rotate(t, ang):
    cos = jnp.cos(ang)[None, :, None, :].astype(t.dtype)
    sin = jnp.sin(ang)[None, :, None, :].astype(t.dtype)
    t1, t2 = jnp.split(t, 2, axis=-1)
    return jnp.concatenate([t1 * cos - t2 * sin, t1 * sin + t2 * cos], axis=-1)


def axial_rope(t, ang_r, ang_c):
    tr, tc = jnp.split(t, 2, axis=-1)
    return jnp.concatenate([rope_rotate(tr, ang_r), rope_rotate(tc, ang_c)], axis=-1)


def neighbourhood_attention(q, k, v, kc, vc, rpb, rows):
    b, s, h, d = q.shape
    kr = min(NA_WIN_R, rows)
    scale = NA_HEAD_DIM ** -0.5
    qg = q.reshape(b, rows, GRID_W, h, d)
    kg = k.reshape(b, rows, GRID_W, h, d)
    vg = v.reshape(b, rows, GRID_W, h, d)
    col = jnp.arange(GRID_W)
    col_start = jnp.clip(col - NA_WIN_C // 2, 0, GRID_W - NA_WIN_C)
    col_idx = col_start[:, None] + jnp.arange(NA_WIN_C)[None, :]
    col_off = col_idx - col[:, None] + (NA_WIN_C - 1)
    n_loc = kr * NA_WIN_C

    def one_row(r):
        r_start = jnp.clip(r - kr // 2, 0, rows - kr)
        q_r = lax.dynamic_index_in_dim(qg, r, axis=1, keepdims=False)
        k_rows = lax.dynamic_slice_in_dim(kg, r_start, kr, axis=1)
        v_rows = lax.dynamic_slice_in_dim(vg, r_start, kr, axis=1)
        k_win = jnp.take(k_rows, col_idx, axis=2)
        v_win = jnp.take(v_rows, col_idx, axis=2)
        row_off = r_start + jnp.arange(kr) - r + (NA_WIN_R - 1)
        bias = rpb[:, row_off[:, None, None], col_off[None, :, :]]
        bias = jnp.transpose(bias, (0, 2, 1, 3))
        s_loc = jnp.einsum('bqhd,biqjhd->bhqij', q_r, k_win) * scale + bias[None]
        s_ctx = jnp.einsum('bqhd,bchd->bhqc', q_r, kc) * scale
        logits = jnp.concatenate([s_loc.reshape(b, h, GRID_W, n_loc), s_ctx], axis=-1)
        p = jax.nn.softmax(logits.astype(jnp.float32), axis=-1).astype(v.dtype)
        p_loc = p[..., :n_loc].reshape(b, h, GRID_W, kr, NA_WIN_C)
        p_ctx = p[..., n_loc:]
        return (jnp.einsum('bhqij,biqjhd->bqhd', p_loc, v_win)
                + jnp.einsum('bhqc,bchd->bqhd', p_ctx, vc))

    out = lax.map(one_row, jnp.arange(rows))
    return jnp.transpose(out, (1, 0, 2, 3, 4)).reshape(b, s, h, d)


def context_attention(q, k, v):
    s = jnp.einsum('bqhd,bkhd->bhqk', q, k) * (NA_HEAD_DIM ** -0.5)
    p = jax.nn.softmax(s.astype(jnp.float32), axis=-1).astype(v.dtype)
    return jnp.einsum('bhqk,bkhd->bqhd', p, v)


def spatial_gating(u, v, w_s, b_s, ln_w, ln_b):
    b, l, _ = v.shape
    u = jax.nn.gelu(u)
    v = jax.nn.gelu(v)
    vf = v.reshape(b, l, SG_GROUPS, SG_GROUP_DIM).astype(jnp.float32)
    mu = jnp.mean(vf, axis=-1, keepdims=True)
    var = jnp.mean(jnp.square(vf - mu), axis=-1, keepdims=True)
    vn = ((vf - mu) * lax.rsqrt(var + LN_EPS)).reshape(b, l, SG_WIDTH).astype(v.dtype) * ln_w + ln_b
    vch = vn.reshape(b, l // SG_CHUNK, SG_CHUNK, SG_GROUPS, SG_GROUP_DIM)
    mixed = jnp.einsum('gpq,bnqgc->bnpgc', w_s, vch) + b_s.T[:, :, None]
    return u * mixed.reshape(b, l, SG_WIDTH)


def retention_final_state(k, v, log_g):
    l = k.shape[1]
    w = jnp.exp((l - 1 - jnp.arange(l, dtype=jnp.float32))[None, :] * log_g[:, None])
    return jnp.einsum('blhd,hl,blhe->bhde', k.astype(jnp.float32), w, v.astype(jnp.float32))


def retention_chunkwise(q, k, v, log_g, state0):
    q, k, v = (a.astype(jnp.float32) for a in (q, k, v))
    b, l, h, d = q.shape
    n = l // RET_CHUNK
    qc = q.reshape(b, n, RET_CHUNK, h, d)
    kc = k.reshape(b, n, RET_CHUNK, h, d)
    vc = v.reshape(b, n, RET_CHUNK, h, d)
    pos = jnp.arange(RET_CHUNK, dtype=jnp.float32)
    diff = pos[:, None] - pos[None, :]
    intra_decay = jnp.where(diff >= 0, jnp.exp(jnp.maximum(diff, 0.0)[None] * log_g[:, None, None]), 0.0)
    scores = jnp.einsum('bnihd,bnjhd->bnhij', qc, kc) * intra_decay
    o_intra = jnp.einsum('bnhij,bnjhe->bnihe', scores, vc)
    to_end = jnp.exp((RET_CHUNK - 1 - pos)[None, :] * log_g[:, None])
    chunk_kv = jnp.einsum('bnjhd,hj,bnjhe->nbhde', kc, to_end, vc)
    chunk_decay = jnp.exp(RET_CHUNK * log_g)[None, :, None, None]

    def step(state, kv):
        return chunk_decay * state + kv, state

    _, prev = lax.scan(step, state0, chunk_kv)
    from_start = jnp.exp((pos + 1.0)[None, :] * log_g[:, None])
    o_cross = jnp.einsum('bnihd,nbhde,hi->bnihe', qc, prev, from_start)
    return (o_intra + o_cross).reshape(b, l, h, d)


def head_group_norm(o):
    b, l, h, d = o.shape
    mu = jnp.mean(o, axis=-1, keepdims=True)
    var = jnp.mean(jnp.square(o - mu), axis=-1, keepdims=True)
    return ((o - mu) * lax.rsqrt(var + LN_EPS)).reshape(b, l, h * d)


def retention_merge(o_f, o_b, g_f, g_b, gn_w):
    y = (head_group_norm(o_f) * jax.nn.silu(g_f.astype(jnp.float32))
         + head_group_norm(o_b) * jax.nn.silu(g_b.astype(jnp.float32)))
    return (y * gn_w.astype(jnp.float32)).astype(g_f.dtype)


def merge_groups(na, sg, ret, w_out):
    b, l = sg.shape[:2]
    return jnp.concatenate([na.reshape(b, l, NA_WIDTH), sg, ret], axis=-1) @ w_out


def hybrid_mixer(hx, hc, rows, ang_r, ang_c, w_in, w_out, na_qn, na_kn, na_rpb,
                 sg_w, sg_b, sg_ln_w, sg_ln_b, ret_logit, ret_gn_w, with_ctx_out):
    sizes = (NA_WIDTH,) * 3 + (SG_WIDTH,) * 2 + (RET_WIDTH,) * 5
    cuts = tuple(int(s) for s in np.cumsum(sizes)[:-1])
    px = jnp.split(hx @ w_in, cuts, axis=-1)
    pc = jnp.split(hc @ w_in, cuts, axis=-1)
    log_g = jax.nn.log_sigmoid(ret_logit.astype(jnp.float32))

    kc_na = qk_norm(split_heads(pc[1], NA_HEADS), na_kn)
    vc_na = split_heads(pc[2], NA_HEADS)
    na_x = neighbourhood_attention(qk_norm(split_heads(px[0], NA_HEADS), na_qn),
                                   qk_norm(split_heads(px[1], NA_HEADS), na_kn),
                                   split_heads(px[2], NA_HEADS), kc_na, vc_na, na_rpb, rows)
    sg_x = spatial_gating(px[3], px[4], sg_w, sg_b, sg_ln_w, sg_ln_b)
    k_scale = RET_HEAD_DIM ** -0.5
    rq_x = axial_rope(split_heads(px[5], RET_HEADS), ang_r, ang_c)
    rk_x = axial_rope(split_heads(px[6], RET_HEADS), ang_r, ang_c) * k_scale
    rv_x = split_heads(px[7], RET_HEADS)
    rk_c = split_heads(pc[6], RET_HEADS) * k_scale
    rv_c = split_heads(pc[7], RET_HEADS)
    flip = lambda t: jnp.flip(t, axis=1)
    state_f = retention_final_state(rk_c, rv_c, log_g[0])
    state_b = retention_final_state(flip(rk_c), flip(rv_c), log_g[1])
    o_f = retention_chunkwise(rq_x, rk_x, rv_x, log_g[0], state_f)
    o_b = flip(retention_chunkwise(flip(rq_x), flip(rk_x), flip(rv_x), log_g[1], state_b))
    ret_x = retention_merge(o_f, o_b, px[8], px[9], ret_gn_w)
    yx = merge_groups(na_x, sg_x, ret_x, w_out)
    if not with_ctx_out:
        return yx, None

    na_c = context_attention(qk_norm(split_heads(pc[0], NA_HEADS), na_qn), kc_na, vc_na)
    sg_c = spatial_gating(pc[3], pc[4], sg_w, sg_b, sg_ln_w, sg_ln_b)
    rq_c = split_heads(pc[5], RET_HEADS)
    zero = jnp.zeros_like(state_f)
    oc_f = retention_chunkwise(rq_c, rk_c, rv_c, log_g[0], zero)
    oc_b = flip(retention_chunkwise(flip(rq_c), flip(rk_c), flip(rv_c), log_g[1], zero))
    ret_c = retention_merge(oc_f, oc_b, pc[8], pc[9], ret_gn_w)
    yc = merge_groups(na_c, sg_c, ret_c, w_out)
    return yx, yc


def setup_inputs(seed: int = 0) -> dict:
    key = jax.random.key(seed)
    ks = jax.random.split(key, 20)
    f32 = jnp.float32

    def nrm(k, shape, scale):
        return jax.random.normal(k, shape, f32) * scale

    base_logit = jnp.log(2.0 ** (5.0 + jnp.arange(RET_HEADS, dtype=f32)) - 1.0)
    return {
        'x': nrm(ks[0], (BATCH, SEQ, D_MODEL), 1.0),
        'c': nrm(ks[1], (BATCH, D_MODEL), 1.0),
        'ctx': nrm(ks[2], (BATCH, CTX_LEN, D_MODEL), 1.0),
        'c_ctx': nrm(ks[3], (D_MODEL,), 1.0),
        'ada_w': nrm(ks[4], (DEPTH, D_MODEL, N_MOD * D_MODEL), D_MODEL ** -0.5),
        'ada_b': nrm(ks[5], (DEPTH, N_MOD * D_MODEL), 0.02),
        'norm_w': 1.0 + nrm(ks[6], (DEPTH, 3, D_MODEL), 0.02),
        'ffn_w1': nrm(ks[7], (DEPTH, 2, D_MODEL, 2 * D_FF), D_MODEL ** -0.5),
        'ffn_w2': nrm(ks[8], (DEPTH, 2, D_FF, D_MODEL), D_FF ** -0.5),
        'mix_w_in': nrm(ks[9], (DEPTH, D_MODEL, IN_COLS), D_MODEL ** -0.5),
        'mix_w_out': nrm(ks[10], (DEPTH, D_MIX, D_MODEL), D_MIX ** -0.5),
        'na_q_norm': 1.0 + nrm(ks[11], (DEPTH, NA_HEAD_DIM), 0.02),
        'na_k_norm': 1.0 + nrm(ks[12], (DEPTH, NA_HEAD_DIM), 0.02),
        'na_rpb': nrm(ks[13], (DEPTH, NA_HEADS, 2 * NA_WIN_R - 1, 2 * NA_WIN_C - 1), 0.02),
        'sg_w': nrm(ks[14], (DEPTH, SG_GROUPS, SG_CHUNK, SG_CHUNK), SG_CHUNK ** -0.5),
        'sg_b': 1.0 + nrm(ks[15], (DEPTH, SG_GROUPS, SG_CHUNK), 0.02),
        'sg_ln_w': 1.0 + nrm(ks[16], (DEPTH, SG_WIDTH), 0.02),
        'sg_ln_b': nrm(ks[17], (DEPTH, SG_WIDTH), 0.02),
        'ret_decay_logit': base_logit + nrm(ks[18], (DEPTH, 2, RET_HEADS), 0.1),
        'ret_gn_w': 1.0 + nrm(ks[19], (DEPTH, RET_WIDTH), 0.02),
    }


def reference(x, c, ctx, c_ctx, ada_w, ada_b, norm_w, ffn_w1, ffn_w2, mix_w_in, mix_w_out,
              na_q_norm, na_k_norm, na_rpb, sg_w, sg_b, sg_ln_w, sg_ln_b, ret_decay_logit, ret_gn_w):
    b, n_lat, d = x.shape
    rows = n_lat // GRID_W
    t = jnp.arange(n_lat)
    inv = ROPE_BASE ** (-jnp.arange(ROPE_FREQS, dtype=jnp.float32) / ROPE_FREQS)
    ang_r = (t // GRID_W).astype(jnp.float32)[:, None] * inv[None, :]
    ang_c = (t % GRID_W).astype(jnp.float32)[:, None] * inv[None, :]
    for layer in range(DEPTH):
        last = layer == DEPTH - 1
        mod_x = (jax.nn.silu(c) @ ada_w[layer] + ada_b[layer]).reshape(b, N_MOD, 1, d)
        mod_c = (jax.nn.silu(c_ctx) @ ada_w[layer] + ada_b[layer]).reshape(1, N_MOD, 1, d)
        x = ffn_branch(x, mod_x, 0, norm_w[layer, 0], ffn_w1[layer, 0], ffn_w2[layer, 0])
        ctx = ffn_branch(ctx, mod_c, 0, norm_w[layer, 0], ffn_w1[layer, 0], ffn_w2[layer, 0])
        hx = ada_rms(x, norm_w[layer, 1], mod_x[:, 3], mod_x[:, 4])
        hc = ada_rms(ctx, norm_w[layer, 1], mod_c[:, 3], mod_c[:, 4])
        yx, yc = hybrid_mixer(hx, hc, rows, ang_r, ang_c, mix_w_in[layer], mix_w_out[layer],
                              na_q_norm[layer], na_k_norm[layer], na_rpb[layer],
                              sg_w[layer], sg_b[layer], sg_ln_w[layer], sg_ln_b[layer],
                              ret_decay_logit[layer], ret_gn_w[layer], not last)
        x = x + mod_x[:, 5] * yx
        x = ffn_branch(x, mod_x, 6, norm_w[layer, 2], ffn_w1[layer, 1], ffn_w2[layer, 1])
        if not last:
            ctx = ctx + mod_c[:, 5] * yc
            ctx = ffn_branch(ctx, mod_c, 6, norm_w[layer, 2], ffn_w1[layer, 1], ffn_w2[layer, 1])
    return x
```

```python
import math
from contextlib import ExitStack

import numpy as np
import concourse.bass as bass
import concourse.mybir as mybir
from concourse.bass_utils import run_bass_kernel_spmd

F32 = mybir.dt.float32
BF16 = mybir.dt.bfloat16
AF = mybir.ActivationFunctionType
ALU = mybir.AluOpType
AX = mybir.AxisListType

ENGS = ['pe', 'act', 'dve', 'pool', 'sp']

D = 1024
KC = 8
DFF = 2816
FC = 22
NCTX = 256
GW = 64
NMOD = 9
INX = 4352
RMS_EPS = 1e-6
LN_EPS = 1e-5
LN8 = math.log(0.125)
import os
MIXDBG = os.environ.get('MIXDBG', 'scan,na,sg,ret,tr').split(',')


class Res:
    __slots__ = ('name', 'last_w', 'readers')

    def __init__(self, name=''):
        self.name = name
        self.last_w = None
        self.readers = []


class Op:
    __slots__ = ('eng', 'fn', 'deps', 'signal', 'seq', 'is_dma', 'dsem', 'dval', 'dprev')


class Prog:
    def __init__(self, nc, es, n_dma_sems=24):
        self.nc = nc
        self.n_dma_sems = n_dma_sems
        self.csem = {e: es.enter_context(nc.semaphore('c_' + e)) for e in ENGS}
        self.nsem = {'sp': 16, 'pool': 6}
        self.dsem = {e: [es.enter_context(nc.semaphore('d_%s_%d' % (e, i))) for i in range(self.nsem[e])]
                     for e in ('sp', 'pool')}
        self.dma_cnt = {e: 0 for e in ENGS}
        self.dma_use = {e: [0] * n_dma_sems for e in ENGS}
        self.seqc = {e: 0 for e in ENGS}
        self.ops = {e: [] for e in ENGS}
        self.waited = {e: {} for e in ENGS}

    def add(self, eng, fn, reads=(), writes=(), dma=False):
        op = Op()
        op.eng = eng
        op.fn = fn
        op.signal = False
        op.seq = 0
        op.is_dma = dma
        op.dsem = None
        op.dval = 0
        op.dprev = 0
        deps = []
        seen = set()

        def push(d):
            if d is None or id(d) in seen:
                return
            seen.add(id(d))
            if d.eng == 'pe' and eng == 'pe' and not d.is_dma and not dma:
                return
            deps.append(d)

        for r in reads:
            push(r.last_w)
        for w in writes:
            push(w.last_w)
            for rd in w.readers:
                push(rd)
        op.deps = deps
        for d in deps:
            d.signal = True
        for r in reads:
            r.readers.append(op)
        for w in writes:
            w.last_w = op
            w.readers = []
        if dma:
            assert eng in ('sp', 'pool')
            k = self.dma_cnt[eng] % self.nsem[eng]
            self.dma_cnt[eng] += 1
            op.dsem = k
            op.dprev = 16 * self.dma_use[eng][k]
            self.dma_use[eng][k] += 1
            op.dval = 16 * self.dma_use[eng][k]
        self.ops[eng].append(op)
        return op

    def emit_pass(self):
        nc = self.nc
        for e in ENGS:
            last = None
            for op in self.ops[e]:
                if not op.is_dma:
                    last = op
            if last is not None:
                last.signal = True
            for op in self.ops[e]:
                if op.signal and not op.is_dma:
                    self.seqc[e] += 1
                    op.seq = self.seqc[e]
        final_c = dict(self.seqc)
        final_d = {e: [16 * u for u in self.dma_use[e]] for e in ('sp', 'pool')}
        with nc.Block() as block:
            regs = {'pe': block.tensor, 'act': block.scalar, 'dve': block.vector,
                    'pool': block.gpsimd, 'sp': block.sync}
            for e in ENGS:
                ops = self.ops[e]

                def body(eng, e=e, ops=ops):
                    waited = self.waited[e]
                    for op in ops:
                        for d in op.deps:
                            if d.is_dma:
                                key = ('d', d.eng, d.dsem)
                                sem = self.dsem[d.eng][d.dsem]
                                val = d.dval
                            else:
                                key = ('c', d.eng)
                                sem = self.csem[d.eng]
                                val = d.seq
                            if waited.get(key, 0) >= val:
                                continue
                            waited[key] = val
                            eng.wait_ge(sem, val)
                        if op.is_dma:
                            key = ('d', e, op.dsem)
                            if op.dprev > 0 and waited.get(key, 0) < op.dprev:
                                waited[key] = op.dprev
                                eng.wait_ge(self.dsem[e][op.dsem], op.dprev)
                            ins = op.fn(eng)
                            ins.then_inc(self.dsem[e][op.dsem], 16)
                        else:
                            ins = op.fn(eng)
                            if op.signal:
                                ins.then_inc(self.csem[e], 1)
                    for e2 in ENGS:
                        if e2 != e and final_c[e2] > waited.get(('c', e2), 0):
                            waited[('c', e2)] = final_c[e2]
                            eng.wait_ge(self.csem[e2], final_c[e2])
                    for q in ('sp', 'pool'):
                        for k in range(self.nsem[q]):
                            if final_d[q][k] > waited.get(('d', q, k), 0):
                                waited[('d', q, k)] = final_d[q][k]
                                eng.wait_ge(self.dsem[q][k], final_d[q][k])

                regs[e](body)
        self.ops = {e: [] for e in ENGS}


def na_tables(nchunks):
    rows = 2 * nchunks
    kr = min(8, rows)
    sigs = {}
    var_of = []
    base_of = []
    tabs = []
    ir = np.arange(128) // 64
    cc = np.arange(128) % 64
    for j in range(nchunks):
        base = int(np.clip(j - 2, 0, nchunks - 5))
        qrow = 2 * j + ir[None, :]
        qcol = cc[None, :]
        rstart = np.clip(qrow - kr // 2, 0, rows - kr)
        cstart = np.clip(qcol - 8, 0, GW - 16)
        rid = np.zeros((5, 128, 128), np.int64)
        cid = np.zeros((5, 128, 128), np.int64)
        val = np.zeros((5, 128, 128), bool)
        for m in range(5):
            krow = 2 * (base + m) + ir[:, None]
            kcol = cc[:, None]
            v = (krow >= rstart) & (krow < rstart + kr) & (kcol >= cstart) & (kcol < cstart + 16)
            rid[m] = np.clip(krow - qrow + 7, 0, 14)
            cid[m] = np.clip(kcol - qcol + 15, 0, 30)
            val[m] = v
        sig = (base - j, val.tobytes())
        if sig not in sigs:
            sigs[sig] = len(tabs)
            tabs.append((rid, cid, val))
        var_of.append(sigs[sig])
        base_of.append(base)
    ridx = np.stack([t[0] for t in tabs])
    cidx = np.stack([t[1] for t in tabs])
    valid = np.stack([t[2] for t in tabs])
    return var_of, base_of, ridx, cidx, valid


def rope_tables(NT):
    NTOK = NT + NCTX
    t = np.arange(NT)
    inv = (10000.0 ** (-np.arange(16, dtype=np.float32) / 16)).astype(np.float32)
    ang_r = (t // GW).astype(np.float32)[:, None] * inv[None, :]
    ang_c = (t % GW).astype(np.float32)[:, None] * inv[None, :]
    C = np.ones((128, NTOK), np.float32)
    S = np.zeros((128, NTOK), np.float32)
    for p in range(128):
        d = p % 64
        ang = ang_r if d < 32 else ang_c
        f = d % 16
        first = (d % 32) < 16
        C[p, :NT] = np.cos(ang[:, f])
        S[p, :NT] = (-np.sin(ang[:, f])) if first else np.sin(ang[:, f])
    return C, S


def const_table():
    i = np.arange(128, dtype=np.float32)
    diffT = i[None, :] - i[:, None]
    mF = (diffT >= 0).astype(np.float32)
    mB = (diffT <= 0).astype(np.float32)
    ip1 = np.broadcast_to(i[None, :] + 1, (128, 128))
    rev = np.broadcast_to(128 - i[None, :], (128, 128))
    ident = np.eye(128, dtype=np.float32)
    bd = np.zeros((128, 128), np.float32)
    bd[:64, :64] = 1.0 / 64
    bd[64:, 64:] = 1.0 / 64
    pcol = i[:, None]
    prev = 127 - i[:, None]
    return np.ascontiguousarray(np.concatenate([diffT, mF, mB, ip1, rev, ident, bd, pcol, prev], axis=1).astype(np.float32))


C_DIFF, C_MF, C_MB, C_IP1, C_REV, C_ID, C_BD, C_PCOL, C_PREV = [k * 128 for k in range(7)] + [896, 897]
NCONST = 898


def build(NT, n_layers=2, stop_after=None, dbg=False):
    nc = bass.Bass('TRN2', target_bir_lowering=False)
    _uid = [0]

    def _sbuf_tensor(name, shape, dt):
        _uid[0] += 1
        return _orig_sb('%s_u%d' % (name, _uid[0]), shape, dt)

    def _psum_tensor(name, shape, dt):
        _uid[0] += 1
        return _orig_ps('%s_u%d' % (name, _uid[0]), shape, dt)

    _orig_sb = nc.sbuf_tensor
    _orig_ps = nc.psum_tensor
    NTOK = NT + NCTX
    NLC = NT // 128
    NCH = NLC + 2
    tiles = [(i * 512, 512, 0) for i in range(NT // 512)] + [(NT, 256, 1)]
    var_of, base_of, ridx, cidx, valid = na_tables(NLC)
    NV = ridx.shape[0]

    def din(name, shape, dt=F32):
        return nc.dram_tensor(name, list(shape), dt, kind='ExternalInput').ap()

    x_in = din('x', [NT, D])
    ctx_in = din('ctx', [NCTX, D])
    cT_in = din('cT', [128, 16])
    ada_w = din('ada_w', [2, D, NMOD * D])
    ada_bT = din('ada_bT', [2, 128, 72])
    norm_wT = din('norm_wT', [128, 48])
    w1_in = din('ffn_w1', [2, 2, D, 2 * DFF])
    w2_in = din('ffn_w2', [2, 2, DFF, D])
    win_in = din('w_in_ext', [2, D, INX])
    wout_in = din('w_out', [2, D, D])
    qkn_in = din('qknT', [128, 4])
    bias_in = din('na_biasT', [2, NV, 6, 128, 5, 128])
    sgw_in = din('sg_wT', [2, 4, 128, 128])
    sgb_in = din('sgb_rep', [2, 2, 128, 128])
    lnw_in = din('sg_lnw_rep', [128, 512])
    lnb_in = din('sg_lnb_rep', [128, 512])
    dlog_in = din('dlog_rep', [128, 24])
    gnw_in = din('gnw_rep', [128, 768])
    ropeC = din('ropeC', [128, NTOK])
    ropeS = din('ropeS', [128, NTOK])
    const_in = din('consts', [128, NCONST])
    y_out = nc.dram_tensor('y', [NT, D], F32, kind='ExternalOutput').ap()

    dbg_kind = 'ExternalOutput' if dbg else 'Internal'

    def dscr(name, shape, dt):
        return nc.dram_tensor(name, list(shape), dt, kind=dbg_kind).ap()

    xTa = dscr('xTa', [D, NTOK], F32)
    xTb = dscr('xTb', [D, NTOK], F32)
    qT_na = dscr('qT_na', [384, NTOK], BF16)
    kT_na = dscr('kT_na', [384, NTOK], BF16)
    v_na = dscr('v_na', [NTOK, 384], BF16)
    uT_sg = dscr('uT_sg', [256, NTOK], BF16)
    vn_sg = dscr('vn_sg', [NTOK, 256], BF16)
    rqT = dscr('rqT', [384, NTOK], BF16)
    rkT = dscr('rkT', [384, NTOK], BF16)
    rv_d = dscr('rv', [NTOK, 384], BF16)
    gates_d = dscr('gates', [NTOK, 768], BF16)
    catT = dscr('catT', [D, NTOK], BF16)

    dres = {}

    def DR(name, tile):
        k = (name, tile)
        if k not in dres:
            dres[k] = Res('%s_%s' % (name, tile))
        return dres[k]

    def tile_of_chunk(c):
        return c // 4 if c < NLC else NT // 512

    def chunk_tok(c):
        return c * 128

    with ExitStack() as ges:
        P = Prog(nc, ges)
        gsb = lambda name, shape, dt=F32: ges.enter_context(_sbuf_tensor(name, list(shape), dt))
        cst = gsb('cst', [128, NCONST])
        cst_bf = gsb('cst_bf', [128, 3 * 128], BF16)
        modT = gsb('modT', [128, 72, 2])
        mtab = gsb('mtab', [128, 9, 8, 2])
        r_cst = Res('cst')
        r_cstbf = Res('cst_bf')
        r_mtab = Res('mtab')
        lnb8 = gsb('lnb8', [128, 1])
        lnbe = gsb('lnbe', [128, 1])
        ident_f = cst[:, C_ID:C_ID + 128]
        ident_b = cst_bf[:, 0:128]
        bd_b = cst_bf[:, 128:256]
        ones_b = cst_bf[:, 256:384]

        P.add('sp', lambda e: e.dma_start(out=cst[:], in_=const_in), writes=[r_cst], dma=True)
        P.add('dve', lambda e: e.tensor_copy(out=cst_bf[:, 0:256], in_=cst[:, C_ID:C_ID + 256]), reads=[r_cst], writes=[r_cstbf])
        P.add('dve', lambda e: e.memset(cst_bf[:, 256:384], 1.0 / 1024), reads=[], writes=[r_cstbf])
        P.add('dve', lambda e: e.memset(lnb8[:], LN8), reads=[], writes=[r_cstbf])
        P.add('dve', lambda e: e.memset(lnbe[:], LN_EPS), reads=[], writes=[r_cstbf])
        P.emit_pass()

        def pass_xpose_in(dst):
            with ExitStack() as es:
                conv_issue()
                stg_in = [es.enter_context(_sbuf_tensor('xi%d' % i, [128, D], F32)) for i in range(3)]
                r_in = [Res() for _ in range(3)]
                stg = [es.enter_context(_sbuf_tensor('xo%d' % i, [128, KC, 512], F32)) for i in range(2)]
                r_stg = [Res() for _ in range(2)]
                pss = [es.enter_context(_psum_tensor('pxp%d' % i, [128, 512], F32)) for i in range(4)]
                r_ps = [Res() for _ in range(4)]
                ci = 0
                pi = 0
                for ti, (t0, T, j) in enumerate(tiles):
                    so = stg[ti % 2]
                    rso = r_stg[ti % 2]
                    for s in range(T // 128):
                        tok = t0 + s * 128
                        src = x_in[tok:tok + 128, :] if j == 0 else ctx_in[tok - NT:tok - NT + 128, :]
                        b = ci % 3
                        ci += 1
                        P.add('sp', lambda e, b=b, src=src: e.dma_start(out=stg_in[b][:], in_=src), writes=[r_in[b]], dma=True)
                        for half in range(2):
                            pb = pi % 4
                            pi += 1
                            for q in range(4):
                                kc = half * 4 + q
                                P.add('pe', lambda e, pb=pb, q=q, b=b, kc=kc: e.transpose(pss[pb][:, q * 128:(q + 1) * 128], stg_in[b][:, kc * 128:(kc + 1) * 128], ident_f),
                                      reads=[r_in[b], r_cst], writes=[r_ps[pb]])
                            eng = 'act' if half == 0 else 'dve'
                            dstv = so[:, half * 4:half * 4 + 4, s * 128:(s + 1) * 128]
                            srcv = pss[pb][:].rearrange('p (q t) -> p q t', q=4)
                            if eng == 'act':
                                P.add('act', lambda e, dstv=dstv, srcv=srcv: e.copy(out=dstv, in_=srcv), reads=[r_ps[pb]], writes=[rso])
                            else:
                                P.add('dve', lambda e, dstv=dstv, srcv=srcv: e.tensor_copy(out=dstv, in_=srcv), reads=[r_ps[pb]], writes=[rso])
                    dv = dst.rearrange('(kc p) t -> p kc t', p=128)[:, :, t0:t0 + T]
                    P.add('pool', lambda e, so=so, dv=dv, T=T: e.dma_start(out=dv, in_=so[:, :, 0:T]), reads=[rso], writes=[DR(id(dst), ti)], dma=True)
                P.emit_pass()

        def pass_xpose_out(src):
            with ExitStack() as es:
                stg_in = [es.enter_context(_sbuf_tensor('yi%d' % i, [128, KC, 512], F32)) for i in range(2)]
                r_in = [Res() for _ in range(2)]
                stg = [es.enter_context(_sbuf_tensor('yo%d' % i, [128, D], F32)) for i in range(3)]
                r_stg = [Res() for _ in range(3)]
                pss = [es.enter_context(_psum_tensor('pyp%d' % i, [128, 512], F32)) for i in range(4)]
                r_ps = [Res() for _ in range(4)]
                ci = 0
                pi = 0
                for ti, (t0, T, j) in enumerate(tiles):
                    if j == 1:
                        continue
                    si = stg_in[ti % 2]
                    rsi = r_in[ti % 2]
                    sv = src.rearrange('(kc p) t -> p kc t', p=128)[:, :, t0:t0 + T]
                    P.add('sp', lambda e, si=si, sv=sv: e.dma_start(out=si[:], in_=sv), reads=[DR(id(src), ti)], writes=[rsi], dma=True)
                    for s in range(T // 128):
                        b = ci % 3
                        ci += 1
                        for half in range(2):
                            pb = pi % 4
                            pi += 1
                            for q in range(4):
                                kc = half * 4 + q
                                P.add('pe', lambda e, pb=pb, q=q, si=si, kc=kc, s=s: e.transpose(pss[pb][:, q * 128:(q + 1) * 128], si[:, kc, s * 128:(s + 1) * 128], ident_f),
                                      reads=[rsi, r_cst], writes=[r_ps[pb]])
                            dstv = stg[b][:, half * 512:(half + 1) * 512]
                            if half == 0:
                                P.add('act', lambda e, dstv=dstv, pb=pb: e.copy(out=dstv, in_=pss[pb][:]), reads=[r_ps[pb]], writes=[r_stg[b]])
                            else:
                                P.add('dve', lambda e, dstv=dstv, pb=pb: e.tensor_copy(out=dstv, in_=pss[pb][:]), reads=[r_ps[pb]], writes=[r_stg[b]])
                        tok = t0 + s * 128
                        P.add('pool', lambda e, b=b, tok=tok: e.dma_start(out=y_out[tok:tok + 128, :], in_=stg[b][:]), reads=[r_stg[b]], writes=[DR('y', tok)], dma=True)
                P.emit_pass()

        def pass_mod(l):
            with ExitStack() as es:
                conv_issue()
                sb = lambda name, shape, dt=F32: es.enter_context(_sbuf_tensor(name, list(shape), dt))
                cT = sb('cT', [128, 16])
                sig = sb('csig', [128, 16])
                scT = sb('scT', [128, 16])
                abT = sb('abT', [128, 72])
                nwT = sb('nwT', [128, 48])
                wbuf = [sb('adaw%d' % i, [128, KC, 512]) for i in range(2)]
                r_w = [Res() for _ in range(2)]
                pm = es.enter_context(_psum_tensor('pmod', [128, 72, 2], F32))
                r_pm = Res()
                r_c = Res(); r_sc = Res(); r_ab = Res(); r_nw = Res(); r_mod = Res(); r_sig = Res()
                P.add('sp', lambda e: e.dma_start(out=cT[:], in_=cT_in), writes=[r_c], dma=True)
                P.add('sp', lambda e: e.dma_start(out=abT[:], in_=ada_bT[l]), writes=[r_ab], dma=True)
                P.add('sp', lambda e: e.dma_start(out=nwT[:], in_=norm_wT), writes=[r_nw], dma=True)
                P.add('act', lambda e: e.activation(out=sig[:], in_=cT[:], func=AF.Sigmoid), reads=[r_c], writes=[r_sig])
                P.add('dve', lambda e: e.tensor_tensor(out=scT[:], in0=cT[:], in1=sig[:], op=ALU.mult), reads=[r_c, r_sig], writes=[r_sc])
                wv = ada_w[l].rearrange('(kc p) n -> p kc n', p=128)
                for g in range(18):
                    b = g % 2
                    P.add('sp', lambda e, b=b, g=g: e.dma_start(out=wbuf[b][:], in_=wv[:, :, g * 512:(g + 1) * 512]), writes=[r_w[b]], dma=True)
                    for q in range(4):
                        oc = g * 4 + q
                        for kc in range(KC):
                            P.add('pe', lambda e, b=b, q=q, oc=oc, kc=kc: e.matmul(pm[:, oc, :], lhsT=wbuf[b][:, kc, q * 128:(q + 1) * 128], rhs=scT[:, 2 * kc:2 * kc + 2],
                                                                                  start=(kc == 0), stop=(kc == KC - 1)),
                                  reads=[r_w[b], r_sc], writes=[r_pm])
                P.add('dve', lambda e: e.tensor_tensor(out=modT[:], in0=pm[:], in1=abT[:].unsqueeze(2).broadcast_to([128, 72, 2]), op=ALU.add),
                      reads=[r_pm, r_ab], writes=[r_mod])
                for n in range(3):
                    sc = modT[:, (3 * n + 1) * 8:(3 * n + 2) * 8, :]
                    nw = nwT[:, (l * 3 + n) * 8:(l * 3 + n + 1) * 8].unsqueeze(2).broadcast_to([128, 8, 2])
                    P.add('dve', lambda e, n=n, sc=sc, nw=nw: e.scalar_tensor_tensor(out=mtab[:, n, :, :], in0=sc, scalar=1.0, in1=nw, op0=ALU.add, op1=ALU.mult),
                          reads=[r_mod, r_nw], writes=[r_mtab])
                    sh = modT[:, (3 * n) * 8:(3 * n + 1) * 8, :]
                    P.add('dve', lambda e, n=n, sh=sh: e.tensor_copy(out=mtab[:, 3 + n, :, :], in_=sh), reads=[r_mod], writes=[r_mtab])
                    gt = modT[:, (3 * n + 2) * 8:(3 * n + 3) * 8, :]
                    gm = 1.0 if n == 1 else 0.5
                    P.add('dve', lambda e, n=n, gt=gt, gm=gm: e.tensor_scalar(out=mtab[:, 6 + n, :, :], in0=gt, scalar1=gm, scalar2=None, op0=ALU.mult),
                          reads=[r_mod], writes=[r_mtab])
                P.emit_pass()

        def mt(kind, n, kc, j):
            return mtab[:, kind * 3 + n, kc, j:j + 1]

        wb_cache = {}

        def wb_tensor(key, src):
            if key not in wb_cache:
                rows, cols = src.shape
                wb_cache[key] = (dscr('wb_' + key, [rows, cols], BF16), src, Res('wb_' + key))
            return wb_cache[key]

        conv_jobs = []

        def conv_issue():
            if not conv_jobs:
                return
            for key, src in conv_jobs.pop(0):
                dstt, _, res = wb_tensor(key, src)
                rows, cols = src.shape
                r0 = 0
                while r0 < rows:
                    r1 = min(rows, r0 + 256)
                    c0 = 0
                    while c0 < cols:
                        c1 = min(cols, c0 + 2048)
                        P.add('pool', lambda e, r0=r0, r1=r1, c0=c0, c1=c1, dstt=dstt, src=src: e.dma_start(out=dstt[r0:r1, c0:c1], in_=src[r0:r1, c0:c1]),
                              writes=[res], dma=True)
                        c0 = c1
                    r0 = r1

        def load_w(dst, key, src, rows_chunks, ncols, res_list):
            dstt, _, res = wb_tensor(key, src)
            assert res.last_w is not None, key
            sv = dstt.rearrange('(kc p) n -> p kc n', p=128)
            for kc in range(rows_chunks):
                P.add('sp', lambda e, kc=kc: e.dma_start(out=dst[:, kc, :], in_=sv[:, kc, :]), reads=[res], writes=[res_list[kc]], dma=True)

        class Prep:
            def __init__(self, es, tag):
                sb = lambda name, shape, dt=F32: es.enter_context(_sbuf_tensor(tag + name, list(shape), dt))
                self.hT = [sb('hT%d' % i, [128, KC, 512], BF16) for i in range(2)]
                self.r_hT = [Res() for _ in range(2)]
                self.sq = [sb('sq%d' % i, [128, 512], BF16) for i in range(2)]
                self.r_sq = [Res() for _ in range(2)]
                self.ln = sb('ln', [128, 512])
                self.r_ln = Res()
                self.rstd = sb('rstd', [128, 512])
                self.r_rstd = Res()
                self.tmp = [sb('tmp%d' % i, [128, 512]) for i in range(2)]
                self.r_tmp = [Res() for _ in range(2)]
                self.pst = es.enter_context(_psum_tensor(tag + 'pst', [128, 512], F32))
                self.r_pst = Res()
                self.cnt = 0

            def chunk_a(self, slot, kc, T, xap, xres):
                b = kc % 2
                sq = self.sq[b]
                P.add('act', lambda e: e.activation(out=sq[:, 0:T], in_=xap, func=AF.Square), reads=[xres], writes=[self.r_sq[b]])
                hT = self.hT[slot]
                P.add('pool', lambda e: e.tensor_copy(out=hT[:, kc, 0:T], in_=xap), reads=[xres], writes=[self.r_hT[slot]])

            def chunk_b(self, kc, T):
                b = kc % 2
                sq = self.sq[b]
                P.add('pe', lambda e: e.matmul(self.pst[:, 0:T], lhsT=ones_b, rhs=sq[:, 0:T], start=(kc == 0), stop=(kc == KC - 1)),
                      reads=[self.r_sq[b], r_cstbf], writes=[self.r_pst])

            def chunk_in(self, slot, kc, T, xap, xres):
                self.chunk_a(slot, kc, T, xap, xres)
                self.chunk_b(kc, T)

            def finish_a(self, T):
                P.add('act', lambda e: e.activation(out=self.ln[:, 0:T], in_=self.pst[:, 0:T], func=AF.Ln, bias=RMS_EPS), reads=[self.r_pst], writes=[self.r_ln])
                P.add('act', lambda e: e.activation(out=self.rstd[:, 0:T], in_=self.ln[:, 0:T], func=AF.Exp, scale=-0.5), reads=[self.r_ln], writes=[self.r_rstd])

            def finish_kc(self, slot, kc, T, n, j):
                hT = self.hT[slot]
                b = kc % 2
                tmp = self.tmp[b]
                P.add('dve', lambda e: e.tensor_tensor(out=tmp[:, 0:T], in0=hT[:, kc, 0:T], in1=self.rstd[:, 0:T], op=ALU.mult),
                      reads=[self.r_hT[slot], self.r_rstd], writes=[self.r_tmp[b]])
                P.add('dve', lambda e: e.tensor_scalar(out=hT[:, kc, 0:T], in0=tmp[:, 0:T], scalar1=mt(0, n, kc, j), scalar2=mt(1, n, kc, j), op0=ALU.mult, op1=ALU.add),
                      reads=[self.r_tmp[b], r_mtab], writes=[self.r_hT[slot]])

            def finish(self, slot, T, n, j):
                self.finish_a(T)
                for kc in range(KC):
                    self.finish_kc(slot, kc, T, n, j)

            def steps(self, slot, T, n, j, load_chunk):
                def A(kc):
                    xap, xres = load_chunk(kc)
                    self.chunk_a(slot, kc, T, xap, xres)

                st = [lambda: A(0)]
                for kc in range(1, KC):
                    st.append(lambda kc=kc: (A(kc), self.chunk_b(kc - 1, T)))
                st.append(lambda: self.chunk_b(KC - 1, T))
                st.append(lambda: self.finish_a(T))
                for kc in range(KC):
                    st.append(lambda kc=kc: self.finish_kc(slot, kc, T, n, j))
                return st

        def pass_ffn(l, which, src, dst, skip_ctx=False):
            n = 0 if which == 0 else 2
            with ExitStack() as es:
                sb = lambda name, shape, dt=F32: es.enter_context(_sbuf_tensor(name, list(shape), dt))
                w1 = sb('w1', [128, KC, 2 * DFF], BF16)
                r_w1 = [Res() for _ in range(KC)]
                w2 = sb('w2', [128, FC, D], BF16)
                r_w2 = [Res() for _ in range(FC)]
                act = sb('actb', [128, FC, 512], BF16)
                r_act = [Res() for _ in range(FC)]
                xin = [sb('xin%d' % i, [128, 512]) for i in range(3)]
                r_xin = [Res() for _ in range(3)]
                xrs = [sb('xrs%d' % i, [128, 512]) for i in range(2)]
                r_xrs = [Res() for _ in range(2)]
                xo = [sb('xo%d' % i, [128, 512]) for i in range(2)]
                r_xo = [Res() for _ in range(2)]
                sg = [sb('sg%d' % i, [128, 512], BF16) for i in range(2)]
                r_sg = [Res() for _ in range(2)]
                prep = Prep(es, 'f')
                pg = [es.enter_context(_psum_tensor('pg%d' % i, [128, 512], F32)) for i in range(2)]
                pu = [es.enter_context(_psum_tensor('pu%d' % i, [128, 512], F32)) for i in range(2)]
                po = [es.enter_context(_psum_tensor('po%d' % i, [128, 512], F32)) for i in range(2)]
                r_pg = [Res() for _ in range(2)]
                r_pu = [Res() for _ in range(2)]
                r_po = [Res() for _ in range(2)]
                load_w(w1, 'w1_%d_%d' % (l, which), w1_in[l, which], KC, 2 * DFF, r_w1)
                load_w(w2, 'w2_%d_%d' % (l, which), w2_in[l, which], FC, D, r_w2)
                conv_issue()
                my_tiles = [(ti, t) for ti, t in enumerate(tiles) if not (skip_ctx and t[2] == 1)]
                srcv = src.rearrange('(kc p) t -> p kc t', p=128)
                dstv = dst.rearrange('(kc p) t -> p kc t', p=128)
                cnt = {'xin': 0, 'x2': 0}

                def prepare_steps(k):
                    ti, (t0, T, j) = my_tiles[k]
                    slot = k % 2

                    def load_chunk(kc):
                        b = cnt['xin'] % 3
                        cnt['xin'] += 1
                        P.add('sp', lambda e: e.dma_start(out=xin[b][:, 0:T], in_=srcv[:, kc, t0:t0 + T]), reads=[DR(id(src), ti)], writes=[r_xin[b]], dma=True)
                        return xin[b][:, 0:T], r_xin[b]

                    return prep.steps(slot, T, n, j, load_chunk)

                def prepare(k):
                    for st_ in prepare_steps(k):
                        st_()

                def do_tile(k):
                    ti, (t0, T, j) = my_tiles[k]
                    nsteps = prepare_steps(k + 1) if k + 1 < len(my_tiles) else []
                    slot = k % 2
                    hT = prep.hT[slot]
                    r_hT = prep.r_hT[slot]
                    for mo in range(FC):
                        pb = mo % 2
                        for kc in range(KC):
                            P.add('pe', lambda e, pb=pb, mo=mo, kc=kc: e.matmul(pg[pb][:, 0:T], lhsT=w1[:, kc, mo * 128:(mo + 1) * 128], rhs=hT[:, kc, 0:T],
                                                                               start=(kc == 0), stop=(kc == KC - 1)),
                                  reads=[r_w1[kc], r_hT], writes=[r_pg[pb]])
                        for kc in range(KC):
                            P.add('pe', lambda e, pb=pb, mo=mo, kc=kc: e.matmul(pu[pb][:, 0:T], lhsT=w1[:, kc, DFF + mo * 128:DFF + (mo + 1) * 128], rhs=hT[:, kc, 0:T],
                                                                               start=(kc == 0), stop=(kc == KC - 1)),
                                  reads=[r_w1[kc], r_hT], writes=[r_pu[pb]])
                        P.add('act', lambda e, pb=pb: e.activation(out=sg[pb][:, 0:T], in_=pg[pb][:, 0:T], func=AF.Silu), reads=[r_pg[pb]], writes=[r_sg[pb]])
                        P.add('dve', lambda e, pb=pb, mo=mo: e.tensor_tensor(out=act[:, mo, 0:T], in0=pu[pb][:, 0:T], in1=sg[pb][:, 0:T], op=ALU.mult),
                              reads=[r_pu[pb], r_sg[pb]], writes=[r_act[mo]])
                        if nsteps and mo >= 1:
                            nsteps.pop(0)()
                    while nsteps:
                        nsteps.pop(0)()
                    xsrc = src
                    xsv = xsrc.rearrange('(kc p) t -> p kc t', p=128)
                    def xrs_load(mo_):
                        pb_ = mo_ % 2
                        P.add('pool', lambda e: e.dma_start(out=xrs[pb_][:, 0:T], in_=xsv[:, mo_, t0:t0 + T]), reads=[DR(id(xsrc), ti)], writes=[r_xrs[pb_]], dma=True)

                    xrs_load(0)
                    xrs_load(1)
                    for mo in range(KC):
                        pb = mo % 2
                        for kc in range(FC):
                            P.add('pe', lambda e, pb=pb, mo=mo, kc=kc: e.matmul(po[pb][:, 0:T], lhsT=w2[:, kc, mo * 128:(mo + 1) * 128], rhs=act[:, kc, 0:T],
                                                                               start=(kc == 0), stop=(kc == FC - 1)),
                                  reads=[r_w2[kc], r_act[kc]], writes=[r_po[pb]])
                        P.add('dve', lambda e, pb=pb, mo=mo: e.scalar_tensor_tensor(out=xo[pb][:, 0:T], in0=po[pb][:, 0:T], scalar=mt(2, n, mo, j), in1=xrs[pb][:, 0:T],
                                                                                   op0=ALU.mult, op1=ALU.add),
                              reads=[r_po[pb], r_xrs[pb], r_mtab], writes=[r_xo[pb]])
                        P.add('pool', lambda e, pb=pb, mo=mo: e.dma_start(out=dstv[:, mo, t0:t0 + T], in_=xo[pb][:, 0:T]), reads=[r_xo[pb]], writes=[DR(id(dst), ti)], dma=True)
                        if mo + 2 < KC:
                            xrs_load(mo + 2)

                prepare(0)
                for k in range(len(my_tiles)):
                    do_tile(k)
                P.emit_pass()

        def pass_inproj(l, src):
            with ExitStack() as es:
                sb = lambda name, shape, dt=F32: es.enter_context(_sbuf_tensor(name, list(shape), dt))
                win = sb('win', [128, KC, INX], BF16)
                r_win = [Res() for _ in range(KC)]
                load_w(win, 'win_%d' % l, win_in[l], KC, INX, r_win)
                conv_issue()
                prep = Prep(es, 'i')
                xin = [sb('xin%d' % i, [128, 512]) for i in range(3)]
                r_xin = [Res() for _ in range(3)]
                rC = [sb('rC%d' % i, [128, 512]) for i in range(2)]
                rS = [sb('rS%d' % i, [128, 512]) for i in range(2)]
                r_rope = [Res() for _ in range(2)]
                qkw = sb('qkw', [128, 4])
                qkws = sb('qkws', [128, 2])
                r_qkw = Res()
                lnw = sb('lnw', [128, 256])
                lnb = sb('lnb', [128, 256])
                r_ln = Res()
                P.add('sp', lambda e: e.dma_start(out=qkw[:], in_=qkn_in), writes=[r_qkw], dma=True)
                P.add('dve', lambda e: e.tensor_scalar(out=qkws[:, 0:1], in0=qkw[:, 2 * l:2 * l + 1], scalar1=0.125, scalar2=None, op0=ALU.mult), reads=[r_qkw], writes=[r_qkw])
                P.add('dve', lambda e: e.tensor_copy(out=qkws[:, 1:2], in_=qkw[:, 2 * l + 1:2 * l + 2]), reads=[r_qkw], writes=[r_qkw])
                P.add('sp', lambda e: e.dma_start(out=lnw[:], in_=lnw_in[:, l * 256:(l + 1) * 256]), writes=[r_ln], dma=True)
                P.add('sp', lambda e: e.dma_start(out=lnb[:], in_=lnb_in[:, l * 256:(l + 1) * 256]), writes=[r_ln], dma=True)
                st_q = [sb('stq%d' % i, [128, 3, 512], BF16) for i in range(2)]
                st_k = [sb('stk%d' % i, [128, 3, 512], BF16) for i in range(2)]
                st_u = [sb('stu%d' % i, [128, 2, 512], BF16) for i in range(2)]
                st_rq = [sb('strq%d' % i, [128, 3, 512], BF16) for i in range(2)]
                st_rk = [sb('strk%d' % i, [128, 3, 512], BF16) for i in range(2)]
                r_stq = [Res() for _ in range(2)]; r_stk = [Res() for _ in range(2)]; r_stu = [Res() for _ in range(2)]
                r_strq = [Res() for _ in range(2)]; r_strk = [Res() for _ in range(2)]
                st_v = [sb('stv%d' % i, [128, 4, 384], BF16) for i in range(2)]
                st_rv = [sb('strv%d' % i, [128, 4, 384], BF16) for i in range(2)]
                st_g = [sb('stg%d' % i, [128, 4, 768], BF16) for i in range(2)]
                st_vn = [sb('stvn%d' % i, [128, 4, 256], BF16) for i in range(2)]
                r_stv = [[Res() for _ in range(4)] for _ in range(2)]; r_strv = [[Res() for _ in range(4)] for _ in range(2)]; r_stg = [[Res() for _ in range(4)] for _ in range(2)]; r_stvn = [[Res() for _ in range(4)] for _ in range(2)]
                sqb = [sb('qsq%d' % i, [128, 512], BF16) for i in range(2)]
                r_sqb = [Res() for _ in range(2)]
                lnq = [sb('lnq%d' % i, [128, 512]) for i in range(2)]
                r_lnq = [Res() for _ in range(2)]
                rsq = [sb('rsq%d' % i, [128, 512]) for i in range(2)]
                r_rsq = [Res() for _ in range(2)]
                t1 = [sb('t1%d' % i, [128, 512]) for i in range(2)]
                t2 = [sb('t2%d' % i, [128, 512]) for i in range(2)]
                r_t1 = [Res() for _ in range(2)]; r_t2 = [Res() for _ in range(2)]
                gv = [sb('gv%d' % i, [128, 256]) for i in range(2)]
                r_gv = [Res() for _ in range(2)]
                gsq = sb('gsq', [128, 256]); r_gsq = Res()
                cen = sb('cen', [128, 256]); r_cen = Res()
                sm = sb('sm', [128, 24]); r_sm = Res()
                pf = [es.enter_context(_psum_tensor('pf%d' % i, [128, 512], F32)) for i in range(4)]
                r_pf = [Res() for _ in range(4)]
                pt = [es.enter_context(_psum_tensor('pt%d' % i, [128, 512], F32)) for i in range(3)]
                r_pt = [Res() for _ in range(3)]
                srcv = src.rearrange('(kc p) t -> p kc t', p=128)
                cnt = {'xin': 0, 'pf': 0, 'pt': 0, 'w': 0}

                def prepare_steps(k):
                    ti, (t0, T, j) = k, tiles[k]
                    slot = k % 2

                    def load_chunk(kc):
                        b = cnt['xin'] % 3
                        cnt['xin'] += 1
                        P.add('sp', lambda e: e.dma_start(out=xin[b][:, 0:T], in_=srcv[:, kc, t0:t0 + T]), reads=[DR(id(src), ti)], writes=[r_xin[b]], dma=True)
                        return xin[b][:, 0:T], r_xin[b]

                    return prep.steps(slot, T, 1, j, load_chunk)

                def prepare(k):
                    for st_ in prepare_steps(k):
                        st_()

                nsteps = []

                def pop_step():
                    if nsteps:
                        nsteps.pop(0)()

                def fm_mm(colbase, c, T, hT, r_hT):
                    pb = cnt['pf'] % 4
                    cnt['pf'] += 1
                    c0 = colbase + c * 128
                    for kc in range(KC):
                        P.add('pe', lambda e, kc=kc: e.matmul(pf[pb][:, 0:T], lhsT=win[:, kc, c0:c0 + 128], rhs=hT[:, kc, 0:T], start=(kc == 0), stop=(kc == KC - 1)),
                              reads=[r_win[kc], r_hT], writes=[r_pf[pb]])
                    pop_step()
                    return pb

                def do_tile(k):
                    ti, (t0, T, j) = k, tiles[k]
                    slot = k % 2
                    hT = prep.hT[slot]
                    r_hT = prep.r_hT[slot]
                    sl = k % 2
                    P.add('sp', lambda e: e.dma_start(out=rC[sl][:, 0:T], in_=ropeC[:, t0:t0 + T]), writes=[r_rope[sl]], dma=True)
                    P.add('sp', lambda e: e.dma_start(out=rS[sl][:, 0:T], in_=ropeS[:, t0:t0 + T]), writes=[r_rope[sl]], dma=True)
                    def qk_a(colbase, c):
                        A = fm_mm(colbase, c, T, hT, r_hT)
                        w = cnt['w'] % 2
                        cnt['w'] += 1
                        P.add('act', lambda e: e.activation(out=sqb[w][:, 0:T], in_=pf[A][:, 0:T], func=AF.Square), reads=[r_pf[A]], writes=[r_sqb[w]])
                        return A, w

                    def qk_b(c, stg, r_stg_, wi, A, w):
                        B = cnt['pf'] % 4
                        cnt['pf'] += 1
                        P.add('pe', lambda e: e.matmul(pf[B][:, 0:T], lhsT=bd_b, rhs=sqb[w][:, 0:T], start=True, stop=True), reads=[r_sqb[w], r_cstbf], writes=[r_pf[B]])
                        P.add('act', lambda e: e.activation(out=lnq[w][:, 0:T], in_=pf[B][:, 0:T], func=AF.Ln, bias=RMS_EPS), reads=[r_pf[B]], writes=[r_lnq[w]])
                        P.add('act', lambda e: e.activation(out=rsq[w][:, 0:T], in_=lnq[w][:, 0:T], func=AF.Exp, scale=-0.5), reads=[r_lnq[w]], writes=[r_rsq[w]])
                        P.add('dve', lambda e: e.scalar_tensor_tensor(out=stg[:, c, 0:T], in0=pf[A][:, 0:T], scalar=qkws[:, wi:wi + 1], in1=rsq[w][:, 0:T], op0=ALU.mult, op1=ALU.mult),
                              reads=[r_pf[A], r_rsq[w], r_qkw], writes=[r_stg_])

                    items = [(0, c, st_q[sl], r_stq[sl], 0) for c in range(3)] + [(384, c, st_k[sl], r_stk[sl], 1) for c in range(3)]
                    prev = None
                    for (colbase, c, stg, r_stg_, wi) in items:
                        A, w = qk_a(colbase, c)
                        if prev is not None:
                            qk_b(*prev)
                        prev = (c, stg, r_stg_, wi, A, w)
                    qk_b(*prev)
                    def fm_store(stg, r_stg_, dten):
                        dv = dten.rearrange('(c p) t -> p c t', p=128)[:, :, t0:t0 + T]
                        P.add('pool', lambda e: e.dma_start(out=dv, in_=stg[:, :, 0:T]), reads=[r_stg_], writes=[DR(id(dten), ti)], dma=True)

                    fm_store(st_q[sl], r_stq[sl], qT_na)
                    fm_store(st_k[sl], r_stk[sl], kT_na)
                    for c in range(2):
                        A = fm_mm(1152, c, T, hT, r_hT)
                        P.add('act', lambda e, A=A, c=c: e.activation(out=st_u[sl][:, c, 0:T], in_=pf[A][:, 0:T], func=AF.Gelu_apprx_tanh), reads=[r_pf[A]], writes=[r_stu[sl]])
                    fm_store(st_u[sl], r_stu[sl], uT_sg)
                    for (colbase, pbase, stg, r_stg_) in ((1664, 3584, st_rq[sl], r_strq[sl]), (2048, 3968, st_rk[sl], r_strk[sl])):
                        for c in range(3):
                            A = fm_mm(colbase, c, T, hT, r_hT)
                            A2 = fm_mm(pbase, c, T, hT, r_hT)
                            w = cnt['w'] % 2
                            cnt['w'] += 1
                            P.add('dve', lambda e, A=A, w=w: e.tensor_tensor(out=t1[w][:, 0:T], in0=pf[A][:, 0:T], in1=rC[sl][:, 0:T], op=ALU.mult), reads=[r_pf[A], r_rope[sl]], writes=[r_t1[w]])
                            P.add('dve', lambda e, A2=A2, w=w: e.tensor_tensor(out=t2[w][:, 0:T], in0=pf[A2][:, 0:T], in1=rS[sl][:, 0:T], op=ALU.mult), reads=[r_pf[A2], r_rope[sl]], writes=[r_t2[w]])
                            P.add('pool', lambda e, w=w, c=c, stg=stg: e.tensor_tensor(out=stg[:, c, 0:T], in0=t1[w][:, 0:T], in1=t2[w][:, 0:T], op=ALU.add), reads=[r_t1[w], r_t2[w]], writes=[r_stg_])
                        fm_store(stg, r_stg_, rqT if colbase == 1664 else rkT)
                    def do_sub(s):
                        def tm_mm(c0, ncol):
                            pb = cnt['pt'] % 3
                            cnt['pt'] += 1
                            for kc in range(KC):
                                P.add('pe', lambda e, kc=kc: e.matmul(pt[pb][:, 0:ncol], lhsT=hT[:, kc, s * 128:(s + 1) * 128], rhs=win[:, kc, c0:c0 + ncol], start=(kc == 0), stop=(kc == KC - 1)),
                                      reads=[r_win[kc], r_hT], writes=[r_pt[pb]])
                            return pb
                        A = tm_mm(768, 384)
                        P.add('act', lambda e, A=A: e.copy(out=st_v[sl][:, s, :], in_=pt[A][:, 0:384]), reads=[r_pt[A]], writes=[r_stv[sl][s]])
                        A = tm_mm(2432, 384)
                        P.add('dve', lambda e, A=A: e.tensor_copy(out=st_rv[sl][:, s, :], in_=pt[A][:, 0:384]), reads=[r_pt[A]], writes=[r_strv[sl][s]])
                        A = tm_mm(2816, 384)
                        P.add('act', lambda e, A=A: e.activation(out=st_g[sl][:, s, 0:384], in_=pt[A][:, 0:384], func=AF.Silu), reads=[r_pt[A]], writes=[r_stg[sl][s]])
                        A = tm_mm(3200, 384)
                        P.add('act', lambda e, A=A: e.activation(out=st_g[sl][:, s, 384:768], in_=pt[A][:, 0:384], func=AF.Silu), reads=[r_pt[A]], writes=[r_stg[sl][s]])
                        A = tm_mm(1408, 256)
                        g = cnt['w'] % 2
                        cnt['w'] += 1
                        P.add('act', lambda e, A=A, g=g: e.activation(out=gv[g][:], in_=pt[A][:, 0:256], func=AF.Gelu_apprx_tanh), reads=[r_pt[A]], writes=[r_gv[g]])
                        g3 = lambda ap: ap.rearrange('p (g c) -> p g c', g=4)
                        bc = lambda ap: ap.unsqueeze(2).broadcast_to([128, 4, 64])
                        P.add('dve', lambda e, g=g: e.tensor_reduce(out=sm[:, 0:4], in_=g3(gv[g][:]), axis=AX.X, op=ALU.add), reads=[r_gv[g]], writes=[r_sm])
                        P.add('dve', lambda e, g=g: e.tensor_tensor(out=gsq[:], in0=gv[g][:], in1=gv[g][:], op=ALU.mult), reads=[r_gv[g]], writes=[r_gsq])
                        P.add('dve', lambda e: e.tensor_reduce(out=sm[:, 4:8], in_=g3(gsq[:]), axis=AX.X, op=ALU.add), reads=[r_gsq], writes=[r_sm])
                        P.add('dve', lambda e: e.tensor_scalar(out=sm[:, 8:12], in0=sm[:, 0:4], scalar1=1.0 / 64, scalar2=None, op0=ALU.mult), reads=[r_sm], writes=[r_sm])
                        P.add('dve', lambda e: e.tensor_tensor(out=sm[:, 12:16], in0=sm[:, 8:12], in1=sm[:, 8:12], op=ALU.mult), reads=[r_sm], writes=[r_sm])
                        P.add('dve', lambda e: e.scalar_tensor_tensor(out=sm[:, 16:20], in0=sm[:, 4:8], scalar=1.0 / 64, in1=sm[:, 12:16], op0=ALU.mult, op1=ALU.subtract), reads=[r_sm], writes=[r_sm])
                        P.add('act', lambda e: e.activation(out=sm[:, 16:20], in_=sm[:, 16:20], func=AF.Sqrt, bias=LN_EPS), reads=[r_sm], writes=[r_sm])
                        P.add('dve', lambda e: e.reciprocal(out=sm[:, 20:24], in_=sm[:, 16:20]), reads=[r_sm], writes=[r_sm])
                        P.add('dve', lambda e, g=g: e.tensor_tensor(out=g3(cen[:]), in0=g3(gv[g][:]), in1=bc(sm[:, 8:12]), op=ALU.subtract), reads=[r_gv[g], r_sm], writes=[r_cen])
                        P.add('dve', lambda e: e.tensor_tensor(out=g3(cen[:]), in0=g3(cen[:]), in1=bc(sm[:, 20:24]), op=ALU.mult), reads=[r_cen, r_sm], writes=[r_cen])
                        P.add('pool', lambda e: e.tensor_tensor(out=cen[:], in0=cen[:], in1=lnw[:], op=ALU.mult), reads=[r_cen, r_ln], writes=[r_cen])
                        P.add('pool', lambda e: e.tensor_tensor(out=st_vn[sl][:, s, :], in0=cen[:], in1=lnb[:], op=ALU.add), reads=[r_cen, r_ln], writes=[r_stvn[sl][s]])
                        for (stg, r_stg_, dten) in ((st_v[sl], r_stv[sl][s], v_na), (st_rv[sl], r_strv[sl][s], rv_d), (st_g[sl], r_stg[sl][s], gates_d), (st_vn[sl], r_stvn[sl][s], vn_sg)):
                            P.add('pool', lambda e, stg=stg, dten=dten: e.dma_start(out=dten[t0 + s * 128:t0 + (s + 1) * 128, :], in_=stg[:, s, :]), reads=[r_stg_], writes=[DR(id(dten), ti)], dma=True)
                    for s_ in range(T // 128):
                        do_sub(s_)

                prepare(0)
                for k in range(len(tiles)):
                    if k + 1 < len(tiles):
                        nsteps.extend(prepare_steps(k + 1))
                    do_tile(k)
                    while nsteps:
                        nsteps.pop(0)()
                P.emit_pass()

        def pass_outproj(l, src, dst, skip_ctx):
            with ExitStack() as es:
                sb = lambda name, shape, dt=F32: es.enter_context(_sbuf_tensor(name, list(shape), dt))
                wo = sb('wo', [128, KC, D], BF16)
                r_wo = [Res() for _ in range(KC)]
                load_w(wo, 'wo_%d' % l, wout_in[l], KC, D, r_wo)
                conv_issue()
                ct = [sb('ct%d' % i, [128, KC, 512], BF16) for i in range(2)]
                r_ct = [Res() for _ in range(2)]
                xin = [sb('xin%d' % i, [128, 512]) for i in range(3)]
                r_xin = [Res() for _ in range(3)]
                xo = [sb('xo%d' % i, [128, 512]) for i in range(3)]
                r_xo = [Res() for _ in range(3)]
                po = [es.enter_context(_psum_tensor('po%d' % i, [128, 512], F32)) for i in range(3)]
                r_po = [Res() for _ in range(3)]
                srcv = src.rearrange('(kc p) t -> p kc t', p=128)
                dstv = dst.rearrange('(kc p) t -> p kc t', p=128)
                catv = catT.rearrange('(kc p) t -> p kc t', p=128)
                cnt = {'i': 0}

                def do_tile(ti):
                    t0, T, j = tiles[ti]
                    sl = ti % 2
                    P.add('sp', lambda e: e.dma_start(out=ct[sl][:, :, 0:T], in_=catv[:, :, t0:t0 + T]), reads=[DR(id(catT), ti)], writes=[r_ct[sl]], dma=True)
                    for mo in range(KC):
                        b = cnt['i'] % 3
                        cnt['i'] += 1
                        P.add('sp', lambda e, b=b, mo=mo: e.dma_start(out=xin[b][:, 0:T], in_=srcv[:, mo, t0:t0 + T]), reads=[DR(id(src), ti)], writes=[r_xin[b]], dma=True)
                        for kc in range(KC):
                            P.add('pe', lambda e, b=b, mo=mo, kc=kc: e.matmul(po[b][:, 0:T], lhsT=wo[:, kc, mo * 128:(mo + 1) * 128], rhs=ct[sl][:, kc, 0:T], start=(kc == 0), stop=(kc == KC - 1)),
                                  reads=[r_wo[kc], r_ct[sl]], writes=[r_po[b]])
                        P.add('dve', lambda e, b=b, mo=mo: e.scalar_tensor_tensor(out=xo[b][:, 0:T], in0=po[b][:, 0:T], scalar=mt(2, 1, mo, j), in1=xin[b][:, 0:T], op0=ALU.mult, op1=ALU.add),
                              reads=[r_po[b], r_xin[b], r_mtab], writes=[r_xo[b]])
                        P.add('pool', lambda e, b=b, mo=mo: e.dma_start(out=dstv[:, mo, t0:t0 + T], in_=xo[b][:, 0:T]), reads=[r_xo[b]], writes=[DR(id(dst), ti)], dma=True)

                for ti in range(len(tiles)):
                    if skip_ctx and tiles[ti][2] == 1:
                        continue
                    do_tile(ti)
                P.emit_pass()

        def pass_mixer(l, last):
            with ExitStack() as es:
                conv_issue()
                sb = lambda name, shape, dt=F32: es.enter_context(_sbuf_tensor(name, list(shape), dt))
                E = sb('E', [128, NV, 6, 5, 128], BF16)
                r_E = Res()
                Sst = sb('Sst', [128, NCH, 2, 3, 64], BF16)
                r_Sst = [[Res(), Res()] for _ in range(NCH)]
                dl = sb('dl', [128, 12]); e1 = sb('e1', [128, 12]); lg = sb('lg', [128, 12]); nlg = sb('nlg', [128, 12])
                lgsel = sb('lgsel', [128, 2, 3]); g128 = sb('g128', [128, 2, 3]); te = sb('te', [128, 2, 6])
                fs = sb('fs', [128, 2, 3, 128]); fsm = sb('fsm', [128, 2, 6, 128]); Dm = sb('Dm', [128, 2, 6, 128])
                r_tab = Res()
                st = sb('st', [128, 2, 3, 64])
                r_st = [Res(), Res()]
                sgw = sb('sgw', [128, 4, 128], BF16); sgbt = sb('sgbt', [128, 2, 128]); gnw = sb('gnw', [128, 384])
                r_sgt = Res()
                pS = es.enter_context(_psum_tensor('pS', [128, 1024], F32)); r_pS = Res()
                pS2 = es.enter_context(_psum_tensor('pS2', [128, 1024], F32)); r_pS2 = Res()
                pO = es.enter_context(_psum_tensor('pO', [128, 512], F32)); r_pO = Res()
                pRo = es.enter_context(_psum_tensor('pRo', [128, 1024], F32)); r_pRo = Res()
                pT = es.enter_context(_psum_tensor('pT', [128, 1024], BF16)); r_pT = Res()
                pG = pRo[:, 768:1024]; r_pG = Res()
                pKV = pS2[:, 0:512]; r_pKV = r_pS2

                P.add('sp', lambda e: e.dma_start(out=dl[:], in_=dlog_in[:, l * 12:(l + 1) * 12]), writes=[r_tab], dma=True)
                P.add('act', lambda e: e.activation(out=e1[:], in_=dl[:], func=AF.Exp, scale=-1.0), reads=[r_tab], writes=[r_tab])
                P.add('act', lambda e: e.activation(out=nlg[:], in_=e1[:], func=AF.Ln, bias=1.0), reads=[r_tab], writes=[r_tab])
                P.add('dve', lambda e: e.tensor_scalar(out=lg[:], in0=nlg[:], scalar1=-1.0, scalar2=None, op0=ALU.mult), reads=[r_tab], writes=[r_tab])
                for d_ in range(2):
                    v2 = lg[:, d_ * 6:(d_ + 1) * 6].rearrange('p (r two) -> p r two', two=2)
                    P.add('dve', lambda e, d_=d_, v2=v2: e.tensor_copy(out=lgsel[0:64, d_, :], in_=v2[0:64, :, 0]), reads=[r_tab], writes=[r_tab])
                    P.add('dve', lambda e, d_=d_, v2=v2: e.tensor_copy(out=lgsel[64:128, d_, :], in_=v2[64:128, :, 1]), reads=[r_tab], writes=[r_tab])
                P.add('act', lambda e: e.activation(out=g128[:], in_=lgsel[:], func=AF.Exp, scale=128.0), reads=[r_tab], writes=[r_tab])
                P.add('act', lambda e: e.activation(out=te[:, 0, :], in_=lg[:, 0:6], func=AF.Exp, scale=cst[:, C_PREV:C_PREV + 1]), reads=[r_tab, r_cst], writes=[r_tab])
                P.add('act', lambda e: e.activation(out=te[:, 1, :], in_=lg[:, 6:12], func=AF.Exp, scale=cst[:, C_PCOL:C_PCOL + 1]), reads=[r_tab, r_cst], writes=[r_tab])
                for pr in range(3):
                    P.add('act', lambda e, pr=pr: e.activation(out=fs[:, 0, pr, :], in_=cst[:, C_IP1:C_IP1 + 128], func=AF.Exp, scale=lgsel[:, 0, pr:pr + 1], bias=lnb8[:, 0:1]),
                          reads=[r_tab, r_cst], writes=[r_tab])
                    P.add('act', lambda e, pr=pr: e.activation(out=fs[:, 1, pr, :], in_=cst[:, C_REV:C_REV + 128], func=AF.Exp, scale=lgsel[:, 1, pr:pr + 1], bias=lnb8[:, 0:1]),
                          reads=[r_tab, r_cst], writes=[r_tab])
                P.add('dve', lambda e: e.memset(fsm[:], 0.0), writes=[r_tab])
                for d_ in range(2):
                    for h in range(6):
                        r0_ = (h % 2) * 64
                        P.add('dve', lambda e, d_=d_, h=h, r0_=r0_: e.tensor_copy(out=fsm[r0_:r0_ + 64, d_, h, :], in_=fs[r0_:r0_ + 64, d_, h // 2, :]), reads=[r_tab], writes=[r_tab])
                for h in range(6):
                    P.add('act', lambda e, h=h: e.activation(out=Dm[:, 0, h, :], in_=cst[:, C_DIFF:C_DIFF + 128], func=AF.Exp, scale=lg[:, h:h + 1], bias=lnb8[:, 0:1]),
                          reads=[r_tab, r_cst], writes=[r_tab])
                    P.add('act', lambda e, h=h: e.activation(out=Dm[:, 1, h, :], in_=cst[:, C_DIFF:C_DIFF + 128], func=AF.Exp, scale=nlg[:, 6 + h:7 + h], bias=lnb8[:, 0:1]),
                          reads=[r_tab, r_cst], writes=[r_tab])
                mFb = cst[:, C_MF:C_MF + 128].unsqueeze(1).broadcast_to([128, 6, 128])
                mBb = cst[:, C_MB:C_MB + 128].unsqueeze(1).broadcast_to([128, 6, 128])
                P.add('dve', lambda e: e.tensor_tensor(out=Dm[:, 0, :, :], in0=Dm[:, 0, :, :], in1=mFb, op=ALU.mult), reads=[r_tab, r_cst], writes=[r_tab])
                P.add('dve', lambda e: e.tensor_tensor(out=Dm[:, 1, :, :], in0=Dm[:, 1, :, :], in1=mBb, op=ALU.mult), reads=[r_tab, r_cst], writes=[r_tab])
                bst = [sb('bst%d' % i, [128, 5, 128]) for i in range(2)]
                r_bst = [Res(), Res()]
                for v in range(NV):
                    for h in range(6):
                        b = (v * 6 + h) % 2
                        P.add('sp', lambda e, b=b, v=v, h=h: e.dma_start(out=bst[b][:], in_=bias_in[l, v, h]), writes=[r_bst[b]], dma=True)
                        P.add('act', lambda e, b=b, v=v, h=h: e.activation(out=E[:, v, h, :, :], in_=bst[b][:], func=AF.Exp), reads=[r_bst[b]], writes=[r_E])
                P.add('pool', lambda e: e.dma_start(out=sgw[:], in_=sgw_in[l].rearrange('g q p -> q g p')), writes=[r_sgt], dma=True)
                P.add('sp', lambda e: e.dma_start(out=sgbt[:], in_=sgb_in[l].rearrange('g p f -> p g f')), writes=[r_sgt], dma=True)
                P.add('sp', lambda e: e.dma_start(out=gnw[:], in_=gnw_in[:, l * 384:(l + 1) * 384]), writes=[r_sgt], dma=True)

                qv = lambda ten: ten.rearrange('(c p) t -> p c t', p=128)
                rkb = [sb('rkb%d' % i, [128, 3, 128], BF16) for i in range(2)]
                rvb = [sb('rvb%d' % i, [128, 384], BF16) for i in range(2)]
                r_rkb = [Res(), Res()]; r_rvb = [Res(), Res()]
                kte = [sb('kte%d' % i, [128, 384], BF16) for i in range(2)]
                r_kte = [Res(), Res()]
                P.add('dve', lambda e: e.memset(st[:], 0.0), writes=[r_st[0], r_st[1]])
                order_f = [NLC, NLC + 1] + list(range(NLC))
                order_b = [NLC + 1, NLC] + list(range(NLC - 1, -1, -1))
                sc = {'i': 0}

                def scan_step(dr, c):
                    i = sc['i'] % 2
                    sc['i'] += 1
                    tok = chunk_tok(c)
                    tl = tile_of_chunk(c)
                    P.add('act', lambda e: e.copy(out=Sst[:, c, dr, :, :], in_=st[:, dr, :, :]), reads=[r_st[dr]], writes=[r_Sst[c][dr]])
                    P.add('sp', lambda e: e.dma_start(out=rkb[i][:], in_=qv(rkT)[:, :, tok:tok + 128]), reads=[DR(id(rkT), tl)], writes=[r_rkb[i]], dma=True)
                    P.add('sp', lambda e: e.dma_start(out=rvb[i][:], in_=rv_d[tok:tok + 128, :]), reads=[DR(id(rv_d), tl)], writes=[r_rvb[i]], dma=True)
                    for pr in range(3):
                        P.add('pe', lambda e, pr=pr: e.transpose(pT[:, pr * 128:(pr + 1) * 128], rkb[i][:, pr, :], ident_b), reads=[r_rkb[i], r_cstbf], writes=[r_pT])
                    P.add('dve', lambda e: e.tensor_tensor(out=kte[i][:].rearrange('p (h d) -> p h d', h=6), in0=pT[:, 0:384].rearrange('p (h d) -> p h d', h=6),
                                                           in1=te[:, dr, :].unsqueeze(2).broadcast_to([128, 6, 64]), op=ALU.mult),
                          reads=[r_pT, r_tab], writes=[r_kte[i]])
                    for pr in range(3):
                        P.add('pe', lambda e, pr=pr: e.matmul(pS2[:, pr * 128:(pr + 1) * 128], lhsT=kte[i][:, pr * 128:(pr + 1) * 128], rhs=rvb[i][:, pr * 128:(pr + 1) * 128], start=True, stop=True),
                              reads=[r_kte[i], r_rvb[i]], writes=[r_pKV])
                    P.add('dve', lambda e: e.tensor_tensor(out=st[:, dr, :, :], in0=st[:, dr, :, :], in1=g128[:, dr, :].unsqueeze(2).broadcast_to([128, 3, 64]), op=ALU.mult),
                          reads=[r_st[dr], r_tab], writes=[r_st[dr]])
                    kv3 = pS2[:, 0:384].rearrange('p (r c) -> p r c', r=3)
                    P.add('dve', lambda e: e.tensor_tensor(out=st[0:64, dr, :, :], in0=st[0:64, dr, :, :], in1=kv3[0:64, :, 0:64], op=ALU.add), reads=[r_st[dr], r_pKV], writes=[r_st[dr]])
                    P.add('dve', lambda e: e.tensor_tensor(out=st[64:128, dr, :, :], in0=st[64:128, dr, :, :], in1=kv3[64:128, :, 64:128], op=ALU.add), reads=[r_st[dr], r_pKV], writes=[r_st[dr]])

                if 'scan' in MIXDBG:
                    for c in order_f:
                        scan_step(0, c)
                    for c in order_b:
                        scan_step(1, c)

                NSLOT = 8
                kslot = [sb('ks%d' % i, [128, 3, 128], BF16) for i in range(NSLOT + 2)]
                vslot = [sb('vs%d' % i, [128, 6, 65], BF16) for i in range(NSLOT + 2)]
                r_slot = [Res() for _ in range(NSLOT + 2)]
                slot_chunk = [None] * (NSLOT + 2)
                for i in range(NSLOT + 2):
                    P.add('pool', lambda e, i=i: e.memset(vslot[i][:], 1.0), writes=[r_slot[i]])
                dbl = lambda name, shape, dt=BF16: [sb(name + '%d' % i, shape, dt) for i in range(2)]
                qna = dbl('qna', [128, 3, 128]); uu = dbl('uu', [128, 2, 128]); vnb = dbl('vnb', [128, 256])
                rqb = dbl('rqb', [128, 3, 128]); rkc = dbl('rkc', [128, 3, 128]); rvc = dbl('rvc', [128, 384]); gtb = dbl('gtb', [128, 768])
                r_ld = [Res(), Res()]
                expS = dbl('expS', [128, 7, 128]); r_expS = [Res(), Res()]
                qfs = dbl('qfs', [128, 2, 6, 128]); r_qfs = [Res(), Res()]
                SD = dbl('SD', [128, 2, 6, 128]); r_SD = [Res(), Res()]
                natok = dbl('natok', [128, 384]); r_natok = [Res(), Res()]
                rettok = dbl('rettok', [128, 384]); r_rettok = [Res(), Res()]
                catst = dbl('catst', [128, 8, 128]); r_catst = [Res(), Res()]
                rec = dbl('rec', [128, 6], F32); r_rec = [Res(), Res()]
                sgt = dbl('sgtmp', [128, 2, 128], F32); r_sgtmp = [Res(), Res()]
                osb = dbl('osb', [128, 768], F32); r_osb = [Res(), Res()]
                osq = dbl('osq', [128, 768], F32); r_osq = [Res(), Res()]
                gsm = dbl('gsm', [128, 72], F32); r_gsm = [Res(), Res()]
                g2 = dbl('g2', [128, 768], F32); r_g2 = [Res(), Res()]
                g3t = dbl('g3t', [128, 384], F32); r_g3 = [Res(), Res()]

                def ensure_slot(kc_):
                    if kc_ >= NLC:
                        s = NSLOT + (kc_ - NLC)
                    else:
                        s = kc_ % NSLOT
                    if slot_chunk[s] != kc_:
                        slot_chunk[s] = kc_
                        tok = chunk_tok(kc_)
                        tl = tile_of_chunk(kc_)
                        P.add('sp', lambda e: e.dma_start(out=kslot[s][:], in_=qv(kT_na)[:, :, tok:tok + 128]), reads=[DR(id(kT_na), tl)], writes=[r_slot[s]], dma=True)
                        P.add('sp', lambda e: e.dma_start(out=vslot[s][:, :, 0:64], in_=v_na[tok:tok + 128, :].rearrange('p (h d) -> p h d', h=6)), reads=[DR(id(v_na), tl)], writes=[r_slot[s]], dma=True)
                    return s

                def do_chunk(c, idx):
                    cb = idx % 2
                    tok = chunk_tok(c)
                    tl = tile_of_chunk(c)
                    is_ctx = c >= NLC
                    for (dst_, ten, fm) in ((qna[cb], qT_na, True), (uu[cb], uT_sg, True), (rqb[cb], rqT, True), (rkc[cb], rkT, True)):
                        P.add('sp', lambda e, dst_=dst_, ten=ten: e.dma_start(out=dst_[:], in_=qv(ten)[:, :, tok:tok + 128]), reads=[DR(id(ten), tl)], writes=[r_ld[cb]], dma=True)
                    for (dst_, ten) in ((vnb[cb], vn_sg), (rvc[cb], rv_d), (gtb[cb], gates_d)):
                        P.add('sp', lambda e, dst_=dst_, ten=ten: e.dma_start(out=dst_[:], in_=ten[tok:tok + 128, :]), reads=[DR(id(ten), tl)], writes=[r_ld[cb]], dma=True)
                    if is_ctx:
                        kchunks = []
                    else:
                        kchunks = [base_of[c] + m for m in range(5)]
                    kchunks = kchunks + [NLC, NLC + 1]
                    slots = [ensure_slot(kc_) for kc_ in kchunks]
                    nb = len(slots)
                    var = None if is_ctx else var_of[c]
                    def sec_na():
                        pSb = [pS, pS2]
                        r_pSb = [r_pS, r_pS2]

                        def scores(h):
                            buf, pc, r0 = h % 2, h // 2, (h % 2) * 64
                            for bi, s in enumerate(slots):
                                P.add('pe', lambda e, bi=bi, s=s: e.matmul(pSb[buf][:, bi * 128:(bi + 1) * 128], lhsT=kslot[s][r0:r0 + 64, pc, :], rhs=qna[cb][r0:r0 + 64, pc, :], start=True, stop=True),
                                      reads=[r_slot[s], r_ld[cb]], writes=[r_pSb[buf]])

                        def soft(h):
                            buf = eb = h % 2
                            P.add('act', lambda e: e.activation(out=expS[eb][:, 0:nb, :], in_=pSb[buf][:, 0:nb * 128].rearrange('p (b q) -> p b q', b=nb), func=AF.Exp),
                                  reads=[r_pSb[buf]], writes=[r_expS[eb]])
                            if not is_ctx:
                                P.add('dve' if h % 2 == 0 else 'pool', lambda e: e.tensor_tensor(out=expS[eb][:, 0:5, :], in0=expS[eb][:, 0:5, :], in1=E[:, var, h, :, :], op=ALU.mult),
                                      reads=[r_expS[eb], r_E], writes=[r_expS[eb]])

                        def pv(h):
                            eb = h % 2
                            for bi, s in enumerate(slots):
                                P.add('pe', lambda e, bi=bi, s=s: e.matmul(pO[:, h * 65:(h + 1) * 65], lhsT=expS[eb][:, bi, :], rhs=vslot[s][:, h, :], start=(bi == 0), stop=(bi == nb - 1)),
                                      reads=[r_expS[eb], r_slot[s]], writes=[r_pO])

                        scores(0)
                        for h in range(6):
                            if h + 1 < 6:
                                scores(h + 1)
                            soft(h)
                            pv(h)
                        po3 = pO[:, 0:390].rearrange('p (h d) -> p h d', h=6)
                        P.add('dve', lambda e: e.reciprocal(out=rec[cb][:], in_=po3[:, :, 64]), reads=[r_pO], writes=[r_rec[cb]])
                        P.add('dve', lambda e: e.tensor_tensor(out=natok[cb][:].rearrange('p (h d) -> p h d', h=6), in0=po3[:, :, 0:64], in1=rec[cb][:].unsqueeze(2).broadcast_to([128, 6, 64]), op=ALU.mult),
                              reads=[r_pO, r_rec[cb]], writes=[r_natok[cb]])

                    def sec_sg():
                        for gp in range(2):
                            for half in range(2):
                                g = 2 * gp + half
                                P.add('pe', lambda e, gp=gp, half=half, g=g: e.matmul(pRo[half * 64:(half + 1) * 64, 768 + gp * 128:768 + (gp + 1) * 128], lhsT=vnb[cb][:, g * 64:(g + 1) * 64], rhs=sgw[:, g, :], start=True, stop=True),
                                      reads=[r_ld[cb], r_sgt], writes=[r_pG])
                        P.add('dve', lambda e: e.tensor_tensor(out=sgt[cb][:], in0=pRo[:, 768:1024].rearrange('p (g q) -> p g q', g=2), in1=sgbt[:], op=ALU.add), reads=[r_pG, r_sgt], writes=[r_sgtmp[cb]])
                        P.add('dve', lambda e: e.tensor_tensor(out=catst[cb][:, 3:5, :], in0=sgt[cb][:], in1=uu[cb][:], op=ALU.mult), reads=[r_sgtmp[cb], r_ld[cb]], writes=[r_catst[cb]])

                    def sec_ret():
                        for dr in range(2):
                            P.add('dve', lambda e, dr=dr: e.tensor_tensor(out=qfs[cb][:, dr, :, :].rearrange('p (j two) q -> p j two q', two=2),
                                                                          in0=rqb[cb][:].unsqueeze(2).broadcast_to([128, 3, 2, 128]),
                                                                          in1=fsm[:, dr, :, :].rearrange('p (j two) q -> p j two q', two=2), op=ALU.mult),
                                  reads=[r_ld[cb], r_tab], writes=[r_qfs[cb]])
                        for h in range(6):
                            pc, half = h // 2, h % 2
                            r0 = half * 64
                            P.add('pe', lambda e, h=h, pc=pc, r0=r0, half=half: e.matmul(pS[:, half * 512 + pc * 128:half * 512 + (pc + 1) * 128], lhsT=rkc[cb][r0:r0 + 64, pc, :], rhs=rqb[cb][r0:r0 + 64, pc, :], start=True, stop=True),
                                  reads=[r_ld[cb]], writes=[r_pS])
                        for dr in range(2):
                            for par in range(2):
                                P.add('dve', lambda e, dr=dr, par=par: e.tensor_tensor(out=SD[cb][:, dr, :, :].rearrange('p (j two) q -> p j two q', two=2)[:, :, par, :],
                                                                                      in0=pS[:, par * 512:par * 512 + 384].rearrange('p (j q) -> p j q', j=3),
                                                                                      in1=Dm[:, dr, :, :].rearrange('p (j two) q -> p j two q', two=2)[:, :, par, :], op=ALU.mult),
                                      reads=[r_pS, r_tab], writes=[r_SD[cb]])
                        for dr in range(2):
                            for h in range(6):
                                pc, half = h // 2, h % 2
                                r0 = half * 64
                                ob = (dr * 6 + h) * 64
                                P.add('pe', lambda e, dr=dr, h=h, ob=ob: e.matmul(pRo[:, ob:ob + 64], lhsT=SD[cb][:, dr, h, :], rhs=rvc[cb][:, h * 64:(h + 1) * 64], start=True, stop=False),
                                      reads=[r_SD[cb], r_ld[cb]], writes=[r_pRo])
                                P.add('pe', lambda e, dr=dr, h=h, ob=ob, pc=pc, r0=r0: e.matmul(pRo[:, ob:ob + 64], lhsT=qfs[cb][:, dr, h, :], rhs=Sst[:, c, dr, pc, :], start=False, stop=True),
                                      reads=[r_qfs[cb], r_Sst[c][dr]], writes=[r_pRo])

                    def sec_gn():
                        o3 = lambda ap: ap.rearrange('p (g e) -> p g e', g=12)
                        b12 = lambda ap: ap.unsqueeze(2).broadcast_to([128, 12, 64])
                        P.add('dve', lambda e: e.tensor_copy(out=osb[cb][:], in_=pRo[:, 0:768]), reads=[r_pRo], writes=[r_osb[cb]])
                        P.add('dve', lambda e: e.tensor_reduce(out=gsm[cb][:, 0:12], in_=o3(osb[cb][:]), axis=AX.X, op=ALU.add), reads=[r_osb[cb]], writes=[r_gsm[cb]])
                        P.add('act', lambda e: e.activation(out=osq[cb][:], in_=osb[cb][:], func=AF.Square), reads=[r_osb[cb]], writes=[r_osq[cb]])
                        P.add('dve', lambda e: e.tensor_reduce(out=gsm[cb][:, 12:24], in_=o3(osq[cb][:]), axis=AX.X, op=ALU.add), reads=[r_osq[cb]], writes=[r_gsm[cb]])
                        P.add('dve', lambda e: e.tensor_scalar(out=gsm[cb][:, 24:36], in0=gsm[cb][:, 0:12], scalar1=1.0 / 64, scalar2=None, op0=ALU.mult), reads=[r_gsm[cb]], writes=[r_gsm[cb]])
                        P.add('dve', lambda e: e.tensor_tensor(out=gsm[cb][:, 36:48], in0=gsm[cb][:, 24:36], in1=gsm[cb][:, 24:36], op=ALU.mult), reads=[r_gsm[cb]], writes=[r_gsm[cb]])
                        P.add('dve', lambda e: e.scalar_tensor_tensor(out=gsm[cb][:, 48:60], in0=gsm[cb][:, 12:24], scalar=1.0 / 64, in1=gsm[cb][:, 36:48], op0=ALU.mult, op1=ALU.subtract), reads=[r_gsm[cb]], writes=[r_gsm[cb]])
                        P.add('act', lambda e: e.activation(out=gsm[cb][:, 48:60], in_=gsm[cb][:, 48:60], func=AF.Sqrt, bias=lnbe[:, 0:1]), reads=[r_gsm[cb]], writes=[r_gsm[cb]])
                        P.add('dve', lambda e: e.reciprocal(out=gsm[cb][:, 60:72], in_=gsm[cb][:, 48:60]), reads=[r_gsm[cb]], writes=[r_gsm[cb]])
                        P.add('pool', lambda e: e.tensor_tensor(out=o3(g2[cb][:]), in0=o3(osb[cb][:]), in1=b12(gsm[cb][:, 24:36]), op=ALU.subtract), reads=[r_osb[cb], r_gsm[cb]], writes=[r_g2[cb]])
                        P.add('pool', lambda e: e.tensor_tensor(out=o3(g2[cb][:]), in0=o3(g2[cb][:]), in1=b12(gsm[cb][:, 60:72]), op=ALU.mult), reads=[r_g2[cb], r_gsm[cb]], writes=[r_g2[cb]])
                        P.add('dve', lambda e: e.tensor_tensor(out=g2[cb][:], in0=g2[cb][:], in1=gtb[cb][:], op=ALU.mult), reads=[r_g2[cb], r_ld[cb]], writes=[r_g2[cb]])
                        P.add('pool', lambda e: e.tensor_tensor(out=g3t[cb][:], in0=g2[cb][:, 0:384], in1=g2[cb][:, 384:768], op=ALU.add), reads=[r_g2[cb]], writes=[r_g3[cb]])
                        P.add('dve', lambda e: e.tensor_tensor(out=rettok[cb][:], in0=g3t[cb][:], in1=gnw[:], op=ALU.mult), reads=[r_g3[cb], r_sgt], writes=[r_rettok[cb]])

                    def sec_tr():
                        for pc in range(3):
                            P.add('pe', lambda e, pc=pc: e.transpose(pT[:, pc * 128:(pc + 1) * 128], natok[cb][:, pc * 128:(pc + 1) * 128], ident_b), reads=[r_natok[cb], r_cstbf], writes=[r_pT])
                        for pc in range(3):
                            P.add('pe', lambda e, pc=pc: e.transpose(pT[:, (3 + pc) * 128:(4 + pc) * 128], rettok[cb][:, pc * 128:(pc + 1) * 128], ident_b), reads=[r_rettok[cb], r_cstbf], writes=[r_pT])
                        P.add('act', lambda e: e.copy(out=catst[cb][:, 0:3, :], in_=pT[:, 0:384].rearrange('p (c t) -> p c t', c=3)), reads=[r_pT], writes=[r_catst[cb]])
                        P.add('act', lambda e: e.copy(out=catst[cb][:, 5:8, :], in_=pT[:, 384:768].rearrange('p (c t) -> p c t', c=3)), reads=[r_pT], writes=[r_catst[cb]])
                        P.add('pool', lambda e: e.dma_start(out=catT.rearrange('(kc p) t -> p kc t', p=128)[:, :, tok:tok + 128], in_=catst[cb][:]), reads=[r_catst[cb]], writes=[DR(id(catT), tl)], dma=True)

                    return sec_na, sec_sg, sec_ret, sec_gn, sec_tr

                chunks = list(range(NLC)) + ([] if last else [NLC, NLC + 1])
                pend_gn = pend_tr = None
                for idx, c in enumerate(chunks):
                    na_, sg_, ret_, gn_, tr_ = do_chunk(c, idx)
                    na_()
                    if pend_gn is not None:
                        pend_gn()
                    sg_()
                    ret_()
                    if pend_tr is not None:
                        pend_tr()
                    pend_gn, pend_tr = gn_, tr_
                pend_gn()
                pend_tr()
                P.emit_pass()

        for l_ in range(n_layers):
            conv_jobs.append([('w1_%d_0' % l_, w1_in[l_, 0]), ('w2_%d_0' % l_, w2_in[l_, 0])])
            conv_jobs.append([('win_%d' % l_, win_in[l_])])
            conv_jobs.append([('wo_%d' % l_, wout_in[l_])])
            conv_jobs.append([('w1_%d_1' % l_, w1_in[l_, 1]), ('w2_%d_1' % l_, w2_in[l_, 1])])
        pass_xpose_in(xTa)
        cur, oth = xTa, xTb
        for l in range(n_layers):
            last = (l == n_layers - 1)
            pass_mod(l)
            pass_ffn(l, 0, cur, oth)
            cur, oth = oth, cur
            if stop_after == 'ffn1':
                break
            pass_inproj(l, cur)
            if stop_after == 'inproj':
                break
            pass_mixer(l, last)
            if stop_after == 'mixer':
                break
            pass_outproj(l, cur, oth, skip_ctx=last)
            cur, oth = oth, cur
            if stop_after == 'outproj':
                break
            pass_ffn(l, 1, cur, oth, skip_ctx=last)
            cur, oth = oth, cur
        pass_xpose_out(cur)
    return nc


def prep_inputs(NT, b, inp, shared):
    m = dict(shared)
    m['x'] = np.ascontiguousarray(inp['x'][b])
    m['ctx'] = np.ascontiguousarray(inp['ctx'][b])
    cc = np.stack([inp['c'][b], inp['c_ctx']], axis=0)
    m['cT'] = np.ascontiguousarray(cc.reshape(2, KC, 128).transpose(2, 1, 0).reshape(128, 16))
    return m


def prep_shared(NT, inp):
    f = lambda a: np.ascontiguousarray(np.asarray(a, dtype=np.float32))
    NLC = NT // 128
    var_of, base_of, ridx, cidx, valid = na_tables(NLC)
    sh = {}
    sh['ada_w'] = f(inp['ada_w'])
    sh['ada_bT'] = f(inp['ada_b'].reshape(2, 72, 128).transpose(0, 2, 1))
    sh['norm_wT'] = f(inp['norm_w'].reshape(2, 3, KC, 128).transpose(3, 0, 1, 2).reshape(128, 48))
    sh['ffn_w1'] = f(inp['ffn_w1'])
    sh['ffn_w2'] = f(inp['ffn_w2'])
    w_in = np.asarray(inp['mix_w_in'], dtype=np.float32)
    perm = np.arange(384).reshape(6, 64)
    part = perm.copy()
    for h in range(6):
        for d in range(64):
            part[h, d] = h * 64 + (d + 16 if (d % 32) < 16 else d - 16)
    part = part.reshape(-1)
    sh['w_in_ext'] = f(np.concatenate([w_in, w_in[:, :, 1664 + part], w_in[:, :, 2048 + part]], axis=2))
    sh['w_out'] = f(inp['mix_w_out'])
    qk = np.zeros((128, 4), np.float32)
    for l in range(2):
        qk[:, 2 * l] = np.tile(inp['na_q_norm'][l], 2)
        qk[:, 2 * l + 1] = np.tile(inp['na_k_norm'][l], 2)
    sh['qknT'] = qk
    rpb = np.asarray(inp['na_rpb'], dtype=np.float32)
    g = rpb[:, :, ridx, cidx]
    g = np.where(valid[None, None], g, np.float32(-1e30)).astype(np.float32)
    sh['na_biasT'] = f(g.transpose(0, 2, 1, 4, 3, 5))
    sh['sg_wT'] = f(np.asarray(inp['sg_w']).transpose(0, 1, 3, 2))
    sgb = np.asarray(inp['sg_b'], dtype=np.float32)
    sh['sgb_rep'] = f(np.repeat(sgb.reshape(2, 2, 2, 1, 128), 64, axis=3).reshape(2, 2, 128, 128))
    sh['sg_lnw_rep'] = f(np.broadcast_to(np.asarray(inp['sg_ln_w']).reshape(1, 512), (128, 512)))
    sh['sg_lnb_rep'] = f(np.broadcast_to(np.asarray(inp['sg_ln_b']).reshape(1, 512), (128, 512)))
    sh['dlog_rep'] = f(np.broadcast_to(np.asarray(inp['ret_decay_logit']).reshape(1, 24), (128, 24)))
    sh['gnw_rep'] = f(np.broadcast_to(np.asarray(inp['ret_gn_w']).reshape(1, 768), (128, 768)))
    C, S = rope_tables(NT)
    sh['ropeC'] = C
    sh['ropeS'] = S
    sh['consts'] = const_table()
    return sh


_NC_CACHE = {}


def kernel(**inputs):
    inp = {k: np.asarray(v) for k, v in inputs.items()}
    B, NT, _ = inp['x'].shape
    if NT not in _NC_CACHE:
        _NC_CACHE[NT] = build(NT)
    nc = _NC_CACHE[NT]
    shared = prep_shared(NT, inp)
    in_maps = [prep_inputs(NT, b, inp, shared) for b in range(B)]
    res = run_bass_kernel_spmd(nc, in_maps, core_ids=list(range(B)))
    return np.stack([np.asarray(r['y']) for r in res.results], axis=0).astype(np.float32)
```

```python
import math
from contextlib import ExitStack

import numpy as np
import concourse.bass as bass
import concourse.mybir as mybir
from concourse.bass_utils import run_bass_kernel_spmd

F32 = mybir.dt.float32
BF16 = mybir.dt.bfloat16
AF = mybir.ActivationFunctionType
ALU = mybir.AluOpType
AX = mybir.AxisListType

ENGS = ['pe', 'act', 'dve', 'pool', 'sp']

D = 1024
KC = 8
DFF = 2816
FC = 22
NCTX = 256
GW = 64
NMOD = 9
INX = 4352
RMS_EPS = 1e-6
LN_EPS = 1e-5
LN8 = math.log(0.125)
import os
MIXDBG = os.environ.get('MIXDBG', 'scan,na,sg,ret,tr').split(',')


class Res:
    __slots__ = ('name', 'last_w', 'readers')

    def __init__(self, name=''):
        self.name = name
        self.last_w = None
        self.readers = []


class Op:
    __slots__ = ('eng', 'fn', 'deps', 'signal', 'seq', 'is_dma', 'dsem', 'dval', 'dprev')


class Prog:
    def __init__(self, nc, es, n_dma_sems=24):
        self.nc = nc
        self.n_dma_sems = n_dma_sems
        self.csem = {e: es.enter_context(nc.semaphore('c_' + e)) for e in ENGS}
        self.nsem = {'sp': 16, 'pool': 6}
        self.dsem = {e: [es.enter_context(nc.semaphore('d_%s_%d' % (e, i))) for i in range(self.nsem[e])]
                     for e in ('sp', 'pool')}
        self.dma_cnt = {e: 0 for e in ENGS}
        self.dma_use = {e: [0] * n_dma_sems for e in ENGS}
        self.seqc = {e: 0 for e in ENGS}
        self.ops = {e: [] for e in ENGS}
        self.waited = {e: {} for e in ENGS}

    def add(self, eng, fn, reads=(), writes=(), dma=False):
        op = Op()
        op.eng = eng
        op.fn = fn
        op.signal = False
        op.seq = 0
        op.is_dma = dma
        op.dsem = None
        op.dval = 0
        op.dprev = 0
        deps = []
        seen = set()

        def push(d):
            if d is None or id(d) in seen:
                return
            seen.add(id(d))
            if d.eng == 'pe' and eng == 'pe' and not d.is_dma and not dma:
                return
            deps.append(d)

        for r in reads:
            push(r.last_w)
        for w in writes:
            push(w.last_w)
            for rd in w.readers:
                push(rd)
        op.deps = deps
        for d in deps:
            d.signal = True
        for r in reads:
            r.readers.append(op)
        for w in writes:
            w.last_w = op
            w.readers = []
        if dma:
            assert eng in ('sp', 'pool')
            k = self.dma_cnt[eng] % self.nsem[eng]
            self.dma_cnt[eng] += 1
            op.dsem = k
            op.dprev = 16 * self.dma_use[eng][k]
            self.dma_use[eng][k] += 1
            op.dval = 16 * self.dma_use[eng][k]
        self.ops[eng].append(op)
        return op

    def emit_pass(self):
        nc = self.nc
        for e in ENGS:
            last = None
            for op in self.ops[e]:
                if not op.is_dma:
                    last = op
            if last is not None:
                last.signal = True
            for op in self.ops[e]:
                if op.signal and not op.is_dma:
                    self.seqc[e] += 1
                    op.seq = self.seqc[e]
        final_c = dict(self.seqc)
        final_d = {e: [16 * u for u in self.dma_use[e]] for e in ('sp', 'pool')}
        with nc.Block() as block:
            regs = {'pe': block.tensor, 'act': block.scalar, 'dve': block.vector,
                    'pool': block.gpsimd, 'sp': block.sync}
            for e in ENGS:
                ops = self.ops[e]

                def body(eng, e=e, ops=ops):
                    waited = self.waited[e]
                    for op in ops:
                        for d in op.deps:
                            if d.is_dma:
                                key = ('d', d.eng, d.dsem)
                                sem = self.dsem[d.eng][d.dsem]
                                val = d.dval
                            else:
                                key = ('c', d.eng)
                                sem = self.csem[d.eng]
                                val = d.seq
                            if waited.get(key, 0) >= val:
                                continue
                            waited[key] = val
                            eng.wait_ge(sem, val)
                        if op.is_dma:
                            key = ('d', e, op.dsem)
                            if op.dprev > 0 and waited.get(key, 0) < op.dprev:
                                waited[key] = op.dprev
                                eng.wait_ge(self.dsem[e][op.dsem], op.dprev)
                            ins = op.fn(eng)
                            ins.then_inc(self.dsem[e][op.dsem], 16)
                        else:
                            ins = op.fn(eng)
                            if op.signal:
                                ins.then_inc(self.csem[e], 1)
                    for e2 in ENGS:
                        if e2 != e and final_c[e2] > waited.get(('c', e2), 0):
                            waited[('c', e2)] = final_c[e2]
                            eng.wait_ge(self.csem[e2], final_c[e2])
                    for q in ('sp', 'pool'):
                        for k in range(self.nsem[q]):
                            if final_d[q][k] > waited.get(('d', q, k), 0):
                                waited[('d', q, k)] = final_d[q][k]
                                eng.wait_ge(self.dsem[q][k], final_d[q][k])

                regs[e](body)
        self.ops = {e: [] for e in ENGS}


def na_tables(nchunks):
    rows = 2 * nchunks
    kr = min(8, rows)
    sigs = {}
    var_of = []
    base_of = []
    tabs = []
    ir = np.arange(128) // 64
    cc = np.arange(128) % 64
    for j in range(nchunks):
        base = int(np.clip(j - 2, 0, nchunks - 5))
        qrow = 2 * j + ir[None, :]
        qcol = cc[None, :]
        rstart = np.clip(qrow - kr // 2, 0, rows - kr)
        cstart = np.clip(qcol - 8, 0, GW - 16)
        rid = np.zeros((5, 128, 128), np.int64)
        cid = np.zeros((5, 128, 128), np.int64)
        val = np.zeros((5, 128, 128), bool)
        for m in range(5):
            krow = 2 * (base + m) + ir[:, None]
            kcol = cc[:, None]
            v = (krow >= rstart) & (krow < rstart + kr) & (kcol >= cstart) & (kcol < cstart + 16)
            rid[m] = np.clip(krow - qrow + 7, 0, 14)
            cid[m] = np.clip(kcol - qcol + 15, 0, 30)
            val[m] = v
        sig = (base - j, val.tobytes())
        if sig not in sigs:
            sigs[sig] = len(tabs)
            tabs.append((rid, cid, val))
        var_of.append(sigs[sig])
        base_of.append(base)
    ridx = np.stack([t[0] for t in tabs])
    cidx = np.stack([t[1] for t in tabs])
    valid = np.stack([t[2] for t in tabs])
    return var_of, base_of, ridx, cidx, valid


def rope_tables(NT):
    NTOK = NT + NCTX
    t = np.arange(NT)
    inv = (10000.0 ** (-np.arange(16, dtype=np.float32) / 16)).astype(np.float32)
    ang_r = (t // GW).astype(np.float32)[:, None] * inv[None, :]
    ang_c = (t % GW).astype(np.float32)[:, None] * inv[None, :]
    C = np.ones((128, NTOK), np.float32)
    S = np.zeros((128, NTOK), np.float32)
    for p in range(128):
        d = p % 64
        ang = ang_r if d < 32 else ang_c
        f = d % 16
        first = (d % 32) < 16
        C[p, :NT] = np.cos(ang[:, f])
        S[p, :NT] = (-np.sin(ang[:, f])) if first else np.sin(ang[:, f])
    return C, S


def const_table():
    i = np.arange(128, dtype=np.float32)
    diffT = i[None, :] - i[:, None]
    mF = (diffT >= 0).astype(np.float32)
    mB = (diffT <= 0).astype(np.float32)
    ip1 = np.broadcast_to(i[None, :] + 1, (128, 128))
    rev = np.broadcast_to(128 - i[None, :], (128, 128))
    ident = np.eye(128, dtype=np.float32)
    bd = np.zeros((128, 128), np.float32)
    bd[:64, :64] = 1.0 / 64
    bd[64:, 64:] = 1.0 / 64
    pcol = i[:, None]
    prev = 127 - i[:, None]
    return np.ascontiguousarray(np.concatenate([diffT, mF, mB, ip1, rev, ident, bd, pcol, prev], axis=1).astype(np.float32))


C_DIFF, C_MF, C_MB, C_IP1, C_REV, C_ID, C_BD, C_PCOL, C_PREV = [k * 128 for k in range(7)] + [896, 897]
NCONST = 898


def build(NT, n_layers=2, stop_after=None, dbg=False):
    nc = bass.Bass('TRN2', target_bir_lowering=False)
    _uid = [0]

    def _sbuf_tensor(name, shape, dt):
        _uid[0] += 1
        return _orig_sb('%s_u%d' % (name, _uid[0]), shape, dt)

    def _psum_tensor(name, shape, dt):
        _uid[0] += 1
        return _orig_ps('%s_u%d' % (name, _uid[0]), shape, dt)

    _orig_sb = nc.sbuf_tensor
    _orig_ps = nc.psum_tensor
    NTOK = NT + NCTX
    NLC = NT // 128
    NCH = NLC + 2
    tiles = [(i * 512, 512, 0) for i in range(NT // 512)] + [(NT, 256, 1)]
    var_of, base_of, ridx, cidx, valid = na_tables(NLC)
    NV = ridx.shape[0]

    def din(name, shape, dt=F32):
        return nc.dram_tensor(name, list(shape), dt, kind='ExternalInput').ap()

    x_in = din('x', [NT, D])
    ctx_in = din('ctx', [NCTX, D])
    cT_in = din('cT', [128, 16])
    ada_w = din('ada_w', [2, D, NMOD * D])
    ada_bT = din('ada_bT', [2, 128, 72])
    norm_wT = din('norm_wT', [128, 48])
    w1_in = din('ffn_w1', [2, 2, D, 2 * DFF])
    w2_in = din('ffn_w2', [2, 2, DFF, D])
    win_in = din('w_in_ext', [2, D, INX])
    wout_in = din('w_out', [2, D, D])
    qkn_in = din('qknT', [128, 4])
    bias_in = din('na_biasT', [2, NV, 6, 128, 5, 128])
    sgw_in = din('sg_wT', [2, 4, 128, 128])
    sgb_in = din('sgb_rep', [2, 2, 128, 128])
    lnw_in = din('sg_lnw_rep', [128, 512])
    lnb_in = din('sg_lnb_rep', [128, 512])
    dlog_in = din('dlog_rep', [128, 24])
    gnw_in = din('gnw_rep', [128, 768])
    ropeC = din('ropeC', [128, NTOK])
    ropeS = din('ropeS', [128, NTOK])
    const_in = din('consts', [128, NCONST])
    y_out = nc.dram_tensor('y', [NT, D], F32, kind='ExternalOutput').ap()

    dbg_kind = 'ExternalOutput' if dbg else 'Internal'

    def dscr(name, shape, dt):
        return nc.dram_tensor(name, list(shape), dt, kind=dbg_kind).ap()

    xTa = dscr('xTa', [D, NTOK], F32)
    xTb = dscr('xTb', [D, NTOK], F32)
    qT_na = dscr('qT_na', [384, NTOK], BF16)
    kT_na = dscr('kT_na', [384, NTOK], BF16)
    v_na = dscr('v_na', [NTOK, 384], BF16)
    uT_sg = dscr('uT_sg', [256, NTOK], BF16)
    vn_sg = dscr('vn_sg', [NTOK, 256], BF16)
    rqT = dscr('rqT', [384, NTOK], BF16)
    rkT = dscr('rkT', [384, NTOK], BF16)
    rv_d = dscr('rv', [NTOK, 384], BF16)
    gates_d = dscr('gates', [NTOK, 768], BF16)
    catT = dscr('catT', [D, NTOK], BF16)

    dres = {}

    def DR(name, tile):
        k = (name, tile)
        if k not in dres:
            dres[k] = Res('%s_%s' % (name, tile))
        return dres[k]

    def tile_of_chunk(c):
        return c // 4 if c < NLC else NT // 512

    def chunk_tok(c):
        return c * 128

    with ExitStack() as ges:
        P = Prog(nc, ges)
        gsb = lambda name, shape, dt=F32: ges.enter_context(_sbuf_tensor(name, list(shape), dt))
        cst = gsb('cst', [128, NCONST])
        cst_bf = gsb('cst_bf', [128, 3 * 128], BF16)
        modT = gsb('modT', [128, 72, 2])
        mtab = gsb('mtab', [128, 9, 8, 2])
        r_cst = Res('cst')
        r_cstbf = Res('cst_bf')
        r_mtab = Res('mtab')
        lnb8 = gsb('lnb8', [128, 1])
        lnbe = gsb('lnbe', [128, 1])
        ident_f = cst[:, C_ID:C_ID + 128]
        ident_b = cst_bf[:, 0:128]
        bd_b = cst_bf[:, 128:256]
        ones_b = cst_bf[:, 256:384]

        P.add('sp', lambda e: e.dma_start(out=cst[:], in_=const_in), writes=[r_cst], dma=True)
        P.add('dve', lambda e: e.tensor_copy(out=cst_bf[:, 0:256], in_=cst[:, C_ID:C_ID + 256]), reads=[r_cst], writes=[r_cstbf])
        P.add('dve', lambda e: e.memset(cst_bf[:, 256:384], 1.0 / 1024), reads=[], writes=[r_cstbf])
        P.add('dve', lambda e: e.memset(lnb8[:], LN8), reads=[], writes=[r_cstbf])
        P.add('dve', lambda e: e.memset(lnbe[:], LN_EPS), reads=[], writes=[r_cstbf])
        P.emit_pass()

        def pass_xpose_in(dst):
            with ExitStack() as es:
                conv_issue()
                stg_in = [es.enter_context(_sbuf_tensor('xi%d' % i, [128, D], F32)) for i in range(3)]
                r_in = [Res() for _ in range(3)]
                stg = [es.enter_context(_sbuf_tensor('xo%d' % i, [128, KC, 512], F32)) for i in range(2)]
                r_stg = [Res() for _ in range(2)]
                pss = [es.enter_context(_psum_tensor('pxp%d' % i, [128, 512], F32)) for i in range(4)]
                r_ps = [Res() for _ in range(4)]
                ci = 0
                pi = 0
                for ti, (t0, T, j) in enumerate(tiles):
                    so = stg[ti % 2]
                    rso = r_stg[ti % 2]
                    for s in range(T // 128):
                        tok = t0 + s * 128
                        src = x_in[tok:tok + 128, :] if j == 0 else ctx_in[tok - NT:tok - NT + 128, :]
                        b = ci % 3
                        ci += 1
                        P.add('sp', lambda e, b=b, src=src: e.dma_start(out=stg_in[b][:], in_=src), writes=[r_in[b]], dma=True)
                        for half in range(2):
                            pb = pi % 4
                            pi += 1
                            for q in range(4):
                                kc = half * 4 + q
                                P.add('pe', lambda e, pb=pb, q=q, b=b, kc=kc: e.transpose(pss[pb][:, q * 128:(q + 1) * 128], stg_in[b][:, kc * 128:(kc + 1) * 128], ident_f),
                                      reads=[r_in[b], r_cst], writes=[r_ps[pb]])
                            eng = 'act' if half == 0 else 'dve'
                            dstv = so[:, half * 4:half * 4 + 4, s * 128:(s + 1) * 128]
                            srcv = pss[pb][:].rearrange('p (q t) -> p q t', q=4)
                            if eng == 'act':
                                P.add('act', lambda e, dstv=dstv, srcv=srcv: e.copy(out=dstv, in_=srcv), reads=[r_ps[pb]], writes=[rso])
                            else:
                                P.add('dve', lambda e, dstv=dstv, srcv=srcv: e.tensor_copy(out=dstv, in_=srcv), reads=[r_ps[pb]], writes=[rso])
                    dv = dst.rearrange('(kc p) t -> p kc t', p=128)[:, :, t0:t0 + T]
                    P.add('pool', lambda e, so=so, dv=dv, T=T: e.dma_start(out=dv, in_=so[:, :, 0:T]), reads=[rso], writes=[DR(id(dst), ti)], dma=True)
                P.emit_pass()

        def pass_xpose_out(src):
            with ExitStack() as es:
                stg_in = [es.enter_context(_sbuf_tensor('yi%d' % i, [128, KC, 512], F32)) for i in range(2)]
                r_in = [Res() for _ in range(2)]
                stg = [es.enter_context(_sbuf_tensor('yo%d' % i, [128, D], F32)) for i in range(3)]
                r_stg = [Res() for _ in range(3)]
                pss = [es.enter_context(_psum_tensor('pyp%d' % i, [128, 512], F32)) for i in range(4)]
                r_ps = [Res() for _ in range(4)]
                ci = 0
                pi = 0
                for ti, (t0, T, j) in enumerate(tiles):
                    if j == 1:
                        continue
                    si = stg_in[ti % 2]
                    rsi = r_in[ti % 2]
                    sv = src.rearrange('(kc p) t -> p kc t', p=128)[:, :, t0:t0 + T]
                    P.add('sp', lambda e, si=si, sv=sv: e.dma_start(out=si[:], in_=sv), reads=[DR(id(src), ti)], writes=[rsi], dma=True)
                    for s in range(T // 128):
                        b = ci % 3
                        ci += 1
                        for half in range(2):
                            pb = pi % 4
                            pi += 1
                            for q in range(4):
                                kc = half * 4 + q
                                P.add('pe', lambda e, pb=pb, q=q, si=si, kc=kc, s=s: e.transpose(pss[pb][:, q * 128:(q + 1) * 128], si[:, kc, s * 128:(s + 1) * 128], ident_f),
                                      reads=[rsi, r_cst], writes=[r_ps[pb]])
                            dstv = stg[b][:, half * 512:(half + 1) * 512]
                            if half == 0:
                                P.add('act', lambda e, dstv=dstv, pb=pb: e.copy(out=dstv, in_=pss[pb][:]), reads=[r_ps[pb]], writes=[r_stg[b]])
                            else:
                                P.add('dve', lambda e, dstv=dstv, pb=pb: e.tensor_copy(out=dstv, in_=pss[pb][:]), reads=[r_ps[pb]], writes=[r_stg[b]])
                        tok = t0 + s * 128
                        P.add('pool', lambda e, b=b, tok=tok: e.dma_start(out=y_out[tok:tok + 128, :], in_=stg[b][:]), reads=[r_stg[b]], writes=[DR('y', tok)], dma=True)
                P.emit_pass()

        def pass_mod(l):
            with ExitStack() as es:
                conv_issue()
                sb = lambda name, shape, dt=F32: es.enter_context(_sbuf_tensor(name, list(shape), dt))
                cT = sb('cT', [128, 16])
                sig = sb('csig', [128, 16])
                scT = sb('scT', [128, 16])
                abT = sb('abT', [128, 72])
                nwT = sb('nwT', [128, 48])
                wbuf = [sb('adaw%d' % i, [128, KC, 512]) for i in range(2)]
                r_w = [Res() for _ in range(2)]
                pm = es.enter_context(_psum_tensor('pmod', [128, 72, 2], F32))
                r_pm = Res()
                r_c = Res(); r_sc = Res(); r_ab = Res(); r_nw = Res(); r_mod = Res(); r_sig = Res()
                P.add('sp', lambda e: e.dma_start(out=cT[:], in_=cT_in), writes=[r_c], dma=True)
                P.add('sp', lambda e: e.dma_start(out=abT[:], in_=ada_bT[l]), writes=[r_ab], dma=True)
                P.add('sp', lambda e: e.dma_start(out=nwT[:], in_=norm_wT), writes=[r_nw], dma=True)
                P.add('act', lambda e: e.activation(out=sig[:], in_=cT[:], func=AF.Sigmoid), reads=[r_c], writes=[r_sig])
                P.add('dve', lambda e: e.tensor_tensor(out=scT[:], in0=cT[:], in1=sig[:], op=ALU.mult), reads=[r_c, r_sig], writes=[r_sc])
                wv = ada_w[l].rearrange('(kc p) n -> p kc n', p=128)
                for g in range(18):
                    b = g % 2
                    P.add('sp', lambda e, b=b, g=g: e.dma_start(out=wbuf[b][:], in_=wv[:, :, g * 512:(g + 1) * 512]), writes=[r_w[b]], dma=True)
                    for q in range(4):
                        oc = g * 4 + q
                        for kc in range(KC):
                            P.add('pe', lambda e, b=b, q=q, oc=oc, kc=kc: e.matmul(pm[:, oc, :], lhsT=wbuf[b][:, kc, q * 128:(q + 1) * 128], rhs=scT[:, 2 * kc:2 * kc + 2],
                                                                                  start=(kc == 0), stop=(kc == KC - 1)),
                                  reads=[r_w[b], r_sc], writes=[r_pm])
                P.add('dve', lambda e: e.tensor_tensor(out=modT[:], in0=pm[:], in1=abT[:].unsqueeze(2).broadcast_to([128, 72, 2]), op=ALU.add),
                      reads=[r_pm, r_ab], writes=[r_mod])
                for n in range(3):
                    sc = modT[:, (3 * n + 1) * 8:(3 * n + 2) * 8, :]
                    nw = nwT[:, (l * 3 + n) * 8:(l * 3 + n + 1) * 8].unsqueeze(2).broadcast_to([128, 8, 2])
                    P.add('dve', lambda e, n=n, sc=sc, nw=nw: e.scalar_tensor_tensor(out=mtab[:, n, :, :], in0=sc, scalar=1.0, in1=nw, op0=ALU.add, op1=ALU.mult),
                          reads=[r_mod, r_nw], writes=[r_mtab])
                    sh = modT[:, (3 * n) * 8:(3 * n + 1) * 8, :]
                    P.add('dve', lambda e, n=n, sh=sh: e.tensor_copy(out=mtab[:, 3 + n, :, :], in_=sh), reads=[r_mod], writes=[r_mtab])
                    gt = modT[:, (3 * n + 2) * 8:(3 * n + 3) * 8, :]
                    gm = 1.0 if n == 1 else 0.5
                    P.add('dve', lambda e, n=n, gt=gt, gm=gm: e.tensor_scalar(out=mtab[:, 6 + n, :, :], in0=gt, scalar1=gm, scalar2=None, op0=ALU.mult),
                          reads=[r_mod], writes=[r_mtab])
                P.emit_pass()

        def mt(kind, n, kc, j):
            return mtab[:, kind * 3 + n, kc, j:j + 1]

        wb_cache = {}

        def wb_tensor(key, src):
            if key not in wb_cache:
                rows, cols = src.shape
                wb_cache[key] = (dscr('wb_' + key, [rows, cols], BF16), src, Res('wb_' + key))
            return wb_cache[key]

        conv_jobs = []

        def conv_issue():
            if not conv_jobs:
                return
            for key, src in conv_jobs.pop(0):
                dstt, _, res = wb_tensor(key, src)
                rows, cols = src.shape
                r0 = 0
                while r0 < rows:
                    r1 = min(rows, r0 + 256)
                    c0 = 0
                    while c0 < cols:
                        c1 = min(cols, c0 + 2048)
                        P.add('pool', lambda e, r0=r0, r1=r1, c0=c0, c1=c1, dstt=dstt, src=src: e.dma_start(out=dstt[r0:r1, c0:c1], in_=src[r0:r1, c0:c1]),
                              writes=[res], dma=True)
                        c0 = c1
                    r0 = r1

        def load_w(dst, key, src, rows_chunks, ncols, res_list):
            dstt, _, res = wb_tensor(key, src)
            assert res.last_w is not None, key
            sv = dstt.rearrange('(kc p) n -> p kc n', p=128)
            for kc in range(rows_chunks):
                P.add('sp', lambda e, kc=kc: e.dma_start(out=dst[:, kc, :], in_=sv[:, kc, :]), reads=[res], writes=[res_list[kc]], dma=True)

        class Prep:
            def __init__(self, es, tag):
                sb = lambda name, shape, dt=F32: es.enter_context(_sbuf_tensor(tag + name, list(shape), dt))
                self.hT = [sb('hT%d' % i, [128, KC, 512], BF16) for i in range(2)]
                self.r_hT = [Res() for _ in range(2)]
                self.sq = [sb('sq%d' % i, [128, 512], BF16) for i in range(2)]
                self.r_sq = [Res() for _ in range(2)]
                self.ln = sb('ln', [128, 512])
                self.r_ln = Res()
                self.rstd = sb('rstd', [128, 512])
                self.r_rstd = Res()
                self.tmp = [sb('tmp%d' % i, [128, 512]) for i in range(2)]
                self.r_tmp = [Res() for _ in range(2)]
                self.pst = es.enter_context(_psum_tensor(tag + 'pst', [128, 512], F32))
                self.r_pst = Res()
                self.cnt = 0

            def chunk_a(self, slot, kc, T, xap, xres):
                b = kc % 2
                sq = self.sq[b]
                P.add('act', lambda e: e.activation(out=sq[:, 0:T], in_=xap, func=AF.Square), reads=[xres], writes=[self.r_sq[b]])
                hT = self.hT[slot]
                P.add('pool', lambda e: e.tensor_copy(out=hT[:, kc, 0:T], in_=xap), reads=[xres], writes=[self.r_hT[slot]])

            def chunk_b(self, kc, T):
                b = kc % 2
                sq = self.sq[b]
                P.add('pe', lambda e: e.matmul(self.pst[:, 0:T], lhsT=ones_b, rhs=sq[:, 0:T], start=(kc == 0), stop=(kc == KC - 1)),
                      reads=[self.r_sq[b], r_cstbf], writes=[self.r_pst])

            def chunk_in(self, slot, kc, T, xap, xres):
                self.chunk_a(slot, kc, T, xap, xres)
                self.chunk_b(kc, T)

            def finish_a(self, T):
                P.add('act', lambda e: e.activation(out=self.ln[:, 0:T], in_=self.pst[:, 0:T], func=AF.Ln, bias=RMS_EPS), reads=[self.r_pst], writes=[self.r_ln])
                P.add('act', lambda e: e.activation(out=self.rstd[:, 0:T], in_=self.ln[:, 0:T], func=AF.Exp, scale=-0.5), reads=[self.r_ln], writes=[self.r_rstd])

            def finish_kc(self, slot, kc, T, n, j):
                hT = self.hT[slot]
                b = kc % 2
                tmp = self.tmp[b]
                P.add('dve', lambda e: e.tensor_tensor(out=tmp[:, 0:T], in0=hT[:, kc, 0:T], in1=self.rstd[:, 0:T], op=ALU.mult),
                      reads=[self.r_hT[slot], self.r_rstd], writes=[self.r_tmp[b]])
                P.add('dve', lambda e: e.tensor_scalar(out=hT[:, kc, 0:T], in0=tmp[:, 0:T], scalar1=mt(0, n, kc, j), scalar2=mt(1, n, kc, j), op0=ALU.mult, op1=ALU.add),
                      reads=[self.r_tmp[b], r_mtab], writes=[self.r_hT[slot]])

            def finish(self, slot, T, n, j):
                self.finish_a(T)
                for kc in range(KC):
                    self.finish_kc(slot, kc, T, n, j)

            def steps(self, slot, T, n, j, load_chunk):
                def A(kc):
                    xap, xres = load_chunk(kc)
                    self.chunk_a(slot, kc, T, xap, xres)

                st = [lambda: A(0)]
                for kc in range(1, KC):
                    st.append(lambda kc=kc: (A(kc), self.chunk_b(kc - 1, T)))
                st.append(lambda: self.chunk_b(KC - 1, T))
                st.append(lambda: self.finish_a(T))
                for kc in range(KC):
                    st.append(lambda kc=kc: self.finish_kc(slot, kc, T, n, j))
                return st

        def pass_ffn(l, which, src, dst, skip_ctx=False):
            n = 0 if which == 0 else 2
            with ExitStack() as es:
                sb = lambda name, shape, dt=F32: es.enter_context(_sbuf_tensor(name, list(shape), dt))
                w1 = sb('w1', [128, KC, 2 * DFF], BF16)
                r_w1 = [Res() for _ in range(KC)]
                w2 = sb('w2', [128, FC, D], BF16)
                r_w2 = [Res() for _ in range(FC)]
                act = sb('actb', [128, FC, 512], BF16)
                r_act = [Res() for _ in range(FC)]
                xin = [sb('xin%d' % i, [128, 512]) for i in range(3)]
                r_xin = [Res() for _ in range(3)]
                xrs = [sb('xrs%d' % i, [128, 512]) for i in range(2)]
                r_xrs = [Res() for _ in range(2)]
                xo = [sb('xo%d' % i, [128, 512]) for i in range(2)]
                r_xo = [Res() for _ in range(2)]
                sg = [sb('sg%d' % i, [128, 512], BF16) for i in range(2)]
                r_sg = [Res() for _ in range(2)]
                prep = Prep(es, 'f')
                pg = [es.enter_context(_psum_tensor('pg%d' % i, [128, 512], F32)) for i in range(2)]
                pu = [es.enter_context(_psum_tensor('pu%d' % i, [128, 512], F32)) for i in range(2)]
                po = [es.enter_context(_psum_tensor('po%d' % i, [128, 512], F32)) for i in range(2)]
                r_pg = [Res() for _ in range(2)]
                r_pu = [Res() for _ in range(2)]
                r_po = [Res() for _ in range(2)]
                load_w(w1, 'w1_%d_%d' % (l, which), w1_in[l, which], KC, 2 * DFF, r_w1)
                load_w(w2, 'w2_%d_%d' % (l, which), w2_in[l, which], FC, D, r_w2)
                conv_issue()
                my_tiles = [(ti, t) for ti, t in enumerate(tiles) if not (skip_ctx and t[2] == 1)]
                srcv = src.rearrange('(kc p) t -> p kc t', p=128)
                dstv = dst.rearrange('(kc p) t -> p kc t', p=128)
                cnt = {'xin': 0, 'x2': 0}

                def prepare_steps(k):
                    ti, (t0, T, j) = my_tiles[k]
                    slot = k % 2

                    def load_chunk(kc):
                        b = cnt['xin'] % 3
                        cnt['xin'] += 1
                        P.add('sp', lambda e: e.dma_start(out=xin[b][:, 0:T], in_=srcv[:, kc, t0:t0 + T]), reads=[DR(id(src), ti)], writes=[r_xin[b]], dma=True)
                        return xin[b][:, 0:T], r_xin[b]

                    return prep.steps(slot, T, n, j, load_chunk)

                def prepare(k):
                    for st_ in prepare_steps(k):
                        st_()

                def do_tile(k):
                    ti, (t0, T, j) = my_tiles[k]
                    nsteps = prepare_steps(k + 1) if k + 1 < len(my_tiles) else []
                    slot = k % 2
                    hT = prep.hT[slot]
                    r_hT = prep.r_hT[slot]
                    for mo in range(FC):
                        pb = mo % 2
                        for kc in range(KC):
                            P.add('pe', lambda e, pb=pb, mo=mo, kc=kc: e.matmul(pg[pb][:, 0:T], lhsT=w1[:, kc, mo * 128:(mo + 1) * 128], rhs=hT[:, kc, 0:T],
                                                                               start=(kc == 0), stop=(kc == KC - 1)),
                                  reads=[r_w1[kc], r_hT], writes=[r_pg[pb]])
                        for kc in range(KC):
                            P.add('pe', lambda e, pb=pb, mo=mo, kc=kc: e.matmul(pu[pb][:, 0:T], lhsT=w1[:, kc, DFF + mo * 128:DFF + (mo + 1) * 128], rhs=hT[:, kc, 0:T],
                                                                               start=(kc == 0), stop=(kc == KC - 1)),
                                  reads=[r_w1[kc], r_hT], writes=[r_pu[pb]])
                        P.add('act', lambda e, pb=pb: e.activation(out=sg[pb][:, 0:T], in_=pg[pb][:, 0:T], func=AF.Silu), reads=[r_pg[pb]], writes=[r_sg[pb]])
                        P.add('dve', lambda e, pb=pb, mo=mo: e.tensor_tensor(out=act[:, mo, 0:T], in0=pu[pb][:, 0:T], in1=sg[pb][:, 0:T], op=ALU.mult),
                              reads=[r_pu[pb], r_sg[pb]], writes=[r_act[mo]])
                        if nsteps and mo >= 1:
                            nsteps.pop(0)()
                    while nsteps:
                        nsteps.pop(0)()
                    xsrc = src
                    xsv = xsrc.rearrange('(kc p) t -> p kc t', p=128)
                    def xrs_load(mo_):
                        pb_ = mo_ % 2
                        P.add('pool', lambda e: e.dma_start(out=xrs[pb_][:, 0:T], in_=xsv[:, mo_, t0:t0 + T]), reads=[DR(id(xsrc), ti)], writes=[r_xrs[pb_]], dma=True)

                    xrs_load(0)
                    xrs_load(1)
                    for mo in range(KC):
                        pb = mo % 2
                        for kc in range(FC):
                            P.add('pe', lambda e, pb=pb, mo=mo, kc=kc: e.matmul(po[pb][:, 0:T], lhsT=w2[:, kc, mo * 128:(mo + 1) * 128], rhs=act[:, kc, 0:T],
                                                                               start=(kc == 0), stop=(kc == FC - 1)),
                                  reads=[r_w2[kc], r_act[kc]], writes=[r_po[pb]])
                        P.add('dve', lambda e, pb=pb, mo=mo: e.scalar_tensor_tensor(out=xo[pb][:, 0:T], in0=po[pb][:, 0:T], scalar=mt(2, n, mo, j), in1=xrs[pb][:, 0:T],
                                                                                   op0=ALU.mult, op1=ALU.add),
                              reads=[r_po[pb], r_xrs[pb], r_mtab], writes=[r_xo[pb]])
                        P.add('pool', lambda e, pb=pb, mo=mo: e.dma_start(out=dstv[:, mo, t0:t0 + T], in_=xo[pb][:, 0:T]), reads=[r_xo[pb]], writes=[DR(id(dst), ti)], dma=True)
                        if mo + 2 < KC:
                            xrs_load(mo + 2)

                prepare(0)
                for k in range(len(my_tiles)):
                    do_tile(k)
                P.emit_pass()

        def pass_inproj(l, src):
            with ExitStack() as es:
                sb = lambda name, shape, dt=F32: es.enter_context(_sbuf_tensor(name, list(shape), dt))
                win = sb('win', [128, KC, INX], BF16)
                r_win = [Res() for _ in range(KC)]
                load_w(win, 'win_%d' % l, win_in[l], KC, INX, r_win)
                conv_issue()
                prep = Prep(es, 'i')
                xin = [sb('xin%d' % i, [128, 512]) for i in range(3)]
                r_xin = [Res() for _ in range(3)]
                rC = [sb('rC%d' % i, [128, 512]) for i in range(2)]
                rS = [sb('rS%d' % i, [128, 512]) for i in range(2)]
                r_rope = [Res() for _ in range(2)]
                qkw = sb('qkw', [128, 4])
                qkws = sb('qkws', [128, 2])
                r_qkw = Res()
                lnw = sb('lnw', [128, 256])
                lnb = sb('lnb', [128, 256])
                r_ln = Res()
                P.add('sp', lambda e: e.dma_start(out=qkw[:], in_=qkn_in), writes=[r_qkw], dma=True)
                P.add('dve', lambda e: e.tensor_scalar(out=qkws[:, 0:1], in0=qkw[:, 2 * l:2 * l + 1], scalar1=0.125, scalar2=None, op0=ALU.mult), reads=[r_qkw], writes=[r_qkw])
                P.add('dve', lambda e: e.tensor_copy(out=qkws[:, 1:2], in_=qkw[:, 2 * l + 1:2 * l + 2]), reads=[r_qkw], writes=[r_qkw])
                P.add('sp', lambda e: e.dma_start(out=lnw[:], in_=lnw_in[:, l * 256:(l + 1) * 256]), writes=[r_ln], dma=True)
                P.add('sp', lambda e: e.dma_start(out=lnb[:], in_=lnb_in[:, l * 256:(l + 1) * 256]), writes=[r_ln], dma=True)
                st_q = [sb('stq%d' % i, [128, 3, 512], BF16) for i in range(2)]
                st_k = [sb('stk%d' % i, [128, 3, 512], BF16) for i in range(2)]
                st_u = [sb('stu%d' % i, [128, 2, 512], BF16) for i in range(2)]
                st_rq = [sb('strq%d' % i, [128, 3, 512], BF16) for i in range(2)]
                st_rk = [sb('strk%d' % i, [128, 3, 512], BF16) for i in range(2)]
                r_stq = [Res() for _ in range(2)]; r_stk = [Res() for _ in range(2)]; r_stu = [Res() for _ in range(2)]
                r_strq = [Res() for _ in range(2)]; r_strk = [Res() for _ in range(2)]
                st_v = [sb('stv%d' % i, [128, 4, 384], BF16) for i in range(2)]
                st_rv = [sb('strv%d' % i, [128, 4, 384], BF16) for i in range(2)]
                st_g = [sb('stg%d' % i, [128, 4, 768], BF16) for i in range(2)]
                st_vn = [sb('stvn%d' % i, [128, 4, 256], BF16) for i in range(2)]
                r_stv = [[Res() for _ in range(4)] for _ in range(2)]; r_strv = [[Res() for _ in range(4)] for _ in range(2)]; r_stg = [[Res() for _ in range(4)] for _ in range(2)]; r_stvn = [[Res() for _ in range(4)] for _ in range(2)]
                sqb = [sb('qsq%d' % i, [128, 512], BF16) for i in range(2)]
                r_sqb = [Res() for _ in range(2)]
                lnq = [sb('lnq%d' % i, [128, 512]) for i in range(2)]
                r_lnq = [Res() for _ in range(2)]
                rsq = [sb('rsq%d' % i, [128, 512]) for i in range(2)]
                r_rsq = [Res() for _ in range(2)]
                t1 = [sb('t1%d' % i, [128, 512]) for i in range(2)]
                t2 = [sb('t2%d' % i, [128, 512]) for i in range(2)]
                r_t1 = [Res() for _ in range(2)]; r_t2 = [Res() for _ in range(2)]
                gv = [sb('gv%d' % i, [128, 256]) for i in range(2)]
                r_gv = [Res() for _ in range(2)]
                gsq = sb('gsq', [128, 256]); r_gsq = Res()
                cen = sb('cen', [128, 256]); r_cen = Res()
                sm = sb('sm', [128, 24]); r_sm = Res()
                pf = [es.enter_context(_psum_tensor('pf%d' % i, [128, 512], F32)) for i in range(4)]
                r_pf = [Res() for _ in range(4)]
                pt = [es.enter_context(_psum_tensor('pt%d' % i, [128, 512], F32)) for i in range(3)]
                r_pt = [Res() for _ in range(3)]
                srcv = src.rearrange('(kc p) t -> p kc t', p=128)
                cnt = {'xin': 0, 'pf': 0, 'pt': 0, 'w': 0}

                def prepare_steps(k):
                    ti, (t0, T, j) = k, tiles[k]
                    slot = k % 2

                    def load_chunk(kc):
                        b = cnt['xin'] % 3
                        cnt['xin'] += 1
                        P.add('sp', lambda e: e.dma_start(out=xin[b][:, 0:T], in_=srcv[:, kc, t0:t0 + T]), reads=[DR(id(src), ti)], writes=[r_xin[b]], dma=True)
                        return xin[b][:, 0:T], r_xin[b]

                    return prep.steps(slot, T, 1, j, load_chunk)

                def prepare(k):
                    for st_ in prepare_steps(k):
                        st_()

                nsteps = []

                def pop_step():
                    if nsteps:
                        nsteps.pop(0)()

                def fm_mm(colbase, c, T, hT, r_hT):
                    pb = cnt['pf'] % 4
                    cnt['pf'] += 1
                    c0 = colbase + c * 128
                    for kc in range(KC):
                        P.add('pe', lambda e, kc=kc: e.matmul(pf[pb][:, 0:T], lhsT=win[:, kc, c0:c0 + 128], rhs=hT[:, kc, 0:T], start=(kc == 0), stop=(kc == KC - 1)),
                              reads=[r_win[kc], r_hT], writes=[r_pf[pb]])
                    pop_step()
                    return pb

                def do_tile(k):
                    ti, (t0, T, j) = k, tiles[k]
                    slot = k % 2
                    hT = prep.hT[slot]
                    r_hT = prep.r_hT[slot]
                    sl = k % 2
                    P.add('sp', lambda e: e.dma_start(out=rC[sl][:, 0:T], in_=ropeC[:, t0:t0 + T]), writes=[r_rope[sl]], dma=True)
                    P.add('sp', lambda e: e.dma_start(out=rS[sl][:, 0:T], in_=ropeS[:, t0:t0 + T]), writes=[r_rope[sl]], dma=True)
                    def qk_a(colbase, c):
                        A = fm_mm(colbase, c, T, hT, r_hT)
                        w = cnt['w'] % 2
                        cnt['w'] += 1
                        P.add('act', lambda e: e.activation(out=sqb[w][:, 0:T], in_=pf[A][:, 0:T], func=AF.Square), reads=[r_pf[A]], writes=[r_sqb[w]])
                        return A, w

                    def qk_b(c, stg, r_stg_, wi, A, w):
                        B = cnt['pf'] % 4
                        cnt['pf'] += 1
                        P.add('pe', lambda e: e.matmul(pf[B][:, 0:T], lhsT=bd_b, rhs=sqb[w][:, 0:T], start=True, stop=True), reads=[r_sqb[w], r_cstbf], writes=[r_pf[B]])
                        P.add('act', lambda e: e.activation(out=lnq[w][:, 0:T], in_=pf[B][:, 0:T], func=AF.Ln, bias=RMS_EPS), reads=[r_pf[B]], writes=[r_lnq[w]])
                        P.add('act', lambda e: e.activation(out=rsq[w][:, 0:T], in_=lnq[w][:, 0:T], func=AF.Exp, scale=-0.5), reads=[r_lnq[w]], writes=[r_rsq[w]])
                        P.add('dve', lambda e: e.scalar_tensor_tensor(out=stg[:, c, 0:T], in0=pf[A][:, 0:T], scalar=qkws[:, wi:wi + 1], in1=rsq[w][:, 0:T], op0=ALU.mult, op1=ALU.mult),
                              reads=[r_pf[A], r_rsq[w], r_qkw], writes=[r_stg_])

                    items = [(0, c, st_q[sl], r_stq[sl], 0) for c in range(3)] + [(384, c, st_k[sl], r_stk[sl], 1) for c in range(3)]
                    prev = None
                    for (colbase, c, stg, r_stg_, wi) in items:
                        A, w = qk_a(colbase, c)
                        if prev is not None:
                            qk_b(*prev)
                        prev = (c, stg, r_stg_, wi, A, w)
                    qk_b(*prev)
                    def fm_store(stg, r_stg_, dten):
                        dv = dten.rearrange('(c p) t -> p c t', p=128)[:, :, t0:t0 + T]
                        P.add('pool', lambda e: e.dma_start(out=dv, in_=stg[:, :, 0:T]), reads=[r_stg_], writes=[DR(id(dten), ti)], dma=True)

                    fm_store(st_q[sl], r_stq[sl], qT_na)
                    fm_store(st_k[sl], r_stk[sl], kT_na)
                    for c in range(2):
                        A = fm_mm(1152, c, T, hT, r_hT)
                        P.add('act', lambda e, A=A, c=c: e.activation(out=st_u[sl][:, c, 0:T], in_=pf[A][:, 0:T], func=AF.Gelu_apprx_tanh), reads=[r_pf[A]], writes=[r_stu[sl]])
                    fm_store(st_u[sl], r_stu[sl], uT_sg)
                    for (colbase, pbase, stg, r_stg_) in ((1664, 3584, st_rq[sl], r_strq[sl]), (2048, 3968, st_rk[sl], r_strk[sl])):
                        for c in range(3):
                            A = fm_mm(colbase, c, T, hT, r_hT)
                            A2 = fm_mm(pbase, c, T, hT, r_hT)
                            w = cnt['w'] % 2
                            cnt['w'] += 1
                            P.add('dve', lambda e, A=A, w=w: e.tensor_tensor(out=t1[w][:, 0:T], in0=pf[A][:, 0:T], in1=rC[sl][:, 0:T], op=ALU.mult), reads=[r_pf[A], r_rope[sl]], writes=[r_t1[w]])
                            P.add('dve', lambda e, A2=A2, w=w: e.tensor_tensor(out=t2[w][:, 0:T], in0=pf[A2][:, 0:T], in1=rS[sl][:, 0:T], op=ALU.mult), reads=[r_pf[A2], r_rope[sl]], writes=[r_t2[w]])
                            P.add('pool', lambda e, w=w, c=c, stg=stg: e.tensor_tensor(out=stg[:, c, 0:T], in0=t1[w][:, 0:T], in1=t2[w][:, 0:T], op=ALU.add), reads=[r_t1[w], r_t2[w]], writes=[r_stg_])
                        fm_store(stg, r_stg_, rqT if colbase == 1664 else rkT)
                    def do_sub(s):
                        def tm_mm(c0, ncol):
                            pb = cnt['pt'] % 3
                            cnt['pt'] += 1
                            for kc in range(KC):
                                P.add('pe', lambda e, kc=kc: e.matmul(pt[pb][:, 0:ncol], lhsT=hT[:, kc, s * 128:(s + 1) * 128], rhs=win[:, kc, c0:c0 + ncol], start=(kc == 0), stop=(kc == KC - 1)),
                                      reads=[r_win[kc], r_hT], writes=[r_pt[pb]])
                            return pb
                        A = tm_mm(768, 384)
                        P.add('act', lambda e, A=A: e.copy(out=st_v[sl][:, s, :], in_=pt[A][:, 0:384]), reads=[r_pt[A]], writes=[r_stv[sl][s]])
                        A = tm_mm(2432, 384)
                        P.add('dve', lambda e, A=A: e.tensor_copy(out=st_rv[sl][:, s, :], in_=pt[A][:, 0:384]), reads=[r_pt[A]], writes=[r_strv[sl][s]])
                        A = tm_mm(2816, 384)
                        P.add('act', lambda e, A=A: e.activation(out=st_g[sl][:, s, 0:384], in_=pt[A][:, 0:384], func=AF.Silu), reads=[r_pt[A]], writes=[r_stg[sl][s]])
                        A = tm_mm(3200, 384)
                        P.add('act', lambda e, A=A: e.activation(out=st_g[sl][:, s, 384:768], in_=pt[A][:, 0:384], func=AF.Silu), reads=[r_pt[A]], writes=[r_stg[sl][s]])
                        A = tm_mm(1408, 256)
                        g = cnt['w'] % 2
                        cnt['w'] += 1
                        P.add('act', lambda e, A=A, g=g: e.activation(out=gv[g][:], in_=pt[A][:, 0:256], func=AF.Gelu_apprx_tanh), reads=[r_pt[A]], writes=[r_gv[g]])
                        g3 = lambda ap: ap.rearrange('p (g c) -> p g c', g=4)
                        bc = lambda ap: ap.unsqueeze(2).broadcast_to([128, 4, 64])
                        P.add('dve', lambda e, g=g: e.tensor_reduce(out=sm[:, 0:4], in_=g3(gv[g][:]), axis=AX.X, op=ALU.add), reads=[r_gv[g]], writes=[r_sm])
                        P.add('dve', lambda e, g=g: e.tensor_tensor(out=gsq[:], in0=gv[g][:], in1=gv[g][:], op=ALU.mult), reads=[r_gv[g]], writes=[r_gsq])
                        P.add('dve', lambda e: e.tensor_reduce(out=sm[:, 4:8], in_=g3(gsq[:]), axis=AX.X, op=ALU.add), reads=[r_gsq], writes=[r_sm])
                        P.add('dve', lambda e: e.tensor_scalar(out=sm[:, 8:12], in0=sm[:, 0:4], scalar1=1.0 / 64, scalar2=None, op0=ALU.mult), reads=[r_sm], writes=[r_sm])
                        P.add('dve', lambda e: e.tensor_tensor(out=sm[:, 12:16], in0=sm[:, 8:12], in1=sm[:, 8:12], op=ALU.mult), reads=[r_sm], writes=[r_sm])
                        P.add('dve', lambda e: e.scalar_tensor_tensor(out=sm[:, 16:20], in0=sm[:, 4:8], scalar=1.0 / 64, in1=sm[:, 12:16], op0=ALU.mult, op1=ALU.subtract), reads=[r_sm], writes=[r_sm])
                        P.add('act', lambda e: e.activation(out=sm[:, 16:20], in_=sm[:, 16:20], func=AF.Sqrt, bias=LN_EPS), reads=[r_sm], writes=[r_sm])
                        P.add('dve', lambda e: e.reciprocal(out=sm[:, 20:24], in_=sm[:, 16:20]), reads=[r_sm], writes=[r_sm])
                        P.add('dve', lambda e, g=g: e.tensor_tensor(out=g3(cen[:]), in0=g3(gv[g][:]), in1=bc(sm[:, 8:12]), op=ALU.subtract), reads=[r_gv[g], r_sm], writes=[r_cen])
                        P.add('dve', lambda e: e.tensor_tensor(out=g3(cen[:]), in0=g3(cen[:]), in1=bc(sm[:, 20:24]), op=ALU.mult), reads=[r_cen, r_sm], writes=[r_cen])
                        P.add('pool', lambda e: e.tensor_tensor(out=cen[:], in0=cen[:], in1=lnw[:], op=ALU.mult), reads=[r_cen, r_ln], writes=[r_cen])
                        P.add('pool', lambda e: e.tensor_tensor(out=st_vn[sl][:, s, :], in0=cen[:], in1=lnb[:], op=ALU.add), reads=[r_cen, r_ln], writes=[r_stvn[sl][s]])
                        for (stg, r_stg_, dten) in ((st_v[sl], r_stv[sl][s], v_na), (st_rv[sl], r_strv[sl][s], rv_d), (st_g[sl], r_stg[sl][s], gates_d), (st_vn[sl], r_stvn[sl][s], vn_sg)):
                            P.add('pool', lambda e, stg=stg, dten=dten: e.dma_start(out=dten[t0 + s * 128:t0 + (s + 1) * 128, :], in_=stg[:, s, :]), reads=[r_stg_], writes=[DR(id(dten), ti)], dma=True)
                    for s_ in range(T // 128):
                        do_sub(s_)

                prepare(0)
                for k in range(len(tiles)):
                    if k + 1 < len(tiles):
                        nsteps.extend(prepare_steps(k + 1))
                    do_tile(k)
                    while nsteps:
                        nsteps.pop(0)()
                P.emit_pass()

        def pass_outproj(l, src, dst, skip_ctx):
            with ExitStack() as es:
                sb = lambda name, shape, dt=F32: es.enter_context(_sbuf_tensor(name, list(shape), dt))
                wo = sb('wo', [128, KC, D], BF16)
                r_wo = [Res() for _ in range(KC)]
                load_w(wo, 'wo_%d' % l, wout_in[l], KC, D, r_wo)
                conv_issue()
                ct = [sb('ct%d' % i, [128, KC, 512], BF16) for i in range(2)]
                r_ct = [Res() for _ in range(2)]
                xin = [sb('xin%d' % i, [128, 512]) for i in range(3)]
                r_xin = [Res() for _ in range(3)]
                xo = [sb('xo%d' % i, [128, 512]) for i in range(3)]
                r_xo = [Res() for _ in range(3)]
                po = [es.enter_context(_psum_tensor('po%d' % i, [128, 512], F32)) for i in range(3)]
                r_po = [Res() for _ in range(3)]
                srcv = src.rearrange('(kc p) t -> p kc t', p=128)
                dstv = dst.rearrange('(kc p) t -> p kc t', p=128)
                catv = catT.rearrange('(kc p) t -> p kc t', p=128)
                cnt = {'i': 0}

                def do_tile(ti):
                    t0, T, j = tiles[ti]
                    sl = ti % 2
                    P.add('sp', lambda e: e.dma_start(out=ct[sl][:, :, 0:T], in_=catv[:, :, t0:t0 + T]), reads=[DR(id(catT), ti)], writes=[r_ct[sl]], dma=True)
                    for mo in range(KC):
                        b = cnt['i'] % 3
                        cnt['i'] += 1
                        P.add('sp', lambda e, b=b, mo=mo: e.dma_start(out=xin[b][:, 0:T], in_=srcv[:, mo, t0:t0 + T]), reads=[DR(id(src), ti)], writes=[r_xin[b]], dma=True)
                        for kc in range(KC):
                            P.add('pe', lambda e, b=b, mo=mo, kc=kc: e.matmul(po[b][:, 0:T], lhsT=wo[:, kc, mo * 128:(mo + 1) * 128], rhs=ct[sl][:, kc, 0:T], start=(kc == 0), stop=(kc == KC - 1)),
                                  reads=[r_wo[kc], r_ct[sl]], writes=[r_po[b]])
                        P.add('dve', lambda e, b=b, mo=mo: e.scalar_tensor_tensor(out=xo[b][:, 0:T], in0=po[b][:, 0:T], scalar=mt(2, 1, mo, j), in1=xin[b][:, 0:T], op0=ALU.mult, op1=ALU.add),
                              reads=[r_po[b], r_xin[b], r_mtab], writes=[r_xo[b]])
                        P.add('pool', lambda e, b=b, mo=mo: e.dma_start(out=dstv[:, mo, t0:t0 + T], in_=xo[b][:, 0:T]), reads=[r_xo[b]], writes=[DR(id(dst), ti)], dma=True)

                for ti in range(len(tiles)):
                    if skip_ctx and tiles[ti][2] == 1:
                        continue
                    do_tile(ti)
                P.emit_pass()

        def pass_mixer(l, last):
            with ExitStack() as es:
                conv_issue()
                sb = lambda name, shape, dt=F32: es.enter_context(_sbuf_tensor(name, list(shape), dt))
                E = sb('E', [128, NV, 6, 5, 128], BF16)
                r_E = Res()
                Sst = sb('Sst', [128, NCH, 2, 3, 64], BF16)
                r_Sst = [[Res(), Res()] for _ in range(NCH)]
                dl = sb('dl', [128, 12]); e1 = sb('e1', [128, 12]); lg = sb('lg', [128, 12]); nlg = sb('nlg', [128, 12])
                lgsel = sb('lgsel', [128, 2, 3]); g128 = sb('g128', [128, 2, 3]); te = sb('te', [128, 2, 6])
                fs = sb('fs', [128, 2, 3, 128]); fsm = sb('fsm', [128, 2, 6, 128]); Dm = sb('Dm', [128, 2, 6, 128])
                r_tab = Res()
                st = sb('st', [128, 2, 3, 64])
                r_st = [Res(), Res()]
                sgw = sb('sgw', [128, 4, 128], BF16); sgbt = sb('sgbt', [128, 2, 128]); gnw = sb('gnw', [128, 384])
                r_sgt = Res()
                pS = es.enter_context(_psum_tensor('pS', [128, 1024], F32)); r_pS = Res()
                pS2 = es.enter_context(_psum_tensor('pS2', [128, 1024], F32)); r_pS2 = Res()
                pO = es.enter_context(_psum_tensor('pO', [128, 512], F32)); r_pO = Res()
                pRo = es.enter_context(_psum_tensor('pRo', [128, 1024], F32)); r_pRo = Res()
                pT = es.enter_context(_psum_tensor('pT', [128, 1024], BF16)); r_pT = Res()
                pG = pRo[:, 768:1024]; r_pG = Res()
                pKV = pS2[:, 0:512]; r_pKV = r_pS2

                P.add('sp', lambda e: e.dma_start(out=dl[:], in_=dlog_in[:, l * 12:(l + 1) * 12]), writes=[r_tab], dma=True)
                P.add('act', lambda e: e.activation(out=e1[:], in_=dl[:], func=AF.Exp, scale=-1.0), reads=[r_tab], writes=[r_tab])
                P.add('act', lambda e: e.activation(out=nlg[:], in_=e1[:], func=AF.Ln, bias=1.0), reads=[r_tab], writes=[r_tab])
                P.add('dve', lambda e: e.tensor_scalar(out=lg[:], in0=nlg[:], scalar1=-1.0, scalar2=None, op0=ALU.mult), reads=[r_tab], writes=[r_tab])
                for d_ in range(2):
                    v2 = lg[:, d_ * 6:(d_ + 1) * 6].rearrange('p (r two) -> p r two', two=2)
                    P.add('dve', lambda e, d_=d_, v2=v2: e.tensor_copy(out=lgsel[0:64, d_, :], in_=v2[0:64, :, 0]), reads=[r_tab], writes=[r_tab])
                    P.add('dve', lambda e, d_=d_, v2=v2: e.tensor_copy(out=lgsel[64:128, d_, :], in_=v2[64:128, :, 1]), reads=[r_tab], writes=[r_tab])
                P.add('act', lambda e: e.activation(out=g128[:], in_=lgsel[:], func=AF.Exp, scale=128.0), reads=[r_tab], writes=[r_tab])
                P.add('act', lambda e: e.activation(out=te[:, 0, :], in_=lg[:, 0:6], func=AF.Exp, scale=cst[:, C_PREV:C_PREV + 1]), reads=[r_tab, r_cst], writes=[r_tab])
                P.add('act', lambda e: e.activation(out=te[:, 1, :], in_=lg[:, 6:12], func=AF.Exp, scale=cst[:, C_PCOL:C_PCOL + 1]), reads=[r_tab, r_cst], writes=[r_tab])
                for pr in range(3):
                    P.add('act', lambda e, pr=pr: e.activation(out=fs[:, 0, pr, :], in_=cst[:, C_IP1:C_IP1 + 128], func=AF.Exp, scale=lgsel[:, 0, pr:pr + 1], bias=lnb8[:, 0:1]),
                          reads=[r_tab, r_cst], writes=[r_tab])
                    P.add('act', lambda e, pr=pr: e.activation(out=fs[:, 1, pr, :], in_=cst[:, C_REV:C_REV + 128], func=AF.Exp, scale=lgsel[:, 1, pr:pr + 1], bias=lnb8[:, 0:1]),
                          reads=[r_tab, r_cst], writes=[r_tab])
                P.add('dve', lambda e: e.memset(fsm[:], 0.0), writes=[r_tab])
                for d_ in range(2):
                    for h in range(6):
                        r0_ = (h % 2) * 64
                        P.add('dve', lambda e, d_=d_, h=h, r0_=r0_: e.tensor_copy(out=fsm[r0_:r0_ + 64, d_, h, :], in_=fs[r0_:r0_ + 64, d_, h // 2, :]), reads=[r_tab], writes=[r_tab])
                for h in range(6):
                    P.add('act', lambda e, h=h: e.activation(out=Dm[:, 0, h, :], in_=cst[:, C_DIFF:C_DIFF + 128], func=AF.Exp, scale=lg[:, h:h + 1], bias=lnb8[:, 0:1]),
                          reads=[r_tab, r_cst], writes=[r_tab])
                    P.add('act', lambda e, h=h: e.activation(out=Dm[:, 1, h, :], in_=cst[:, C_DIFF:C_DIFF + 128], func=AF.Exp, scale=nlg[:, 6 + h:7 + h], bias=lnb8[:, 0:1]),
                          reads=[r_tab, r_cst], writes=[r_tab])
                mFb = cst[:, C_MF:C_MF + 128].unsqueeze(1).broadcast_to([128, 6, 128])
                mBb = cst[:, C_MB:C_MB + 128].unsqueeze(1).broadcast_to([128, 6, 128])
                P.add('dve', lambda e: e.tensor_tensor(out=Dm[:, 0, :, :], in0=Dm[:, 0, :, :], in1=mFb, op=ALU.mult), reads=[r_tab, r_cst], writes=[r_tab])
                P.add('dve', lambda e: e.tensor_tensor(out=Dm[:, 1, :, :], in0=Dm[:, 1, :, :], in1=mBb, op=ALU.mult), reads=[r_tab, r_cst], writes=[r_tab])
                bst = [sb('bst%d' % i, [128, 5, 128]) for i in range(2)]
                r_bst = [Res(), Res()]
                for v in range(NV):
                    for h in range(6):
                        b = (v * 6 + h) % 2
                        P.add('sp', lambda e, b=b, v=v, h=h: e.dma_start(out=bst[b][:], in_=bias_in[l, v, h]), writes=[r_bst[b]], dma=True)
                        P.add('act', lambda e, b=b, v=v, h=h: e.activation(out=E[:, v, h, :, :], in_=bst[b][:], func=AF.Exp), reads=[r_bst[b]], writes=[r_E])
                P.add('pool', lambda e: e.dma_start(out=sgw[:], in_=sgw_in[l].rearrange('g q p -> q g p')), writes=[r_sgt], dma=True)
                P.add('sp', lambda e: e.dma_start(out=sgbt[:], in_=sgb_in[l].rearrange('g p f -> p g f')), writes=[r_sgt], dma=True)
                P.add('sp', lambda e: e.dma_start(out=gnw[:], in_=gnw_in[:, l * 384:(l + 1) * 384]), writes=[r_sgt], dma=True)

                qv = lambda ten: ten.rearrange('(c p) t -> p c t', p=128)
                rkb = [sb('rkb%d' % i, [128, 3, 128], BF16) for i in range(2)]
                rvb = [sb('rvb%d' % i, [128, 384], BF16) for i in range(2)]
                r_rkb = [Res(), Res()]; r_rvb = [Res(), Res()]
                kte = [sb('kte%d' % i, [128, 384], BF16) for i in range(2)]
                r_kte = [Res(), Res()]
                P.add('dve', lambda e: e.memset(st[:], 0.0), writes=[r_st[0], r_st[1]])
                order_f = [NLC, NLC + 1] + list(range(NLC))
                order_b = [NLC + 1, NLC] + list(range(NLC - 1, -1, -1))
                sc = {'i': 0}

                def scan_step(dr, c):
                    i = sc['i'] % 2
                    sc['i'] += 1
                    tok = chunk_tok(c)
                    tl = tile_of_chunk(c)
                    P.add('act', lambda e: e.copy(out=Sst[:, c, dr, :, :], in_=st[:, dr, :, :]), reads=[r_st[dr]], writes=[r_Sst[c][dr]])
                    P.add('sp', lambda e: e.dma_start(out=rkb[i][:], in_=qv(rkT)[:, :, tok:tok + 128]), reads=[DR(id(rkT), tl)], writes=[r_rkb[i]], dma=True)
                    P.add('sp', lambda e: e.dma_start(out=rvb[i][:], in_=rv_d[tok:tok + 128, :]), reads=[DR(id(rv_d), tl)], writes=[r_rvb[i]], dma=True)
                    for pr in range(3):
                        P.add('pe', lambda e, pr=pr: e.transpose(pT[:, pr * 128:(pr + 1) * 128], rkb[i][:, pr, :], ident_b), reads=[r_rkb[i], r_cstbf], writes=[r_pT])
                    P.add('dve', lambda e: e.tensor_tensor(out=kte[i][:].rearrange('p (h d) -> p h d', h=6), in0=pT[:, 0:384].rearrange('p (h d) -> p h d', h=6),
                                                           in1=te[:, dr, :].unsqueeze(2).broadcast_to([128, 6, 64]), op=ALU.mult),
                          reads=[r_pT, r_tab], writes=[r_kte[i]])
                    for pr in range(3):
                        P.add('pe', lambda e, pr=pr: e.matmul(pS2[:, pr * 128:(pr + 1) * 128], lhsT=kte[i][:, pr * 128:(pr + 1) * 128], rhs=rvb[i][:, pr * 128:(pr + 1) * 128], start=True, stop=True),
                              reads=[r_kte[i], r_rvb[i]], writes=[r_pKV])
                    P.add('dve', lambda e: e.tensor_tensor(out=st[:, dr, :, :], in0=st[:, dr, :, :], in1=g128[:, dr, :].unsqueeze(2).broadcast_to([128, 3, 64]), op=ALU.mult),
                          reads=[r_st[dr], r_tab], writes=[r_st[dr]])
                    kv3 = pS2[:, 0:384].rearrange('p (r c) -> p r c', r=3)
                    P.add('dve', lambda e: e.tensor_tensor(out=st[0:64, dr, :, :], in0=st[0:64, dr, :, :], in1=kv3[0:64, :, 0:64], op=ALU.add), reads=[r_st[dr], r_pKV], writes=[r_st[dr]])
                    P.add('dve', lambda e: e.tensor_tensor(out=st[64:128, dr, :, :], in0=st[64:128, dr, :, :], in1=kv3[64:128, :, 64:128], op=ALU.add), reads=[r_st[dr], r_pKV], writes=[r_st[dr]])

                if 'scan' in MIXDBG:
                    for c in order_f:
                        scan_step(0, c)
                    for c in order_b:
                        scan_step(1, c)

                NSLOT = 8
                kslot = [sb('ks%d' % i, [128, 3, 128], BF16) for i in range(NSLOT + 2)]
                vslot = [sb('vs%d' % i, [128, 6, 65], BF16) for i in range(NSLOT + 2)]
                r_slot = [Res() for _ in range(NSLOT + 2)]
                slot_chunk = [None] * (NSLOT + 2)
                for i in range(NSLOT + 2):
                    P.add('pool', lambda e, i=i: e.memset(vslot[i][:], 1.0), writes=[r_slot[i]])
                dbl = lambda name, shape, dt=BF16: [sb(name + '%d' % i, shape, dt) for i in range(2)]
                qna = dbl('qna', [128, 3, 128]); uu = dbl('uu', [128, 2, 128]); vnb = dbl('vnb', [128, 256])
                rqb = dbl('rqb', [128, 3, 128]); rkc = dbl('rkc', [128, 3, 128]); rvc = dbl('rvc', [128, 384]); gtb = dbl('gtb', [128, 768])
                r_ld = [Res(), Res()]
                expS = dbl('expS', [128, 7, 128]); r_expS = [Res(), Res()]
                qfs = dbl('qfs', [128, 2, 6, 128]); r_qfs = [Res(), Res()]
                SD = dbl('SD', [128, 2, 6, 128]); r_SD = [Res(), Res()]
                natok = dbl('natok', [128, 384]); r_natok = [Res(), Res()]
                rettok = dbl('rettok', [128, 384]); r_rettok = [Res(), Res()]
                catst = dbl('catst', [128, 8, 128]); r_catst = [Res(), Res()]
                rec = dbl('rec', [128, 6], F32); r_rec = [Res(), Res()]
                sgt = dbl('sgtmp', [128, 2, 128], F32); r_sgtmp = [Res(), Res()]
                osb = dbl('osb', [128, 768], F32); r_osb = [Res(), Res()]
                osq = dbl('osq', [128, 768], F32); r_osq = [Res(), Res()]
                gsm = dbl('gsm', [128, 72], F32); r_gsm = [Res(), Res()]
                g2 = dbl('g2', [128, 768], F32); r_g2 = [Res(), Res()]
                g3t = dbl('g3t', [128, 384], F32); r_g3 = [Res(), Res()]

                def ensure_slot(kc_):
                    if kc_ >= NLC:
                        s = NSLOT + (kc_ - NLC)
                    else:
                        s = kc_ % NSLOT
                    if slot_chunk[s] != kc_:
                        slot_chunk[s] = kc_
                        tok = chunk_tok(kc_)
                        tl = tile_of_chunk(kc_)
                        P.add('sp', lambda e: e.dma_start(out=kslot[s][:], in_=qv(kT_na)[:, :, tok:tok + 128]), reads=[DR(id(kT_na), tl)], writes=[r_slot[s]], dma=True)
                        P.add('sp', lambda e: e.dma_start(out=vslot[s][:, :, 0:64], in_=v_na[tok:tok + 128, :].rearrange('p (h d) -> p h d', h=6)), reads=[DR(id(v_na), tl)], writes=[r_slot[s]], dma=True)
                    return s

                def do_chunk(c, idx):
                    cb = idx % 2
                    tok = chunk_tok(c)
                    tl = tile_of_chunk(c)
                    is_ctx = c >= NLC
                    for (dst_, ten, fm) in ((qna[cb], qT_na, True), (uu[cb], uT_sg, True), (rqb[cb], rqT, True), (rkc[cb], rkT, True)):
                        P.add('sp', lambda e, dst_=dst_, ten=ten: e.dma_start(out=dst_[:], in_=qv(ten)[:, :, tok:tok + 128]), reads=[DR(id(ten), tl)], writes=[r_ld[cb]], dma=True)
                    for (dst_, ten) in ((vnb[cb], vn_sg), (rvc[cb], rv_d), (gtb[cb], gates_d)):
                        P.add('sp', lambda e, dst_=dst_, ten=ten: e.dma_start(out=dst_[:], in_=ten[tok:tok + 128, :]), reads=[DR(id(ten), tl)], writes=[r_ld[cb]], dma=True)
                    if is_ctx:
                        kchunks = []
                    else:
                        kchunks = [base_of[c] + m for m in range(5)]
                    kchunks = kchunks + [NLC, NLC + 1]
                    slots = [ensure_slot(kc_) for kc_ in kchunks]
                    nb = len(slots)
                    var = None if is_ctx else var_of[c]
                    def sec_na(pend):
                        pSb = [pS, pS2]
                        r_pSb = [r_pS, r_pS2]

                        def scores(h):
                            buf, pc, r0 = h % 2, h // 2, (h % 2) * 64
                            for bi, s in enumerate(slots):
                                P.add('pe', lambda e, bi=bi, s=s: e.matmul(pSb[buf][:, bi * 128:(bi + 1) * 128], lhsT=kslot[s][r0:r0 + 64, pc, :], rhs=qna[cb][r0:r0 + 64, pc, :], start=True, stop=True),
                                      reads=[r_slot[s], r_ld[cb]], writes=[r_pSb[buf]])

                        def soft(h):
                            buf = eb = h % 2
                            P.add('act', lambda e: e.activation(out=expS[eb][:, 0:nb, :], in_=pSb[buf][:, 0:nb * 128].rearrange('p (b q) -> p b q', b=nb), func=AF.Exp),
                                  reads=[r_pSb[buf]], writes=[r_expS[eb]])
                            if not is_ctx:
                                P.add('dve' if h % 2 == 0 else 'pool', lambda e: e.tensor_tensor(out=expS[eb][:, 0:5, :], in0=expS[eb][:, 0:5, :], in1=E[:, var, h, :, :], op=ALU.mult),
                                      reads=[r_expS[eb], r_E], writes=[r_expS[eb]])

                        def pv(h):
                            eb = h % 2
                            for bi, s in enumerate(slots):
                                P.add('pe', lambda e, bi=bi, s=s: e.matmul(pO[:, h * 65:(h + 1) * 65], lhsT=expS[eb][:, bi, :], rhs=vslot[s][:, h, :], start=(bi == 0), stop=(bi == nb - 1)),
                                      reads=[r_expS[eb], r_slot[s]], writes=[r_pO])

                        scores(0)
                        for h in range(6):
                            if h + 1 < 6:
                                scores(h + 1)
                            soft(h)
                            pv(h)
                            for _ in range(3):
                                if pend:
                                    pend.pop(0)()
                        while pend:
                            pend.pop(0)()
                        po3 = pO[:, 0:390].rearrange('p (h d) -> p h d', h=6)
                        P.add('dve', lambda e: e.reciprocal(out=rec[cb][:], in_=po3[:, :, 64]), reads=[r_pO], writes=[r_rec[cb]])
                        P.add('dve', lambda e: e.tensor_tensor(out=natok[cb][:].rearrange('p (h d) -> p h d', h=6), in0=po3[:, :, 0:64], in1=rec[cb][:].unsqueeze(2).broadcast_to([128, 6, 64]), op=ALU.mult),
                              reads=[r_pO, r_rec[cb]], writes=[r_natok[cb]])

                    def sec_sg():
                        for gp in range(2):
                            for half in range(2):
                                g = 2 * gp + half
                                P.add('pe', lambda e, gp=gp, half=half, g=g: e.matmul(pRo[half * 64:(half + 1) * 64, 768 + gp * 128:768 + (gp + 1) * 128], lhsT=vnb[cb][:, g * 64:(g + 1) * 64], rhs=sgw[:, g, :], start=True, stop=True),
                                      reads=[r_ld[cb], r_sgt], writes=[r_pG])
                        P.add('dve', lambda e: e.tensor_tensor(out=sgt[cb][:], in0=pRo[:, 768:1024].rearrange('p (g q) -> p g q', g=2), in1=sgbt[:], op=ALU.add), reads=[r_pG, r_sgt], writes=[r_sgtmp[cb]])
                        P.add('dve', lambda e: e.tensor_tensor(out=catst[cb][:, 3:5, :], in0=sgt[cb][:], in1=uu[cb][:], op=ALU.mult), reads=[r_sgtmp[cb], r_ld[cb]], writes=[r_catst[cb]])

                    def sec_ret():
                        for dr in range(2):
                            P.add('dve', lambda e, dr=dr: e.tensor_tensor(out=qfs[cb][:, dr, :, :].rearrange('p (j two) q -> p j two q', two=2),
                                                                          in0=rqb[cb][:].unsqueeze(2).broadcast_to([128, 3, 2, 128]),
                                                                          in1=fsm[:, dr, :, :].rearrange('p (j two) q -> p j two q', two=2), op=ALU.mult),
                                  reads=[r_ld[cb], r_tab], writes=[r_qfs[cb]])
                        for h in range(6):
                            pc, half = h // 2, h % 2
                            r0 = half * 64
                            P.add('pe', lambda e, h=h, pc=pc, r0=r0, half=half: e.matmul(pS[:, half * 512 + pc * 128:half * 512 + (pc + 1) * 128], lhsT=rkc[cb][r0:r0 + 64, pc, :], rhs=rqb[cb][r0:r0 + 64, pc, :], start=True, stop=True),
                                  reads=[r_ld[cb]], writes=[r_pS])
                        for dr in range(2):
                            for par in range(2):
                                P.add('dve', lambda e, dr=dr, par=par: e.tensor_tensor(out=SD[cb][:, dr, :, :].rearrange('p (j two) q -> p j two q', two=2)[:, :, par, :],
                                                                                      in0=pS[:, par * 512:par * 512 + 384].rearrange('p (j q) -> p j q', j=3),
                                                                                      in1=Dm[:, dr, :, :].rearrange('p (j two) q -> p j two q', two=2)[:, :, par, :], op=ALU.mult),
                                      reads=[r_pS, r_tab], writes=[r_SD[cb]])
                        for dr in range(2):
                            for h in range(6):
                                pc, half = h // 2, h % 2
                                r0 = half * 64
                                ob = (dr * 6 + h) * 64
                                P.add('pe', lambda e, dr=dr, h=h, ob=ob: e.matmul(pRo[:, ob:ob + 64], lhsT=SD[cb][:, dr, h, :], rhs=rvc[cb][:, h * 64:(h + 1) * 64], start=True, stop=False),
                                      reads=[r_SD[cb], r_ld[cb]], writes=[r_pRo])
                                P.add('pe', lambda e, dr=dr, h=h, ob=ob, pc=pc, r0=r0: e.matmul(pRo[:, ob:ob + 64], lhsT=qfs[cb][:, dr, h, :], rhs=Sst[:, c, dr, pc, :], start=False, stop=True),
                                      reads=[r_qfs[cb], r_Sst[c][dr]], writes=[r_pRo])

                    def gn_steps():
                        st_ = []
                        o3 = lambda ap: ap.rearrange('p (g e) -> p g e', g=12)
                        b12 = lambda ap: ap.unsqueeze(2).broadcast_to([128, 12, 64])
                        st_.append(lambda: P.add('dve', lambda e: e.tensor_copy(out=osb[cb][:], in_=pRo[:, 0:768]), reads=[r_pRo], writes=[r_osb[cb]]))
                        st_.append(lambda: P.add('dve', lambda e: e.tensor_reduce(out=gsm[cb][:, 0:12], in_=o3(osb[cb][:]), axis=AX.X, op=ALU.add), reads=[r_osb[cb]], writes=[r_gsm[cb]]))
                        st_.append(lambda: P.add('act', lambda e: e.activation(out=osq[cb][:], in_=osb[cb][:], func=AF.Square), reads=[r_osb[cb]], writes=[r_osq[cb]]))
                        st_.append(lambda: P.add('dve', lambda e: e.tensor_reduce(out=gsm[cb][:, 12:24], in_=o3(osq[cb][:]), axis=AX.X, op=ALU.add), reads=[r_osq[cb]], writes=[r_gsm[cb]]))
                        st_.append(lambda: P.add('dve', lambda e: e.tensor_scalar(out=gsm[cb][:, 24:36], in0=gsm[cb][:, 0:12], scalar1=1.0 / 64, scalar2=None, op0=ALU.mult), reads=[r_gsm[cb]], writes=[r_gsm[cb]]))
                        st_.append(lambda: P.add('dve', lambda e: e.tensor_tensor(out=gsm[cb][:, 36:48], in0=gsm[cb][:, 24:36], in1=gsm[cb][:, 24:36], op=ALU.mult), reads=[r_gsm[cb]], writes=[r_gsm[cb]]))
                        st_.append(lambda: P.add('dve', lambda e: e.scalar_tensor_tensor(out=gsm[cb][:, 48:60], in0=gsm[cb][:, 12:24], scalar=1.0 / 64, in1=gsm[cb][:, 36:48], op0=ALU.mult, op1=ALU.subtract), reads=[r_gsm[cb]], writes=[r_gsm[cb]]))
                        st_.append(lambda: P.add('act', lambda e: e.activation(out=gsm[cb][:, 48:60], in_=gsm[cb][:, 48:60], func=AF.Sqrt, bias=lnbe[:, 0:1]), reads=[r_gsm[cb]], writes=[r_gsm[cb]]))
                        st_.append(lambda: P.add('dve', lambda e: e.reciprocal(out=gsm[cb][:, 60:72], in_=gsm[cb][:, 48:60]), reads=[r_gsm[cb]], writes=[r_gsm[cb]]))
                        st_.append(lambda: P.add('pool', lambda e: e.tensor_tensor(out=o3(g2[cb][:]), in0=o3(osb[cb][:]), in1=b12(gsm[cb][:, 24:36]), op=ALU.subtract), reads=[r_osb[cb], r_gsm[cb]], writes=[r_g2[cb]]))
                        st_.append(lambda: P.add('pool', lambda e: e.tensor_tensor(out=o3(g2[cb][:]), in0=o3(g2[cb][:]), in1=b12(gsm[cb][:, 60:72]), op=ALU.mult), reads=[r_g2[cb], r_gsm[cb]], writes=[r_g2[cb]]))
                        st_.append(lambda: P.add('dve', lambda e: e.tensor_tensor(out=g2[cb][:], in0=g2[cb][:], in1=gtb[cb][:], op=ALU.mult), reads=[r_g2[cb], r_ld[cb]], writes=[r_g2[cb]]))
                        st_.append(lambda: P.add('pool', lambda e: e.tensor_tensor(out=g3t[cb][:], in0=g2[cb][:, 0:384], in1=g2[cb][:, 384:768], op=ALU.add), reads=[r_g2[cb]], writes=[r_g3[cb]]))
                        st_.append(lambda: P.add('dve', lambda e: e.tensor_tensor(out=rettok[cb][:], in0=g3t[cb][:], in1=gnw[:], op=ALU.mult), reads=[r_g3[cb], r_sgt], writes=[r_rettok[cb]]))
                        return st_

                    def sec_tr():
                        for pc in range(3):
                            P.add('pe', lambda e, pc=pc: e.transpose(pT[:, pc * 128:(pc + 1) * 128], natok[cb][:, pc * 128:(pc + 1) * 128], ident_b), reads=[r_natok[cb], r_cstbf], writes=[r_pT])
                        for pc in range(3):
                            P.add('pe', lambda e, pc=pc: e.transpose(pT[:, (3 + pc) * 128:(4 + pc) * 128], rettok[cb][:, pc * 128:(pc + 1) * 128], ident_b), reads=[r_rettok[cb], r_cstbf], writes=[r_pT])
                        P.add('act', lambda e: e.copy(out=catst[cb][:, 0:3, :], in_=pT[:, 0:384].rearrange('p (c t) -> p c t', c=3)), reads=[r_pT], writes=[r_catst[cb]])
                        P.add('act', lambda e: e.copy(out=catst[cb][:, 5:8, :], in_=pT[:, 384:768].rearrange('p (c t) -> p c t', c=3)), reads=[r_pT], writes=[r_catst[cb]])
                        P.add('pool', lambda e: e.dma_start(out=catT.rearrange('(kc p) t -> p kc t', p=128)[:, :, tok:tok + 128], in_=catst[cb][:]), reads=[r_catst[cb]], writes=[DR(id(catT), tl)], dma=True)

                    return sec_na, sec_sg, sec_ret, gn_steps, sec_tr

                chunks = list(range(NLC)) + ([] if last else [NLC, NLC + 1])
                pend_gn = []
                pend_tr = None
                for idx, c in enumerate(chunks):
                    na_, sg_, ret_, gn_, tr_ = do_chunk(c, idx)
                    na_(pend_gn)
                    sg_()
                    ret_()
                    if pend_tr is not None:
                        pend_tr()
                    pend_gn, pend_tr = gn_(), tr_
                while pend_gn:
                    pend_gn.pop(0)()
                pend_tr()
                P.emit_pass()

        for l_ in range(n_layers):
            conv_jobs.append([('w1_%d_0' % l_, w1_in[l_, 0]), ('w2_%d_0' % l_, w2_in[l_, 0])])
            conv_jobs.append([('win_%d' % l_, win_in[l_])])
            conv_jobs.append([('wo_%d' % l_, wout_in[l_])])
            conv_jobs.append([('w1_%d_1' % l_, w1_in[l_, 1]), ('w2_%d_1' % l_, w2_in[l_, 1])])
        pass_xpose_in(xTa)
        cur, oth = xTa, xTb
        for l in range(n_layers):
            last = (l == n_layers - 1)
            pass_mod(l)
            pass_ffn(l, 0, cur, oth)
            cur, oth = oth, cur
            if stop_after == 'ffn1':
                break
            pass_inproj(l, cur)
            if stop_after == 'inproj':
                break
            pass_mixer(l, last)
            if stop_after == 'mixer':
                break
            pass_outproj(l, cur, oth, skip_ctx=last)
            cur, oth = oth, cur
            if stop_after == 'outproj':
                break
            pass_ffn(l, 1, cur, oth, skip_ctx=last)
            cur, oth = oth, cur
        pass_xpose_out(cur)
    return nc


def prep_inputs(NT, b, inp, shared):
    m = dict(shared)
    m['x'] = np.ascontiguousarray(inp['x'][b])
    m['ctx'] = np.ascontiguousarray(inp['ctx'][b])
    cc = np.stack([inp['c'][b], inp['c_ctx']], axis=0)
    m['cT'] = np.ascontiguousarray(cc.reshape(2, KC, 128).transpose(2, 1, 0).reshape(128, 16))
    return m


def prep_shared(NT, inp):
    f = lambda a: np.ascontiguousarray(np.asarray(a, dtype=np.float32))
    NLC = NT // 128
    var_of, base_of, ridx, cidx, valid = na_tables(NLC)
    sh = {}
    sh['ada_w'] = f(inp['ada_w'])
    sh['ada_bT'] = f(inp['ada_b'].reshape(2, 72, 128).transpose(0, 2, 1))
    sh['norm_wT'] = f(inp['norm_w'].reshape(2, 3, KC, 128).transpose(3, 0, 1, 2).reshape(128, 48))
    sh['ffn_w1'] = f(inp['ffn_w1'])
    sh['ffn_w2'] = f(inp['ffn_w2'])
    w_in = np.asarray(inp['mix_w_in'], dtype=np.float32)
    perm = np.arange(384).reshape(6, 64)
    part = perm.copy()
    for h in range(6):
        for d in range(64):
            part[h, d] = h * 64 + (d + 16 if (d % 32) < 16 else d - 16)
    part = part.reshape(-1)
    sh['w_in_ext'] = f(np.concatenate([w_in, w_in[:, :, 1664 + part], w_in[:, :, 2048 + part]], axis=2))
    sh['w_out'] = f(inp['mix_w_out'])
    qk = np.zeros((128, 4), np.float32)
    for l in range(2):
        qk[:, 2 * l] = np.tile(inp['na_q_norm'][l], 2)
        qk[:, 2 * l + 1] = np.tile(inp['na_k_norm'][l], 2)
    sh['qknT'] = qk
    rpb = np.asarray(inp['na_rpb'], dtype=np.float32)
    g = rpb[:, :, ridx, cidx]
    g = np.where(valid[None, None], g, np.float32(-1e30)).astype(np.float32)
    sh['na_biasT'] = f(g.transpose(0, 2, 1, 4, 3, 5))
    sh['sg_wT'] = f(np.asarray(inp['sg_w']).transpose(0, 1, 3, 2))
    sgb = np.asarray(inp['sg_b'], dtype=np.float32)
    sh['sgb_rep'] = f(np.repeat(sgb.reshape(2, 2, 2, 1, 128), 64, axis=3).reshape(2, 2, 128, 128))
    sh['sg_lnw_rep'] = f(np.broadcast_to(np.asarray(inp['sg_ln_w']).reshape(1, 512), (128, 512)))
    sh['sg_lnb_rep'] = f(np.broadcast_to(np.asarray(inp['sg_ln_b']).reshape(1, 512), (128, 512)))
    sh['dlog_rep'] = f(np.broadcast_to(np.asarray(inp['ret_decay_logit']).reshape(1, 24), (128, 24)))
    sh['gnw_rep'] = f(np.broadcast_to(np.asarray(inp['ret_gn_w']).reshape(1, 768), (128, 768)))
    C, S = rope_tables(NT)
    sh['ropeC'] = C
    sh['ropeS'] = S
    sh['consts'] = const_table()
    return sh


_NC_CACHE = {}


def kernel(**inputs):
    inp = {k: np.asarray(v) for k, v in inputs.items()}
    B, NT, _ = inp['x'].shape
    if NT not in _NC_CACHE:
        _NC_CACHE[NT] = build(NT)
    nc = _NC_CACHE[NT]
    shared = prep_shared(NT, inp)
    in_maps = [prep_inputs(NT, b, inp, shared) for b in range(B)]
    res = run_bass_kernel_spmd(nc, in_maps, core_ids=list(range(B)))
    return np.stack([np.asarray(r['y']) for r in res.results], axis=0).astype(np.float32)
```

```python
import math
from contextlib import ExitStack

import numpy as np
import concourse.bass as bass
import concourse.mybir as mybir
from concourse.bass_utils import run_bass_kernel_spmd

F32 = mybir.dt.float32
BF16 = mybir.dt.bfloat16
AF = mybir.ActivationFunctionType
ALU = mybir.AluOpType
AX = mybir.AxisListType

ENGS = ['pe', 'act', 'dve', 'pool', 'sp']

D = 1024
KC = 8
DFF = 2816
FC = 22
NCTX = 256
GW = 64
NMOD = 9
INX = 4352
RMS_EPS = 1e-6
LN_EPS = 1e-5
LN8 = math.log(0.125)
import os
MIXDBG = os.environ.get('MIXDBG', 'scan,na,sg,ret,tr').split(',')


class Res:
    __slots__ = ('name', 'last_w', 'readers')

    def __init__(self, name=''):
        self.name = name
        self.last_w = None
        self.readers = []


class Op:
    __slots__ = ('eng', 'fn', 'deps', 'signal', 'seq', 'is_dma', 'dsem', 'dval', 'dprev')


class Prog:
    def __init__(self, nc, es, n_dma_sems=24):
        self.nc = nc
        self.n_dma_sems = n_dma_sems
        self.csem = {e: es.enter_context(nc.semaphore('c_' + e)) for e in ENGS}
        self.nsem = {'sp': 16, 'pool': 6}
        self.dsem = {e: [es.enter_context(nc.semaphore('d_%s_%d' % (e, i))) for i in range(self.nsem[e])]
                     for e in ('sp', 'pool')}
        self.dma_cnt = {e: 0 for e in ENGS}
        self.dma_use = {e: [0] * n_dma_sems for e in ENGS}
        self.seqc = {e: 0 for e in ENGS}
        self.ops = {e: [] for e in ENGS}
        self.waited = {e: {} for e in ENGS}

    def add(self, eng, fn, reads=(), writes=(), dma=False):
        op = Op()
        op.eng = eng
        op.fn = fn
        op.signal = False
        op.seq = 0
        op.is_dma = dma
        op.dsem = None
        op.dval = 0
        op.dprev = 0
        deps = []
        seen = set()

        def push(d):
            if d is None or id(d) in seen:
                return
            seen.add(id(d))
            if d.eng == 'pe' and eng == 'pe' and not d.is_dma and not dma:
                return
            deps.append(d)

        for r in reads:
            push(r.last_w)
        for w in writes:
            push(w.last_w)
            for rd in w.readers:
                push(rd)
        op.deps = deps
        for d in deps:
            d.signal = True
        for r in reads:
            r.readers.append(op)
        for w in writes:
            w.last_w = op
            w.readers = []
        if dma:
            assert eng in ('sp', 'pool')
            k = self.dma_cnt[eng] % self.nsem[eng]
            self.dma_cnt[eng] += 1
            op.dsem = k
            op.dprev = 16 * self.dma_use[eng][k]
            self.dma_use[eng][k] += 1
            op.dval = 16 * self.dma_use[eng][k]
        self.ops[eng].append(op)
        return op

    def emit_pass(self):
        nc = self.nc
        for e in ENGS:
            last = None
            for op in self.ops[e]:
                if not op.is_dma:
                    last = op
            if last is not None:
                last.signal = True
            for op in self.ops[e]:
                if op.signal and not op.is_dma:
                    self.seqc[e] += 1
                    op.seq = self.seqc[e]
        final_c = dict(self.seqc)
        final_d = {e: [16 * u for u in self.dma_use[e]] for e in ('sp', 'pool')}
        with nc.Block() as block:
            regs = {'pe': block.tensor, 'act': block.scalar, 'dve': block.vector,
                    'pool': block.gpsimd, 'sp': block.sync}
            for e in ENGS:
                ops = self.ops[e]

                def body(eng, e=e, ops=ops):
                    waited = self.waited[e]
                    for op in ops:
                        for d in op.deps:
                            if d.is_dma:
                                key = ('d', d.eng, d.dsem)
                                sem = self.dsem[d.eng][d.dsem]
                                val = d.dval
                            else:
                                key = ('c', d.eng)
                                sem = self.csem[d.eng]
                                val = d.seq
                            if waited.get(key, 0) >= val:
                                continue
                            waited[key] = val
                            eng.wait_ge(sem, val)
                        if op.is_dma:
                            key = ('d', e, op.dsem)
                            if op.dprev > 0 and waited.get(key, 0) < op.dprev:
                                waited[key] = op.dprev
                                eng.wait_ge(self.dsem[e][op.dsem], op.dprev)
                            ins = op.fn(eng)
                            ins.then_inc(self.dsem[e][op.dsem], 16)
                        else:
                            ins = op.fn(eng)
                            if op.signal:
                                ins.then_inc(self.csem[e], 1)
                    for e2 in ENGS:
                        if e2 != e and final_c[e2] > waited.get(('c', e2), 0):
                            waited[('c', e2)] = final_c[e2]
                            eng.wait_ge(self.csem[e2], final_c[e2])
                    for q in ('sp', 'pool'):
                        for k in range(self.nsem[q]):
                            if final_d[q][k] > waited.get(('d', q, k), 0):
                                waited[('d', q, k)] = final_d[q][k]
                                eng.wait_ge(self.dsem[q][k], final_d[q][k])

                regs[e](body)
        self.ops = {e: [] for e in ENGS}


def na_tables(nchunks):
    rows = 2 * nchunks
    kr = min(8, rows)
    sigs = {}
    var_of = []
    base_of = []
    tabs = []
    ir = np.arange(128) // 64
    cc = np.arange(128) % 64
    for j in range(nchunks):
        base = int(np.clip(j - 2, 0, nchunks - 5))
        qrow = 2 * j + ir[None, :]
        qcol = cc[None, :]
        rstart = np.clip(qrow - kr // 2, 0, rows - kr)
        cstart = np.clip(qcol - 8, 0, GW - 16)
        rid = np.zeros((5, 128, 128), np.int64)
        cid = np.zeros((5, 128, 128), np.int64)
        val = np.zeros((5, 128, 128), bool)
        for m in range(5):
            krow = 2 * (base + m) + ir[:, None]
            kcol = cc[:, None]
            v = (krow >= rstart) & (krow < rstart + kr) & (kcol >= cstart) & (kcol < cstart + 16)
            rid[m] = np.clip(krow - qrow + 7, 0, 14)
            cid[m] = np.clip(kcol - qcol + 15, 0, 30)
            val[m] = v
        sig = (base - j, val.tobytes())
        if sig not in sigs:
            sigs[sig] = len(tabs)
            tabs.append((rid, cid, val))
        var_of.append(sigs[sig])
        base_of.append(base)
    ridx = np.stack([t[0] for t in tabs])
    cidx = np.stack([t[1] for t in tabs])
    valid = np.stack([t[2] for t in tabs])
    return var_of, base_of, ridx, cidx, valid


def rope_tables(NT):
    NTOK = NT + NCTX
    t = np.arange(NT)
    inv = (10000.0 ** (-np.arange(16, dtype=np.float32) / 16)).astype(np.float32)
    ang_r = (t // GW).astype(np.float32)[:, None] * inv[None, :]
    ang_c = (t % GW).astype(np.float32)[:, None] * inv[None, :]
    C = np.ones((128, NTOK), np.float32)
    S = np.zeros((128, NTOK), np.float32)
    for p in range(128):
        d = p % 64
        ang = ang_r if d < 32 else ang_c
        f = d % 16
        first = (d % 32) < 16
        C[p, :NT] = np.cos(ang[:, f])
        S[p, :NT] = (-np.sin(ang[:, f])) if first else np.sin(ang[:, f])
    return C, S


def const_table():
    i = np.arange(128, dtype=np.float32)
    diffT = i[None, :] - i[:, None]
    mF = (diffT >= 0).astype(np.float32)
    mB = (diffT <= 0).astype(np.float32)
    ip1 = np.broadcast_to(i[None, :] + 1, (128, 128))
    rev = np.broadcast_to(128 - i[None, :], (128, 128))
    ident = np.eye(128, dtype=np.float32)
    bd = np.zeros((128, 128), np.float32)
    bd[:64, :64] = 1.0 / 64
    bd[64:, 64:] = 1.0 / 64
    pcol = i[:, None]
    prev = 127 - i[:, None]
    return np.ascontiguousarray(np.concatenate([diffT, mF, mB, ip1, rev, ident, bd, pcol, prev], axis=1).astype(np.float32))


C_DIFF, C_MF, C_MB, C_IP1, C_REV, C_ID, C_BD, C_PCOL, C_PREV = [k * 128 for k in range(7)] + [896, 897]
NCONST = 898


def build(NT, n_layers=2, stop_after=None, dbg=False):
    nc = bass.Bass('TRN2', target_bir_lowering=False)
    _uid = [0]

    def _sbuf_tensor(name, shape, dt):
        _uid[0] += 1
        return _orig_sb('%s_u%d' % (name, _uid[0]), shape, dt)

    def _psum_tensor(name, shape, dt):
        _uid[0] += 1
        return _orig_ps('%s_u%d' % (name, _uid[0]), shape, dt)

    _orig_sb = nc.sbuf_tensor
    _orig_ps = nc.psum_tensor
    NTOK = NT + NCTX
    NLC = NT // 128
    NCH = NLC + 2
    tiles = [(i * 512, 512, 0) for i in range(NT // 512)] + [(NT, 256, 1)]
    var_of, base_of, ridx, cidx, valid = na_tables(NLC)
    NV = ridx.shape[0]

    def din(name, shape, dt=F32):
        return nc.dram_tensor(name, list(shape), dt, kind='ExternalInput').ap()

    x_in = din('x', [NT, D])
    ctx_in = din('ctx', [NCTX, D])
    cT_in = din('cT', [128, 16])
    ada_w = din('ada_w', [2, D, NMOD * D])
    ada_bT = din('ada_bT', [2, 128, 72])
    norm_wT = din('norm_wT', [128, 48])
    w1_in = din('ffn_w1', [2, 2, D, 2 * DFF])
    w2_in = din('ffn_w2', [2, 2, DFF, D])
    win_in = din('w_in_ext', [2, D, INX])
    wout_in = din('w_out', [2, D, D])
    qkn_in = din('qknT', [128, 4])
    bias_in = din('na_biasT', [2, NV, 6, 128, 5, 128])
    sgw_in = din('sg_wT', [2, 4, 128, 128])
    sgb_in = din('sgb_rep', [2, 2, 128, 128])
    lnw_in = din('sg_lnw_rep', [128, 512])
    lnb_in = din('sg_lnb_rep', [128, 512])
    dlog_in = din('dlog_rep', [128, 24])
    gnw_in = din('gnw_rep', [128, 768])
    ropeC = din('ropeC', [128, NTOK])
    ropeS = din('ropeS', [128, NTOK])
    const_in = din('consts', [128, NCONST])
    y_out = nc.dram_tensor('y', [NT, D], F32, kind='ExternalOutput').ap()

    dbg_kind = 'ExternalOutput' if dbg else 'Internal'

    def dscr(name, shape, dt):
        return nc.dram_tensor(name, list(shape), dt, kind=dbg_kind).ap()

    xTa = dscr('xTa', [D, NTOK], F32)
    xTb = dscr('xTb', [D, NTOK], F32)
    qT_na = dscr('qT_na', [384, NTOK], BF16)
    kT_na = dscr('kT_na', [384, NTOK], BF16)
    v_na = dscr('v_na', [NTOK, 384], BF16)
    uT_sg = dscr('uT_sg', [256, NTOK], BF16)
    vn_sg = dscr('vn_sg', [NTOK, 256], BF16)
    rqT = dscr('rqT', [384, NTOK], BF16)
    rkT = dscr('rkT', [384, NTOK], BF16)
    rv_d = dscr('rv', [NTOK, 384], BF16)
    gates_d = dscr('gates', [NTOK, 768], BF16)
    catT = dscr('catT', [D, NTOK], BF16)

    dres = {}

    def DR(name, tile):
        k = (name, tile)
        if k not in dres:
            dres[k] = Res('%s_%s' % (name, tile))
        return dres[k]

    def tile_of_chunk(c):
        return c // 4 if c < NLC else NT // 512

    def chunk_tok(c):
        return c * 128

    with ExitStack() as ges:
        P = Prog(nc, ges)
        gsb = lambda name, shape, dt=F32: ges.enter_context(_sbuf_tensor(name, list(shape), dt))
        cst = gsb('cst', [128, NCONST])
        cst_bf = gsb('cst_bf', [128, 3 * 128], BF16)
        modT = gsb('modT', [128, 72, 2])
        mtab = gsb('mtab', [128, 9, 8, 2])
        r_cst = Res('cst')
        r_cstbf = Res('cst_bf')
        r_mtab = Res('mtab')
        lnb8 = gsb('lnb8', [128, 1])
        lnbe = gsb('lnbe', [128, 1])
        ident_f = cst[:, C_ID:C_ID + 128]
        ident_b = cst_bf[:, 0:128]
        bd_b = cst_bf[:, 128:256]
        ones_b = cst_bf[:, 256:384]

        P.add('sp', lambda e: e.dma_start(out=cst[:], in_=const_in), writes=[r_cst], dma=True)
        P.add('dve', lambda e: e.tensor_copy(out=cst_bf[:, 0:256], in_=cst[:, C_ID:C_ID + 256]), reads=[r_cst], writes=[r_cstbf])
        P.add('dve', lambda e: e.memset(cst_bf[:, 256:384], 1.0 / 1024), reads=[], writes=[r_cstbf])
        P.add('dve', lambda e: e.memset(lnb8[:], LN8), reads=[], writes=[r_cstbf])
        P.add('dve', lambda e: e.memset(lnbe[:], LN_EPS), reads=[], writes=[r_cstbf])
        P.emit_pass()

        def pass_xpose_in(dst):
            with ExitStack() as es:
                conv_issue()
                stg_in = [es.enter_context(_sbuf_tensor('xi%d' % i, [128, D], F32)) for i in range(3)]
                r_in = [Res() for _ in range(3)]
                stg = [es.enter_context(_sbuf_tensor('xo%d' % i, [128, KC, 512], F32)) for i in range(2)]
                r_stg = [Res() for _ in range(2)]
                pss = [es.enter_context(_psum_tensor('pxp%d' % i, [128, 512], F32)) for i in range(4)]
                r_ps = [Res() for _ in range(4)]
                ci = 0
                pi = 0
                for ti, (t0, T, j) in enumerate(tiles):
                    so = stg[ti % 2]
                    rso = r_stg[ti % 2]
                    for s in range(T // 128):
                        tok = t0 + s * 128
                        src = x_in[tok:tok + 128, :] if j == 0 else ctx_in[tok - NT:tok - NT + 128, :]
                        b = ci % 3
                        ci += 1
                        P.add('sp', lambda e, b=b, src=src: e.dma_start(out=stg_in[b][:], in_=src), writes=[r_in[b]], dma=True)
                        for half in range(2):
                            pb = pi % 4
                            pi += 1
                            for q in range(4):
                                kc = half * 4 + q
                                P.add('pe', lambda e, pb=pb, q=q, b=b, kc=kc: e.transpose(pss[pb][:, q * 128:(q + 1) * 128], stg_in[b][:, kc * 128:(kc + 1) * 128], ident_f),
                                      reads=[r_in[b], r_cst], writes=[r_ps[pb]])
                            eng = 'act' if half == 0 else 'dve'
                            dstv = so[:, half * 4:half * 4 + 4, s * 128:(s + 1) * 128]
                            srcv = pss[pb][:].rearrange('p (q t) -> p q t', q=4)
                            if eng == 'act':
                                P.add('act', lambda e, dstv=dstv, srcv=srcv: e.copy(out=dstv, in_=srcv), reads=[r_ps[pb]], writes=[rso])
                            else:
                                P.add('dve', lambda e, dstv=dstv, srcv=srcv: e.tensor_copy(out=dstv, in_=srcv), reads=[r_ps[pb]], writes=[rso])
                    dv = dst.rearrange('(kc p) t -> p kc t', p=128)[:, :, t0:t0 + T]
                    P.add('pool', lambda e, so=so, dv=dv, T=T: e.dma_start(out=dv, in_=so[:, :, 0:T]), reads=[rso], writes=[DR(id(dst), ti)], dma=True)
                P.emit_pass()

        def pass_xpose_out(src):
            with ExitStack() as es:
                stg_in = [es.enter_context(_sbuf_tensor('yi%d' % i, [128, KC, 512], F32)) for i in range(2)]
                r_in = [Res() for _ in range(2)]
                stg = [es.enter_context(_sbuf_tensor('yo%d' % i, [128, D], F32)) for i in range(3)]
                r_stg = [Res() for _ in range(3)]
                pss = [es.enter_context(_psum_tensor('pyp%d' % i, [128, 512], F32)) for i in range(4)]
                r_ps = [Res() for _ in range(4)]
                ci = 0
                pi = 0
                for ti, (t0, T, j) in enumerate(tiles):
                    if j == 1:
                        continue
                    si = stg_in[ti % 2]
                    rsi = r_in[ti % 2]
                    sv = src.rearrange('(kc p) t -> p kc t', p=128)[:, :, t0:t0 + T]
                    P.add('sp', lambda e, si=si, sv=sv: e.dma_start(out=si[:], in_=sv), reads=[DR(id(src), ti)], writes=[rsi], dma=True)
                    for s in range(T // 128):
                        b = ci % 3
                        ci += 1
                        for half in range(2):
                            pb = pi % 4
                            pi += 1
                            for q in range(4):
                                kc = half * 4 + q
                                P.add('pe', lambda e, pb=pb, q=q, si=si, kc=kc, s=s: e.transpose(pss[pb][:, q * 128:(q + 1) * 128], si[:, kc, s * 128:(s + 1) * 128], ident_f),
                                      reads=[rsi, r_cst], writes=[r_ps[pb]])
                            dstv = stg[b][:, half * 512:(half + 1) * 512]
                            if half == 0:
                                P.add('act', lambda e, dstv=dstv, pb=pb: e.copy(out=dstv, in_=pss[pb][:]), reads=[r_ps[pb]], writes=[r_stg[b]])
                            else:
                                P.add('dve', lambda e, dstv=dstv, pb=pb: e.tensor_copy(out=dstv, in_=pss[pb][:]), reads=[r_ps[pb]], writes=[r_stg[b]])
                        tok = t0 + s * 128
                        P.add('pool', lambda e, b=b, tok=tok: e.dma_start(out=y_out[tok:tok + 128, :], in_=stg[b][:]), reads=[r_stg[b]], writes=[DR('y', tok)], dma=True)
                P.emit_pass()

        def pass_mod(l):
            with ExitStack() as es:
                conv_issue()
                sb = lambda name, shape, dt=F32: es.enter_context(_sbuf_tensor(name, list(shape), dt))
                cT = sb('cT', [128, 16])
                sig = sb('csig', [128, 16])
                scT = sb('scT', [128, 16])
                abT = sb('abT', [128, 72])
                nwT = sb('nwT', [128, 48])
                wbuf = [sb('adaw%d' % i, [128, KC, 512]) for i in range(2)]
                r_w = [Res() for _ in range(2)]
                pm = es.enter_context(_psum_tensor('pmod', [128, 72, 2], F32))
                r_pm = Res()
                r_c = Res(); r_sc = Res(); r_ab = Res(); r_nw = Res(); r_mod = Res(); r_sig = Res()
                P.add('sp', lambda e: e.dma_start(out=cT[:], in_=cT_in), writes=[r_c], dma=True)
                P.add('sp', lambda e: e.dma_start(out=abT[:], in_=ada_bT[l]), writes=[r_ab], dma=True)
                P.add('sp', lambda e: e.dma_start(out=nwT[:], in_=norm_wT), writes=[r_nw], dma=True)
                P.add('act', lambda e: e.activation(out=sig[:], in_=cT[:], func=AF.Sigmoid), reads=[r_c], writes=[r_sig])
                P.add('dve', lambda e: e.tensor_tensor(out=scT[:], in0=cT[:], in1=sig[:], op=ALU.mult), reads=[r_c, r_sig], writes=[r_sc])
                wv = ada_w[l].rearrange('(kc p) n -> p kc n', p=128)
                for g in range(18):
                    b = g % 2
                    P.add('sp', lambda e, b=b, g=g: e.dma_start(out=wbuf[b][:], in_=wv[:, :, g * 512:(g + 1) * 512]), writes=[r_w[b]], dma=True)
                    for q in range(4):
                        oc = g * 4 + q
                        for kc in range(KC):
                            P.add('pe', lambda e, b=b, q=q, oc=oc, kc=kc: e.matmul(pm[:, oc, :], lhsT=wbuf[b][:, kc, q * 128:(q + 1) * 128], rhs=scT[:, 2 * kc:2 * kc + 2],
                                                                                  start=(kc == 0), stop=(kc == KC - 1)),
                                  reads=[r_w[b], r_sc], writes=[r_pm])
                P.add('dve', lambda e: e.tensor_tensor(out=modT[:], in0=pm[:], in1=abT[:].unsqueeze(2).broadcast_to([128, 72, 2]), op=ALU.add),
                      reads=[r_pm, r_ab], writes=[r_mod])
                for n in range(3):
                    sc = modT[:, (3 * n + 1) * 8:(3 * n + 2) * 8, :]
                    nw = nwT[:, (l * 3 + n) * 8:(l * 3 + n + 1) * 8].unsqueeze(2).broadcast_to([128, 8, 2])
                    P.add('dve', lambda e, n=n, sc=sc, nw=nw: e.scalar_tensor_tensor(out=mtab[:, n, :, :], in0=sc, scalar=1.0, in1=nw, op0=ALU.add, op1=ALU.mult),
                          reads=[r_mod, r_nw], writes=[r_mtab])
                    sh = modT[:, (3 * n) * 8:(3 * n + 1) * 8, :]
                    P.add('dve', lambda e, n=n, sh=sh: e.tensor_copy(out=mtab[:, 3 + n, :, :], in_=sh), reads=[r_mod], writes=[r_mtab])
                    gt = modT[:, (3 * n + 2) * 8:(3 * n + 3) * 8, :]
                    gm = 1.0 if n == 1 else 0.5
                    P.add('dve', lambda e, n=n, gt=gt, gm=gm: e.tensor_scalar(out=mtab[:, 6 + n, :, :], in0=gt, scalar1=gm, scalar2=None, op0=ALU.mult),
                          reads=[r_mod], writes=[r_mtab])
                P.emit_pass()

        def mt(kind, n, kc, j):
            return mtab[:, kind * 3 + n, kc, j:j + 1]

        wb_cache = {}

        def wb_tensor(key, src):
            if key not in wb_cache:
                rows, cols = src.shape
                wb_cache[key] = (dscr('wb_' + key, [rows, cols], BF16), src, Res('wb_' + key))
            return wb_cache[key]

        conv_jobs = []

        def conv_issue():
            if not conv_jobs:
                return
            for key, src in conv_jobs.pop(0):
                dstt, _, res = wb_tensor(key, src)
                rows, cols = src.shape
                r0 = 0
                while r0 < rows:
                    r1 = min(rows, r0 + 256)
                    c0 = 0
                    while c0 < cols:
                        c1 = min(cols, c0 + 2048)
                        P.add('pool', lambda e, r0=r0, r1=r1, c0=c0, c1=c1, dstt=dstt, src=src: e.dma_start(out=dstt[r0:r1, c0:c1], in_=src[r0:r1, c0:c1]),
                              writes=[res], dma=True)
                        c0 = c1
                    r0 = r1

        def load_w(dst, key, src, rows_chunks, ncols, res_list):
            dstt, _, res = wb_tensor(key, src)
            assert res.last_w is not None, key
            sv = dstt.rearrange('(kc p) n -> p kc n', p=128)
            for kc in range(rows_chunks):
                P.add('sp', lambda e, kc=kc: e.dma_start(out=dst[:, kc, :], in_=sv[:, kc, :]), reads=[res], writes=[res_list[kc]], dma=True)

        class Prep:
            def __init__(self, es, tag):
                sb = lambda name, shape, dt=F32: es.enter_context(_sbuf_tensor(tag + name, list(shape), dt))
                self.hT = [sb('hT%d' % i, [128, KC, 512], BF16) for i in range(2)]
                self.r_hT = [Res() for _ in range(2)]
                self.sq = [sb('sq%d' % i, [128, 512], BF16) for i in range(2)]
                self.r_sq = [Res() for _ in range(2)]
                self.ln = sb('ln', [128, 512])
                self.r_ln = Res()
                self.rstd = sb('rstd', [128, 512])
                self.r_rstd = Res()
                self.tmp = [sb('tmp%d' % i, [128, 512]) for i in range(2)]
                self.r_tmp = [Res() for _ in range(2)]
                self.pst = es.enter_context(_psum_tensor(tag + 'pst', [128, 512], F32))
                self.r_pst = Res()
                self.cnt = 0

            def chunk_a(self, slot, kc, T, xap, xres):
                b = kc % 2
                sq = self.sq[b]
                P.add('act', lambda e: e.activation(out=sq[:, 0:T], in_=xap, func=AF.Square), reads=[xres], writes=[self.r_sq[b]])
                hT = self.hT[slot]
                P.add('pool', lambda e: e.tensor_copy(out=hT[:, kc, 0:T], in_=xap), reads=[xres], writes=[self.r_hT[slot]])

            def chunk_b(self, kc, T):
                b = kc % 2
                sq = self.sq[b]
                P.add('pe', lambda e: e.matmul(self.pst[:, 0:T], lhsT=ones_b, rhs=sq[:, 0:T], start=(kc == 0), stop=(kc == KC - 1)),
                      reads=[self.r_sq[b], r_cstbf], writes=[self.r_pst])

            def chunk_in(self, slot, kc, T, xap, xres):
                self.chunk_a(slot, kc, T, xap, xres)
                self.chunk_b(kc, T)

            def finish_a(self, T):
                P.add('act', lambda e: e.activation(out=self.ln[:, 0:T], in_=self.pst[:, 0:T], func=AF.Ln, bias=RMS_EPS), reads=[self.r_pst], writes=[self.r_ln])
                P.add('act', lambda e: e.activation(out=self.rstd[:, 0:T], in_=self.ln[:, 0:T], func=AF.Exp, scale=-0.5), reads=[self.r_ln], writes=[self.r_rstd])

            def finish_kc(self, slot, kc, T, n, j):
                hT = self.hT[slot]
                b = kc % 2
                tmp = self.tmp[b]
                P.add('dve', lambda e: e.tensor_tensor(out=tmp[:, 0:T], in0=hT[:, kc, 0:T], in1=self.rstd[:, 0:T], op=ALU.mult),
                      reads=[self.r_hT[slot], self.r_rstd], writes=[self.r_tmp[b]])
                P.add('dve', lambda e: e.tensor_scalar(out=hT[:, kc, 0:T], in0=tmp[:, 0:T], scalar1=mt(0, n, kc, j), scalar2=mt(1, n, kc, j), op0=ALU.mult, op1=ALU.add),
                      reads=[self.r_tmp[b], r_mtab], writes=[self.r_hT[slot]])

            def finish(self, slot, T, n, j):
                self.finish_a(T)
                for kc in range(KC):
                    self.finish_kc(slot, kc, T, n, j)

            def steps(self, slot, T, n, j, load_chunk):
                def A(kc):
                    xap, xres = load_chunk(kc)
                    self.chunk_a(slot, kc, T, xap, xres)

                st = [lambda: A(0)]
                for kc in range(1, KC):
                    st.append(lambda kc=kc: (A(kc), self.chunk_b(kc - 1, T)))
                st.append(lambda: self.chunk_b(KC - 1, T))
                st.append(lambda: self.finish_a(T))
                for kc in range(KC):
                    st.append(lambda kc=kc: self.finish_kc(slot, kc, T, n, j))
                return st

        def pass_ffn(l, which, src, dst, skip_ctx=False):
            n = 0 if which == 0 else 2
            with ExitStack() as es:
                sb = lambda name, shape, dt=F32: es.enter_context(_sbuf_tensor(name, list(shape), dt))
                w1 = sb('w1', [128, KC, 2 * DFF], BF16)
                r_w1 = [Res() for _ in range(KC)]
                w2 = sb('w2', [128, FC, D], BF16)
                r_w2 = [Res() for _ in range(FC)]
                act = sb('actb', [128, FC, 512], BF16)
                r_act = [Res() for _ in range(FC)]
                xin = [sb('xin%d' % i, [128, 512]) for i in range(3)]
                r_xin = [Res() for _ in range(3)]
                xrs = [sb('xrs%d' % i, [128, 512]) for i in range(2)]
                r_xrs = [Res() for _ in range(2)]
                xo = [sb('xo%d' % i, [128, 512]) for i in range(2)]
                r_xo = [Res() for _ in range(2)]
                sg = [sb('sg%d' % i, [128, 512], BF16) for i in range(2)]
                r_sg = [Res() for _ in range(2)]
                prep = Prep(es, 'f')
                pg = [es.enter_context(_psum_tensor('pg%d' % i, [128, 512], F32)) for i in range(2)]
                pu = [es.enter_context(_psum_tensor('pu%d' % i, [128, 512], F32)) for i in range(2)]
                po = [es.enter_context(_psum_tensor('po%d' % i, [128, 512], F32)) for i in range(2)]
                r_pg = [Res() for _ in range(2)]
                r_pu = [Res() for _ in range(2)]
                r_po = [Res() for _ in range(2)]
                load_w(w1, 'w1_%d_%d' % (l, which), w1_in[l, which], KC, 2 * DFF, r_w1)
                load_w(w2, 'w2_%d_%d' % (l, which), w2_in[l, which], FC, D, r_w2)
                conv_issue()
                my_tiles = [(ti, t) for ti, t in enumerate(tiles) if not (skip_ctx and t[2] == 1)]
                srcv = src.rearrange('(kc p) t -> p kc t', p=128)
                dstv = dst.rearrange('(kc p) t -> p kc t', p=128)
                cnt = {'xin': 0, 'x2': 0}

                def prepare_steps(k):
                    ti, (t0, T, j) = my_tiles[k]
                    slot = k % 2

                    def load_chunk(kc):
                        b = cnt['xin'] % 3
                        cnt['xin'] += 1
                        P.add('sp', lambda e: e.dma_start(out=xin[b][:, 0:T], in_=srcv[:, kc, t0:t0 + T]), reads=[DR(id(src), ti)], writes=[r_xin[b]], dma=True)
                        return xin[b][:, 0:T], r_xin[b]

                    return prep.steps(slot, T, n, j, load_chunk)

                def prepare(k):
                    for st_ in prepare_steps(k):
                        st_()

                def do_tile(k):
                    ti, (t0, T, j) = my_tiles[k]
                    nsteps = prepare_steps(k + 1) if k + 1 < len(my_tiles) else []
                    slot = k % 2
                    hT = prep.hT[slot]
                    r_hT = prep.r_hT[slot]
                    for mo in range(FC):
                        pb = mo % 2
                        for kc in range(KC):
                            P.add('pe', lambda e, pb=pb, mo=mo, kc=kc: e.matmul(pg[pb][:, 0:T], lhsT=w1[:, kc, mo * 128:(mo + 1) * 128], rhs=hT[:, kc, 0:T],
                                                                               start=(kc == 0), stop=(kc == KC - 1)),
                                  reads=[r_w1[kc], r_hT], writes=[r_pg[pb]])
                        for kc in range(KC):
                            P.add('pe', lambda e, pb=pb, mo=mo, kc=kc: e.matmul(pu[pb][:, 0:T], lhsT=w1[:, kc, DFF + mo * 128:DFF + (mo + 1) * 128], rhs=hT[:, kc, 0:T],
                                                                               start=(kc == 0), stop=(kc == KC - 1)),
                                  reads=[r_w1[kc], r_hT], writes=[r_pu[pb]])
                        P.add('act', lambda e, pb=pb: e.activation(out=sg[pb][:, 0:T], in_=pg[pb][:, 0:T], func=AF.Silu), reads=[r_pg[pb]], writes=[r_sg[pb]])
                        P.add('dve', lambda e, pb=pb, mo=mo: e.tensor_tensor(out=act[:, mo, 0:T], in0=pu[pb][:, 0:T], in1=sg[pb][:, 0:T], op=ALU.mult),
                              reads=[r_pu[pb], r_sg[pb]], writes=[r_act[mo]])
                        if nsteps and mo >= 1:
                            nsteps.pop(0)()
                    while nsteps:
                        nsteps.pop(0)()
                    xsrc = src
                    xsv = xsrc.rearrange('(kc p) t -> p kc t', p=128)
                    def xrs_load(mo_):
                        pb_ = mo_ % 2
                        P.add('pool', lambda e: e.dma_start(out=xrs[pb_][:, 0:T], in_=xsv[:, mo_, t0:t0 + T]), reads=[DR(id(xsrc), ti)], writes=[r_xrs[pb_]], dma=True)

                    xrs_load(0)
                    xrs_load(1)
                    for mo in range(KC):
                        pb = mo % 2
                        for kc in range(FC):
                            P.add('pe', lambda e, pb=pb, mo=mo, kc=kc: e.matmul(po[pb][:, 0:T], lhsT=w2[:, kc, mo * 128:(mo + 1) * 128], rhs=act[:, kc, 0:T],
                                                                               start=(kc == 0), stop=(kc == FC - 1)),
                                  reads=[r_w2[kc], r_act[kc]], writes=[r_po[pb]])
                        P.add('dve', lambda e, pb=pb, mo=mo: e.scalar_tensor_tensor(out=xo[pb][:, 0:T], in0=po[pb][:, 0:T], scalar=mt(2, n, mo, j), in1=xrs[pb][:, 0:T],
                                                                                   op0=ALU.mult, op1=ALU.add),
                              reads=[r_po[pb], r_xrs[pb], r_mtab], writes=[r_xo[pb]])
                        P.add('pool', lambda e, pb=pb, mo=mo: e.dma_start(out=dstv[:, mo, t0:t0 + T], in_=xo[pb][:, 0:T]), reads=[r_xo[pb]], writes=[DR(id(dst), ti)], dma=True)
                        if mo + 2 < KC:
                            xrs_load(mo + 2)

                prepare(0)
                for k in range(len(my_tiles)):
                    do_tile(k)
                P.emit_pass()

        def pass_inproj(l, src):
            with ExitStack() as es:
                sb = lambda name, shape, dt=F32: es.enter_context(_sbuf_tensor(name, list(shape), dt))
                win = sb('win', [128, KC, INX], BF16)
                r_win = [Res() for _ in range(KC)]
                load_w(win, 'win_%d' % l, win_in[l], KC, INX, r_win)
                conv_issue()
                prep = Prep(es, 'i')
                xin = [sb('xin%d' % i, [128, 512]) for i in range(3)]
                r_xin = [Res() for _ in range(3)]
                rC = [sb('rC%d' % i, [128, 512]) for i in range(2)]
                rS = [sb('rS%d' % i, [128, 512]) for i in range(2)]
                r_rope = [Res() for _ in range(2)]
                qkw = sb('qkw', [128, 4])
                qkws = sb('qkws', [128, 2])
                r_qkw = Res()
                lnw = sb('lnw', [128, 256])
                lnb = sb('lnb', [128, 256])
                r_ln = Res()
                P.add('sp', lambda e: e.dma_start(out=qkw[:], in_=qkn_in), writes=[r_qkw], dma=True)
                P.add('dve', lambda e: e.tensor_scalar(out=qkws[:, 0:1], in0=qkw[:, 2 * l:2 * l + 1], scalar1=0.125, scalar2=None, op0=ALU.mult), reads=[r_qkw], writes=[r_qkw])
                P.add('dve', lambda e: e.tensor_copy(out=qkws[:, 1:2], in_=qkw[:, 2 * l + 1:2 * l + 2]), reads=[r_qkw], writes=[r_qkw])
                P.add('sp', lambda e: e.dma_start(out=lnw[:], in_=lnw_in[:, l * 256:(l + 1) * 256]), writes=[r_ln], dma=True)
                P.add('sp', lambda e: e.dma_start(out=lnb[:], in_=lnb_in[:, l * 256:(l + 1) * 256]), writes=[r_ln], dma=True)
                st_q = [sb('stq%d' % i, [128, 3, 512], BF16) for i in range(2)]
                st_k = [sb('stk%d' % i, [128, 3, 512], BF16) for i in range(2)]
                st_u = [sb('stu%d' % i, [128, 2, 512], BF16) for i in range(2)]
                st_rq = [sb('strq%d' % i, [128, 3, 512], BF16) for i in range(2)]
                st_rk = [sb('strk%d' % i, [128, 3, 512], BF16) for i in range(2)]
                r_stq = [Res() for _ in range(2)]; r_stk = [Res() for _ in range(2)]; r_stu = [Res() for _ in range(2)]
                r_strq = [Res() for _ in range(2)]; r_strk = [Res() for _ in range(2)]
                st_v = [sb('stv%d' % i, [128, 4, 384], BF16) for i in range(2)]
                st_rv = [sb('strv%d' % i, [128, 4, 384], BF16) for i in range(2)]
                st_g = [sb('stg%d' % i, [128, 4, 768], BF16) for i in range(2)]
                st_vn = [sb('stvn%d' % i, [128, 4, 256], BF16) for i in range(2)]
                r_stv = [[Res() for _ in range(4)] for _ in range(2)]; r_strv = [[Res() for _ in range(4)] for _ in range(2)]; r_stg = [[Res() for _ in range(4)] for _ in range(2)]; r_stvn = [[Res() for _ in range(4)] for _ in range(2)]
                sqb = [sb('qsq%d' % i, [128, 512], BF16) for i in range(2)]
                r_sqb = [Res() for _ in range(2)]
                lnq = [sb('lnq%d' % i, [128, 512]) for i in range(2)]
                r_lnq = [Res() for _ in range(2)]
                rsq = [sb('rsq%d' % i, [128, 512]) for i in range(2)]
                r_rsq = [Res() for _ in range(2)]
                t1 = [sb('t1%d' % i, [128, 512]) for i in range(2)]
                t2 = [sb('t2%d' % i, [128, 512]) for i in range(2)]
                r_t1 = [Res() for _ in range(2)]; r_t2 = [Res() for _ in range(2)]
                gv = [sb('gv%d' % i, [128, 256]) for i in range(2)]
                r_gv = [Res() for _ in range(2)]
                gsq = sb('gsq', [128, 256]); r_gsq = Res()
                cen = sb('cen', [128, 256]); r_cen = Res()
                sm = sb('sm', [128, 24]); r_sm = Res()
                pf = [es.enter_context(_psum_tensor('pf%d' % i, [128, 512], F32)) for i in range(4)]
                r_pf = [Res() for _ in range(4)]
                pt = [es.enter_context(_psum_tensor('pt%d' % i, [128, 512], F32)) for i in range(3)]
                r_pt = [Res() for _ in range(3)]
                srcv = src.rearrange('(kc p) t -> p kc t', p=128)
                cnt = {'xin': 0, 'pf': 0, 'pt': 0, 'w': 0}

                def prepare_steps(k):
                    ti, (t0, T, j) = k, tiles[k]
                    slot = k % 2

                    def load_chunk(kc):
                        b = cnt['xin'] % 3
                        cnt['xin'] += 1
                        P.add('sp', lambda e: e.dma_start(out=xin[b][:, 0:T], in_=srcv[:, kc, t0:t0 + T]), reads=[DR(id(src), ti)], writes=[r_xin[b]], dma=True)
                        return xin[b][:, 0:T], r_xin[b]

                    return prep.steps(slot, T, 1, j, load_chunk)

                def prepare(k):
                    for st_ in prepare_steps(k):
                        st_()

                nsteps = []

                def pop_step():
                    if nsteps:
                        nsteps.pop(0)()

                def fm_mm(colbase, c, T, hT, r_hT):
                    pb = cnt['pf'] % 4
                    cnt['pf'] += 1
                    c0 = colbase + c * 128
                    for kc in range(KC):
                        P.add('pe', lambda e, kc=kc: e.matmul(pf[pb][:, 0:T], lhsT=win[:, kc, c0:c0 + 128], rhs=hT[:, kc, 0:T], start=(kc == 0), stop=(kc == KC - 1)),
                              reads=[r_win[kc], r_hT], writes=[r_pf[pb]])
                    pop_step()
                    return pb

                def do_tile(k):
                    ti, (t0, T, j) = k, tiles[k]
                    slot = k % 2
                    hT = prep.hT[slot]
                    r_hT = prep.r_hT[slot]
                    sl = k % 2
                    P.add('sp', lambda e: e.dma_start(out=rC[sl][:, 0:T], in_=ropeC[:, t0:t0 + T]), writes=[r_rope[sl]], dma=True)
                    P.add('sp', lambda e: e.dma_start(out=rS[sl][:, 0:T], in_=ropeS[:, t0:t0 + T]), writes=[r_rope[sl]], dma=True)
                    def qk_a(colbase, c):
                        A = fm_mm(colbase, c, T, hT, r_hT)
                        w = cnt['w'] % 2
                        cnt['w'] += 1
                        P.add('act', lambda e: e.activation(out=sqb[w][:, 0:T], in_=pf[A][:, 0:T], func=AF.Square), reads=[r_pf[A]], writes=[r_sqb[w]])
                        return A, w

                    def qk_b(c, stg, r_stg_, wi, A, w):
                        B = cnt['pf'] % 4
                        cnt['pf'] += 1
                        P.add('pe', lambda e: e.matmul(pf[B][:, 0:T], lhsT=bd_b, rhs=sqb[w][:, 0:T], start=True, stop=True), reads=[r_sqb[w], r_cstbf], writes=[r_pf[B]])
                        P.add('act', lambda e: e.activation(out=lnq[w][:, 0:T], in_=pf[B][:, 0:T], func=AF.Ln, bias=RMS_EPS), reads=[r_pf[B]], writes=[r_lnq[w]])
                        P.add('act', lambda e: e.activation(out=rsq[w][:, 0:T], in_=lnq[w][:, 0:T], func=AF.Exp, scale=-0.5), reads=[r_lnq[w]], writes=[r_rsq[w]])
                        P.add('dve', lambda e: e.scalar_tensor_tensor(out=stg[:, c, 0:T], in0=pf[A][:, 0:T], scalar=qkws[:, wi:wi + 1], in1=rsq[w][:, 0:T], op0=ALU.mult, op1=ALU.mult),
                              reads=[r_pf[A], r_rsq[w], r_qkw], writes=[r_stg_])

                    items = [(0, c, st_q[sl], r_stq[sl], 0) for c in range(3)] + [(384, c, st_k[sl], r_stk[sl], 1) for c in range(3)]
                    prev = None
                    for (colbase, c, stg, r_stg_, wi) in items:
                        A, w = qk_a(colbase, c)
                        if prev is not None:
                            qk_b(*prev)
                        prev = (c, stg, r_stg_, wi, A, w)
                    qk_b(*prev)
                    def fm_store(stg, r_stg_, dten):
                        dv = dten.rearrange('(c p) t -> p c t', p=128)[:, :, t0:t0 + T]
                        P.add('pool', lambda e: e.dma_start(out=dv, in_=stg[:, :, 0:T]), reads=[r_stg_], writes=[DR(id(dten), ti)], dma=True)

                    fm_store(st_q[sl], r_stq[sl], qT_na)
                    fm_store(st_k[sl], r_stk[sl], kT_na)
                    for c in range(2):
                        A = fm_mm(1152, c, T, hT, r_hT)
                        P.add('act', lambda e, A=A, c=c: e.activation(out=st_u[sl][:, c, 0:T], in_=pf[A][:, 0:T], func=AF.Gelu_apprx_tanh), reads=[r_pf[A]], writes=[r_stu[sl]])
                    fm_store(st_u[sl], r_stu[sl], uT_sg)
                    for (colbase, pbase, stg, r_stg_) in ((1664, 3584, st_rq[sl], r_strq[sl]), (2048, 3968, st_rk[sl], r_strk[sl])):
                        for c in range(3):
                            A = fm_mm(colbase, c, T, hT, r_hT)
                            A2 = fm_mm(pbase, c, T, hT, r_hT)
                            w = cnt['w'] % 2
                            cnt['w'] += 1
                            P.add('dve', lambda e, A=A, w=w: e.tensor_tensor(out=t1[w][:, 0:T], in0=pf[A][:, 0:T], in1=rC[sl][:, 0:T], op=ALU.mult), reads=[r_pf[A], r_rope[sl]], writes=[r_t1[w]])
                            P.add('dve', lambda e, A2=A2, w=w: e.tensor_tensor(out=t2[w][:, 0:T], in0=pf[A2][:, 0:T], in1=rS[sl][:, 0:T], op=ALU.mult), reads=[r_pf[A2], r_rope[sl]], writes=[r_t2[w]])
                            P.add('pool', lambda e, w=w, c=c, stg=stg: e.tensor_tensor(out=stg[:, c, 0:T], in0=t1[w][:, 0:T], in1=t2[w][:, 0:T], op=ALU.add), reads=[r_t1[w], r_t2[w]], writes=[r_stg_])
                        fm_store(stg, r_stg_, rqT if colbase == 1664 else rkT)
                    def do_sub(s):
                        def tm_mm(c0, ncol):
                            pb = cnt['pt'] % 3
                            cnt['pt'] += 1
                            for kc in range(KC):
                                P.add('pe', lambda e, kc=kc: e.matmul(pt[pb][:, 0:ncol], lhsT=hT[:, kc, s * 128:(s + 1) * 128], rhs=win[:, kc, c0:c0 + ncol], start=(kc == 0), stop=(kc == KC - 1)),
                                      reads=[r_win[kc], r_hT], writes=[r_pt[pb]])
                            return pb
                        A = tm_mm(768, 384)
                        P.add('act', lambda e, A=A: e.copy(out=st_v[sl][:, s, :], in_=pt[A][:, 0:384]), reads=[r_pt[A]], writes=[r_stv[sl][s]])
                        A = tm_mm(2432, 384)
                        P.add('dve', lambda e, A=A: e.tensor_copy(out=st_rv[sl][:, s, :], in_=pt[A][:, 0:384]), reads=[r_pt[A]], writes=[r_strv[sl][s]])
                        A = tm_mm(2816, 384)
                        P.add('act', lambda e, A=A: e.activation(out=st_g[sl][:, s, 0:384], in_=pt[A][:, 0:384], func=AF.Silu), reads=[r_pt[A]], writes=[r_stg[sl][s]])
                        A = tm_mm(3200, 384)
                        P.add('act', lambda e, A=A: e.activation(out=st_g[sl][:, s, 384:768], in_=pt[A][:, 0:384], func=AF.Silu), reads=[r_pt[A]], writes=[r_stg[sl][s]])
                        A = tm_mm(1408, 256)
                        g = cnt['w'] % 2
                        cnt['w'] += 1
                        P.add('act', lambda e, A=A, g=g: e.activation(out=gv[g][:], in_=pt[A][:, 0:256], func=AF.Gelu_apprx_tanh), reads=[r_pt[A]], writes=[r_gv[g]])
                        g3 = lambda ap: ap.rearrange('p (g c) -> p g c', g=4)
                        bc = lambda ap: ap.unsqueeze(2).broadcast_to([128, 4, 64])
                        P.add('dve', lambda e, g=g: e.tensor_reduce(out=sm[:, 0:4], in_=g3(gv[g][:]), axis=AX.X, op=ALU.add), reads=[r_gv[g]], writes=[r_sm])
                        P.add('dve', lambda e, g=g: e.tensor_tensor(out=gsq[:], in0=gv[g][:], in1=gv[g][:], op=ALU.mult), reads=[r_gv[g]], writes=[r_gsq])
                        P.add('dve', lambda e: e.tensor_reduce(out=sm[:, 4:8], in_=g3(gsq[:]), axis=AX.X, op=ALU.add), reads=[r_gsq], writes=[r_sm])
                        P.add('dve', lambda e: e.tensor_scalar(out=sm[:, 8:12], in0=sm[:, 0:4], scalar1=1.0 / 64, scalar2=None, op0=ALU.mult), reads=[r_sm], writes=[r_sm])
                        P.add('dve', lambda e: e.tensor_tensor(out=sm[:, 12:16], in0=sm[:, 8:12], in1=sm[:, 8:12], op=ALU.mult), reads=[r_sm], writes=[r_sm])
                        P.add('dve', lambda e: e.scalar_tensor_tensor(out=sm[:, 16:20], in0=sm[:, 4:8], scalar=1.0 / 64, in1=sm[:, 12:16], op0=ALU.mult, op1=ALU.subtract), reads=[r_sm], writes=[r_sm])
                        P.add('act', lambda e: e.activation(out=sm[:, 16:20], in_=sm[:, 16:20], func=AF.Sqrt, bias=LN_EPS), reads=[r_sm], writes=[r_sm])
                        P.add('dve', lambda e: e.reciprocal(out=sm[:, 20:24], in_=sm[:, 16:20]), reads=[r_sm], writes=[r_sm])
                        P.add('dve', lambda e, g=g: e.tensor_tensor(out=g3(cen[:]), in0=g3(gv[g][:]), in1=bc(sm[:, 8:12]), op=ALU.subtract), reads=[r_gv[g], r_sm], writes=[r_cen])
                        P.add('dve', lambda e: e.tensor_tensor(out=g3(cen[:]), in0=g3(cen[:]), in1=bc(sm[:, 20:24]), op=ALU.mult), reads=[r_cen, r_sm], writes=[r_cen])
                        P.add('pool', lambda e: e.tensor_tensor(out=cen[:], in0=cen[:], in1=lnw[:], op=ALU.mult), reads=[r_cen, r_ln], writes=[r_cen])
                        P.add('pool', lambda e: e.tensor_tensor(out=st_vn[sl][:, s, :], in0=cen[:], in1=lnb[:], op=ALU.add), reads=[r_cen, r_ln], writes=[r_stvn[sl][s]])
                        for (stg, r_stg_, dten) in ((st_v[sl], r_stv[sl][s], v_na), (st_rv[sl], r_strv[sl][s], rv_d), (st_g[sl], r_stg[sl][s], gates_d), (st_vn[sl], r_stvn[sl][s], vn_sg)):
                            P.add('pool', lambda e, stg=stg, dten=dten: e.dma_start(out=dten[t0 + s * 128:t0 + (s + 1) * 128, :], in_=stg[:, s, :]), reads=[r_stg_], writes=[DR(id(dten), ti)], dma=True)
                    for s_ in range(T // 128):
                        do_sub(s_)

                prepare(0)
                for k in range(len(tiles)):
                    if k + 1 < len(tiles):
                        nsteps.extend(prepare_steps(k + 1))
                    do_tile(k)
                    while nsteps:
                        nsteps.pop(0)()
                P.emit_pass()

        def pass_outproj(l, src, dst, skip_ctx):
            with ExitStack() as es:
                sb = lambda name, shape, dt=F32: es.enter_context(_sbuf_tensor(name, list(shape), dt))
                wo = sb('wo', [128, KC, D], BF16)
                r_wo = [Res() for _ in range(KC)]
                load_w(wo, 'wo_%d' % l, wout_in[l], KC, D, r_wo)
                conv_issue()
                ct = [sb('ct%d' % i, [128, KC, 512], BF16) for i in range(2)]
                r_ct = [Res() for _ in range(2)]
                xin = [sb('xin%d' % i, [128, 512]) for i in range(3)]
                r_xin = [Res() for _ in range(3)]
                xo = [sb('xo%d' % i, [128, 512]) for i in range(3)]
                r_xo = [Res() for _ in range(3)]
                po = [es.enter_context(_psum_tensor('po%d' % i, [128, 512], F32)) for i in range(3)]
                r_po = [Res() for _ in range(3)]
                srcv = src.rearrange('(kc p) t -> p kc t', p=128)
                dstv = dst.rearrange('(kc p) t -> p kc t', p=128)
                catv = catT.rearrange('(kc p) t -> p kc t', p=128)
                cnt = {'i': 0}

                def do_tile(ti):
                    t0, T, j = tiles[ti]
                    sl = ti % 2
                    P.add('sp', lambda e: e.dma_start(out=ct[sl][:, :, 0:T], in_=catv[:, :, t0:t0 + T]), reads=[DR(id(catT), ti)], writes=[r_ct[sl]], dma=True)
                    for mo in range(KC):
                        b = cnt['i'] % 3
                        cnt['i'] += 1
                        P.add('sp', lambda e, b=b, mo=mo: e.dma_start(out=xin[b][:, 0:T], in_=srcv[:, mo, t0:t0 + T]), reads=[DR(id(src), ti)], writes=[r_xin[b]], dma=True)
                        for kc in range(KC):
                            P.add('pe', lambda e, b=b, mo=mo, kc=kc: e.matmul(po[b][:, 0:T], lhsT=wo[:, kc, mo * 128:(mo + 1) * 128], rhs=ct[sl][:, kc, 0:T], start=(kc == 0), stop=(kc == KC - 1)),
                                  reads=[r_wo[kc], r_ct[sl]], writes=[r_po[b]])
                        P.add('dve', lambda e, b=b, mo=mo: e.scalar_tensor_tensor(out=xo[b][:, 0:T], in0=po[b][:, 0:T], scalar=mt(2, 1, mo, j), in1=xin[b][:, 0:T], op0=ALU.mult, op1=ALU.add),
                              reads=[r_po[b], r_xin[b], r_mtab], writes=[r_xo[b]])
                        P.add('pool', lambda e, b=b, mo=mo: e.dma_start(out=dstv[:, mo, t0:t0 + T], in_=xo[b][:, 0:T]), reads=[r_xo[b]], writes=[DR(id(dst), ti)], dma=True)

                for ti in range(len(tiles)):
                    if skip_ctx and tiles[ti][2] == 1:
                        continue
                    do_tile(ti)
                P.emit_pass()

        def pass_mixer(l, last):
            with ExitStack() as es:
                conv_issue()
                sb = lambda name, shape, dt=F32: es.enter_context(_sbuf_tensor(name, list(shape), dt))
                E = sb('E', [128, NV, 6, 5, 128], BF16)
                r_E = Res()
                Sst = sb('Sst', [128, NCH, 2, 3, 64], BF16)
                r_Sst = [[Res(), Res()] for _ in range(NCH)]
                dl = sb('dl', [128, 12]); e1 = sb('e1', [128, 12]); lg = sb('lg', [128, 12]); nlg = sb('nlg', [128, 12])
                lgsel = sb('lgsel', [128, 2, 3]); g128 = sb('g128', [128, 2, 3]); te = sb('te', [128, 2, 6])
                fs = sb('fs', [128, 2, 3, 128]); fsm = sb('fsm', [128, 2, 6, 128]); Dm = sb('Dm', [128, 2, 6, 128])
                r_tab = Res()
                st = sb('st', [128, 2, 3, 64])
                r_st = [Res(), Res()]
                sgw = sb('sgw', [128, 4, 128], BF16); sgbt = sb('sgbt', [128, 2, 128]); gnw = sb('gnw', [128, 384])
                r_sgt = Res()
                pS = es.enter_context(_psum_tensor('pS', [128, 1024], F32)); r_pS = Res()
                pS2 = es.enter_context(_psum_tensor('pS2', [128, 1024], F32)); r_pS2 = Res()
                pO = es.enter_context(_psum_tensor('pO', [128, 512], F32)); r_pO = Res()
                pRo = es.enter_context(_psum_tensor('pRo', [128, 1024], F32)); r_pRo = Res()
                pT = es.enter_context(_psum_tensor('pT', [128, 1024], BF16)); r_pT = Res()
                pG = pRo[:, 768:1024]; r_pG = Res()
                pKV = pS2[:, 0:512]; r_pKV = r_pS2

                P.add('sp', lambda e: e.dma_start(out=dl[:], in_=dlog_in[:, l * 12:(l + 1) * 12]), writes=[r_tab], dma=True)
                P.add('act', lambda e: e.activation(out=e1[:], in_=dl[:], func=AF.Exp, scale=-1.0), reads=[r_tab], writes=[r_tab])
                P.add('act', lambda e: e.activation(out=nlg[:], in_=e1[:], func=AF.Ln, bias=1.0), reads=[r_tab], writes=[r_tab])
                P.add('dve', lambda e: e.tensor_scalar(out=lg[:], in0=nlg[:], scalar1=-1.0, scalar2=None, op0=ALU.mult), reads=[r_tab], writes=[r_tab])
                for d_ in range(2):
                    v2 = lg[:, d_ * 6:(d_ + 1) * 6].rearrange('p (r two) -> p r two', two=2)
                    P.add('dve', lambda e, d_=d_, v2=v2: e.tensor_copy(out=lgsel[0:64, d_, :], in_=v2[0:64, :, 0]), reads=[r_tab], writes=[r_tab])
                    P.add('dve', lambda e, d_=d_, v2=v2: e.tensor_copy(out=lgsel[64:128, d_, :], in_=v2[64:128, :, 1]), reads=[r_tab], writes=[r_tab])
                P.add('act', lambda e: e.activation(out=g128[:], in_=lgsel[:], func=AF.Exp, scale=128.0), reads=[r_tab], writes=[r_tab])
                P.add('act', lambda e: e.activation(out=te[:, 0, :], in_=lg[:, 0:6], func=AF.Exp, scale=cst[:, C_PREV:C_PREV + 1]), reads=[r_tab, r_cst], writes=[r_tab])
                P.add('act', lambda e: e.activation(out=te[:, 1, :], in_=lg[:, 6:12], func=AF.Exp, scale=cst[:, C_PCOL:C_PCOL + 1]), reads=[r_tab, r_cst], writes=[r_tab])
                for pr in range(3):
                    P.add('act', lambda e, pr=pr: e.activation(out=fs[:, 0, pr, :], in_=cst[:, C_IP1:C_IP1 + 128], func=AF.Exp, scale=lgsel[:, 0, pr:pr + 1], bias=lnb8[:, 0:1]),
                          reads=[r_tab, r_cst], writes=[r_tab])
                    P.add('act', lambda e, pr=pr: e.activation(out=fs[:, 1, pr, :], in_=cst[:, C_REV:C_REV + 128], func=AF.Exp, scale=lgsel[:, 1, pr:pr + 1], bias=lnb8[:, 0:1]),
                          reads=[r_tab, r_cst], writes=[r_tab])
                P.add('dve', lambda e: e.memset(fsm[:], 0.0), writes=[r_tab])
                for d_ in range(2):
                    for h in range(6):
                        r0_ = (h % 2) * 64
                        P.add('dve', lambda e, d_=d_, h=h, r0_=r0_: e.tensor_copy(out=fsm[r0_:r0_ + 64, d_, h, :], in_=fs[r0_:r0_ + 64, d_, h // 2, :]), reads=[r_tab], writes=[r_tab])
                for h in range(6):
                    P.add('act', lambda e, h=h: e.activation(out=Dm[:, 0, h, :], in_=cst[:, C_DIFF:C_DIFF + 128], func=AF.Exp, scale=lg[:, h:h + 1], bias=lnb8[:, 0:1]),
                          reads=[r_tab, r_cst], writes=[r_tab])
                    P.add('act', lambda e, h=h: e.activation(out=Dm[:, 1, h, :], in_=cst[:, C_DIFF:C_DIFF + 128], func=AF.Exp, scale=nlg[:, 6 + h:7 + h], bias=lnb8[:, 0:1]),
                          reads=[r_tab, r_cst], writes=[r_tab])
                mFb = cst[:, C_MF:C_MF + 128].unsqueeze(1).broadcast_to([128, 6, 128])
                mBb = cst[:, C_MB:C_MB + 128].unsqueeze(1).broadcast_to([128, 6, 128])
                P.add('dve', lambda e: e.tensor_tensor(out=Dm[:, 0, :, :], in0=Dm[:, 0, :, :], in1=mFb, op=ALU.mult), reads=[r_tab, r_cst], writes=[r_tab])
                P.add('dve', lambda e: e.tensor_tensor(out=Dm[:, 1, :, :], in0=Dm[:, 1, :, :], in1=mBb, op=ALU.mult), reads=[r_tab, r_cst], writes=[r_tab])
                bst = [sb('bst%d' % i, [128, 5, 128]) for i in range(2)]
                r_bst = [Res(), Res()]
                for v in range(NV):
                    for h in range(6):
                        b = (v * 6 + h) % 2
                        P.add('sp', lambda e, b=b, v=v, h=h: e.dma_start(out=bst[b][:], in_=bias_in[l, v, h]), writes=[r_bst[b]], dma=True)
                        P.add('act', lambda e, b=b, v=v, h=h: e.activation(out=E[:, v, h, :, :], in_=bst[b][:], func=AF.Exp), reads=[r_bst[b]], writes=[r_E])
                P.add('pool', lambda e: e.dma_start(out=sgw[:], in_=sgw_in[l].rearrange('g q p -> q g p')), writes=[r_sgt], dma=True)
                P.add('sp', lambda e: e.dma_start(out=sgbt[:], in_=sgb_in[l].rearrange('g p f -> p g f')), writes=[r_sgt], dma=True)
                P.add('sp', lambda e: e.dma_start(out=gnw[:], in_=gnw_in[:, l * 384:(l + 1) * 384]), writes=[r_sgt], dma=True)

                qv = lambda ten: ten.rearrange('(c p) t -> p c t', p=128)
                rkb = [sb('rkb%d' % i, [128, 3, 128], BF16) for i in range(2)]
                rvb = [sb('rvb%d' % i, [128, 384], BF16) for i in range(2)]
                r_rkb = [Res(), Res()]; r_rvb = [Res(), Res()]
                kte = [sb('kte%d' % i, [128, 384], BF16) for i in range(2)]
                r_kte = [Res(), Res()]
                P.add('dve', lambda e: e.memset(st[:], 0.0), writes=[r_st[0], r_st[1]])
                order_f = [NLC, NLC + 1] + list(range(NLC))
                order_b = [NLC + 1, NLC] + list(range(NLC - 1, -1, -1))
                sc = {'i': 0}

                def scan_step(dr, c):
                    i = sc['i'] % 2
                    sc['i'] += 1
                    tok = chunk_tok(c)
                    tl = tile_of_chunk(c)
                    P.add('act', lambda e: e.copy(out=Sst[:, c, dr, :, :], in_=st[:, dr, :, :]), reads=[r_st[dr]], writes=[r_Sst[c][dr]])
                    P.add('sp', lambda e: e.dma_start(out=rkb[i][:], in_=qv(rkT)[:, :, tok:tok + 128]), reads=[DR(id(rkT), tl)], writes=[r_rkb[i]], dma=True)
                    P.add('sp', lambda e: e.dma_start(out=rvb[i][:], in_=rv_d[tok:tok + 128, :]), reads=[DR(id(rv_d), tl)], writes=[r_rvb[i]], dma=True)
                    for pr in range(3):
                        P.add('pe', lambda e, pr=pr: e.transpose(pT[:, pr * 128:(pr + 1) * 128], rkb[i][:, pr, :], ident_b), reads=[r_rkb[i], r_cstbf], writes=[r_pT])
                    P.add('dve', lambda e: e.tensor_tensor(out=kte[i][:].rearrange('p (h d) -> p h d', h=6), in0=pT[:, 0:384].rearrange('p (h d) -> p h d', h=6),
                                                           in1=te[:, dr, :].unsqueeze(2).broadcast_to([128, 6, 64]), op=ALU.mult),
                          reads=[r_pT, r_tab], writes=[r_kte[i]])
                    for pr in range(3):
                        P.add('pe', lambda e, pr=pr: e.matmul(pS2[:, pr * 128:(pr + 1) * 128], lhsT=kte[i][:, pr * 128:(pr + 1) * 128], rhs=rvb[i][:, pr * 128:(pr + 1) * 128], start=True, stop=True),
                              reads=[r_kte[i], r_rvb[i]], writes=[r_pKV])
                    P.add('dve', lambda e: e.tensor_tensor(out=st[:, dr, :, :], in0=st[:, dr, :, :], in1=g128[:, dr, :].unsqueeze(2).broadcast_to([128, 3, 64]), op=ALU.mult),
                          reads=[r_st[dr], r_tab], writes=[r_st[dr]])
                    kv3 = pS2[:, 0:384].rearrange('p (r c) -> p r c', r=3)
                    P.add('dve', lambda e: e.tensor_tensor(out=st[0:64, dr, :, :], in0=st[0:64, dr, :, :], in1=kv3[0:64, :, 0:64], op=ALU.add), reads=[r_st[dr], r_pKV], writes=[r_st[dr]])
                    P.add('dve', lambda e: e.tensor_tensor(out=st[64:128, dr, :, :], in0=st[64:128, dr, :, :], in1=kv3[64:128, :, 64:128], op=ALU.add), reads=[r_st[dr], r_pKV], writes=[r_st[dr]])

                if 'scan' in MIXDBG:
                    for c in order_f:
                        scan_step(0, c)
                    for c in order_b:
                        scan_step(1, c)

                NSLOT = 8
                kslot = [sb('ks%d' % i, [128, 3, 128], BF16) for i in range(NSLOT + 2)]
                vslot = [sb('vs%d' % i, [128, 6, 65], BF16) for i in range(NSLOT + 2)]
                r_slot = [Res() for _ in range(NSLOT + 2)]
                slot_chunk = [None] * (NSLOT + 2)
                for i in range(NSLOT + 2):
                    P.add('pool', lambda e, i=i: e.memset(vslot[i][:], 1.0), writes=[r_slot[i]])
                dbl = lambda name, shape, dt=BF16: [sb(name + '%d' % i, shape, dt) for i in range(2)]
                qna = dbl('qna', [128, 3, 128]); uu = dbl('uu', [128, 2, 128]); vnb = dbl('vnb', [128, 256])
                rqb = dbl('rqb', [128, 3, 128]); rkc = dbl('rkc', [128, 3, 128]); rvc = dbl('rvc', [128, 384]); gtb = dbl('gtb', [128, 768])
                r_ld = [Res(), Res()]
                expS = dbl('expS', [128, 7, 128]); r_expS = [Res(), Res()]
                qfs = dbl('qfs', [128, 2, 6, 128]); r_qfs = [Res(), Res()]
                SD = dbl('SD', [128, 2, 6, 128]); r_SD = [Res(), Res()]
                natok = dbl('natok', [128, 384]); r_natok = [Res(), Res()]
                rettok = dbl('rettok', [128, 384]); r_rettok = [Res(), Res()]
                catst = dbl('catst', [128, 8, 128]); r_catst = [Res(), Res()]
                rec = dbl('rec', [128, 6], F32); r_rec = [Res(), Res()]
                sgt = dbl('sgtmp', [128, 2, 128], F32); r_sgtmp = [Res(), Res()]
                osb = dbl('osb', [128, 768], F32); r_osb = [Res(), Res()]
                osq = dbl('osq', [128, 768], F32); r_osq = [Res(), Res()]
                gsm = dbl('gsm', [128, 72], F32); r_gsm = [Res(), Res()]
                g2 = dbl('g2', [128, 768], F32); r_g2 = [Res(), Res()]
                g3t = dbl('g3t', [128, 384], F32); r_g3 = [Res(), Res()]

                def ensure_slot(kc_):
                    if kc_ >= NLC:
                        s = NSLOT + (kc_ - NLC)
                    else:
                        s = kc_ % NSLOT
                    if slot_chunk[s] != kc_:
                        slot_chunk[s] = kc_
                        tok = chunk_tok(kc_)
                        tl = tile_of_chunk(kc_)
                        P.add('sp', lambda e: e.dma_start(out=kslot[s][:], in_=qv(kT_na)[:, :, tok:tok + 128]), reads=[DR(id(kT_na), tl)], writes=[r_slot[s]], dma=True)
                        P.add('sp', lambda e: e.dma_start(out=vslot[s][:, :, 0:64], in_=v_na[tok:tok + 128, :].rearrange('p (h d) -> p h d', h=6)), reads=[DR(id(v_na), tl)], writes=[r_slot[s]], dma=True)
                    return s

                def do_chunk(c, idx):
                    cb = idx % 2
                    tok = chunk_tok(c)
                    tl = tile_of_chunk(c)
                    is_ctx = c >= NLC
                    for (dst_, ten, fm) in ((qna[cb], qT_na, True), (uu[cb], uT_sg, True), (rqb[cb], rqT, True), (rkc[cb], rkT, True)):
                        P.add('sp', lambda e, dst_=dst_, ten=ten: e.dma_start(out=dst_[:], in_=qv(ten)[:, :, tok:tok + 128]), reads=[DR(id(ten), tl)], writes=[r_ld[cb]], dma=True)
                    for (dst_, ten) in ((vnb[cb], vn_sg), (rvc[cb], rv_d), (gtb[cb], gates_d)):
                        P.add('sp', lambda e, dst_=dst_, ten=ten: e.dma_start(out=dst_[:], in_=ten[tok:tok + 128, :]), reads=[DR(id(ten), tl)], writes=[r_ld[cb]], dma=True)
                    if is_ctx:
                        kchunks = []
                    else:
                        kchunks = [base_of[c] + m for m in range(5)]
                    kchunks = kchunks + [NLC, NLC + 1]
                    slots = [ensure_slot(kc_) for kc_ in kchunks]
                    nb = len(slots)
                    var = None if is_ctx else var_of[c]
                    def sec_na(pend):
                        pSb = [pS, pS2]
                        r_pSb = [r_pS, r_pS2]

                        def scores(h):
                            buf, pc, r0 = h % 2, h // 2, (h % 2) * 64
                            for bi, s in enumerate(slots):
                                P.add('pe', lambda e, bi=bi, s=s: e.matmul(pSb[buf][:, bi * 128:(bi + 1) * 128], lhsT=kslot[s][r0:r0 + 64, pc, :], rhs=qna[cb][r0:r0 + 64, pc, :], start=True, stop=True),
                                      reads=[r_slot[s], r_ld[cb]], writes=[r_pSb[buf]])

                        def soft(h):
                            buf = eb = h % 2
                            P.add('act', lambda e: e.activation(out=expS[eb][:, 0:nb, :], in_=pSb[buf][:, 0:nb * 128].rearrange('p (b q) -> p b q', b=nb), func=AF.Exp),
                                  reads=[r_pSb[buf]], writes=[r_expS[eb]])
                            if not is_ctx:
                                P.add('dve', lambda e: e.tensor_tensor(out=expS[eb][:, 0:5, :], in0=expS[eb][:, 0:5, :], in1=E[:, var, h, :, :], op=ALU.mult),
                                      reads=[r_expS[eb], r_E], writes=[r_expS[eb]])

                        def pv(h):
                            eb = h % 2
                            for bi, s in enumerate(slots):
                                P.add('pe', lambda e, bi=bi, s=s: e.matmul(pO[:, h * 65:(h + 1) * 65], lhsT=expS[eb][:, bi, :], rhs=vslot[s][:, h, :], start=(bi == 0), stop=(bi == nb - 1)),
                                      reads=[r_expS[eb], r_slot[s]], writes=[r_pO])

                        scores(0)
                        for h in range(6):
                            if h + 1 < 6:
                                scores(h + 1)
                            soft(h)
                            pv(h)
                            for _ in range(3):
                                if pend:
                                    pend.pop(0)()
                        while pend:
                            pend.pop(0)()
                        po3 = pO[:, 0:390].rearrange('p (h d) -> p h d', h=6)
                        P.add('dve', lambda e: e.reciprocal(out=rec[cb][:], in_=po3[:, :, 64]), reads=[r_pO], writes=[r_rec[cb]])
                        P.add('dve', lambda e: e.tensor_tensor(out=natok[cb][:].rearrange('p (h d) -> p h d', h=6), in0=po3[:, :, 0:64], in1=rec[cb][:].unsqueeze(2).broadcast_to([128, 6, 64]), op=ALU.mult),
                              reads=[r_pO, r_rec[cb]], writes=[r_natok[cb]])

                    def sec_sg():
                        for gp in range(2):
                            for half in range(2):
                                g = 2 * gp + half
                                P.add('pe', lambda e, gp=gp, half=half, g=g: e.matmul(pRo[half * 64:(half + 1) * 64, 768 + gp * 128:768 + (gp + 1) * 128], lhsT=vnb[cb][:, g * 64:(g + 1) * 64], rhs=sgw[:, g, :], start=True, stop=True),
                                      reads=[r_ld[cb], r_sgt], writes=[r_pG])
                        P.add('dve', lambda e: e.tensor_tensor(out=sgt[cb][:], in0=pRo[:, 768:1024].rearrange('p (g q) -> p g q', g=2), in1=sgbt[:], op=ALU.add), reads=[r_pG, r_sgt], writes=[r_sgtmp[cb]])
                        P.add('dve', lambda e: e.tensor_tensor(out=catst[cb][:, 3:5, :], in0=sgt[cb][:], in1=uu[cb][:], op=ALU.mult), reads=[r_sgtmp[cb], r_ld[cb]], writes=[r_catst[cb]])

                    def sec_ret():
                        for dr in range(2):
                            P.add('dve', lambda e, dr=dr: e.tensor_tensor(out=qfs[cb][:, dr, :, :].rearrange('p (j two) q -> p j two q', two=2),
                                                                          in0=rqb[cb][:].unsqueeze(2).broadcast_to([128, 3, 2, 128]),
                                                                          in1=fsm[:, dr, :, :].rearrange('p (j two) q -> p j two q', two=2), op=ALU.mult),
                                  reads=[r_ld[cb], r_tab], writes=[r_qfs[cb]])
                        for h in range(6):
                            pc, half = h // 2, h % 2
                            r0 = half * 64
                            P.add('pe', lambda e, h=h, pc=pc, r0=r0, half=half: e.matmul(pS[:, half * 512 + pc * 128:half * 512 + (pc + 1) * 128], lhsT=rkc[cb][r0:r0 + 64, pc, :], rhs=rqb[cb][r0:r0 + 64, pc, :], start=True, stop=True),
                                  reads=[r_ld[cb]], writes=[r_pS])
                        for dr in range(2):
                            for par in range(2):
                                P.add('dve', lambda e, dr=dr, par=par: e.tensor_tensor(out=SD[cb][:, dr, :, :].rearrange('p (j two) q -> p j two q', two=2)[:, :, par, :],
                                                                                      in0=pS[:, par * 512:par * 512 + 384].rearrange('p (j q) -> p j q', j=3),
                                                                                      in1=Dm[:, dr, :, :].rearrange('p (j two) q -> p j two q', two=2)[:, :, par, :], op=ALU.mult),
                                      reads=[r_pS, r_tab], writes=[r_SD[cb]])
                        for dr in range(2):
                            for h in range(6):
                                pc, half = h // 2, h % 2
                                r0 = half * 64
                                ob = (dr * 6 + h) * 64
                                P.add('pe', lambda e, dr=dr, h=h, ob=ob: e.matmul(pRo[:, ob:ob + 64], lhsT=SD[cb][:, dr, h, :], rhs=rvc[cb][:, h * 64:(h + 1) * 64], start=True, stop=False),
                                      reads=[r_SD[cb], r_ld[cb]], writes=[r_pRo])
                                P.add('pe', lambda e, dr=dr, h=h, ob=ob, pc=pc, r0=r0: e.matmul(pRo[:, ob:ob + 64], lhsT=qfs[cb][:, dr, h, :], rhs=Sst[:, c, dr, pc, :], start=False, stop=True),
                                      reads=[r_qfs[cb], r_Sst[c][dr]], writes=[r_pRo])

                    def gn_steps():
                        st_ = []
                        o3 = lambda ap: ap.rearrange('p (g e) -> p g e', g=12)
                        b12 = lambda ap: ap.unsqueeze(2).broadcast_to([128, 12, 64])
                        st_.append(lambda: P.add('dve', lambda e: e.tensor_copy(out=osb[cb][:], in_=pRo[:, 0:768]), reads=[r_pRo], writes=[r_osb[cb]]))
                        st_.append(lambda: P.add('dve', lambda e: e.tensor_reduce(out=gsm[cb][:, 0:12], in_=o3(osb[cb][:]), axis=AX.X, op=ALU.add), reads=[r_osb[cb]], writes=[r_gsm[cb]]))
                        st_.append(lambda: P.add('act', lambda e: e.activation(out=osq[cb][:], in_=osb[cb][:], func=AF.Square), reads=[r_osb[cb]], writes=[r_osq[cb]]))
                        st_.append(lambda: P.add('dve', lambda e: e.tensor_reduce(out=gsm[cb][:, 12:24], in_=o3(osq[cb][:]), axis=AX.X, op=ALU.add), reads=[r_osq[cb]], writes=[r_gsm[cb]]))
                        st_.append(lambda: P.add('dve', lambda e: e.tensor_scalar(out=gsm[cb][:, 24:36], in0=gsm[cb][:, 0:12], scalar1=1.0 / 64, scalar2=None, op0=ALU.mult), reads=[r_gsm[cb]], writes=[r_gsm[cb]]))
                        st_.append(lambda: P.add('dve', lambda e: e.tensor_tensor(out=gsm[cb][:, 36:48], in0=gsm[cb][:, 24:36], in1=gsm[cb][:, 24:36], op=ALU.mult), reads=[r_gsm[cb]], writes=[r_gsm[cb]]))
                        st_.append(lambda: P.add('dve', lambda e: e.scalar_tensor_tensor(out=gsm[cb][:, 48:60], in0=gsm[cb][:, 12:24], scalar=1.0 / 64, in1=gsm[cb][:, 36:48], op0=ALU.mult, op1=ALU.subtract), reads=[r_gsm[cb]], writes=[r_gsm[cb]]))
                        st_.append(lambda: P.add('act', lambda e: e.activation(out=gsm[cb][:, 48:60], in_=gsm[cb][:, 48:60], func=AF.Sqrt, bias=lnbe[:, 0:1]), reads=[r_gsm[cb]], writes=[r_gsm[cb]]))
                        st_.append(lambda: P.add('dve', lambda e: e.reciprocal(out=gsm[cb][:, 60:72], in_=gsm[cb][:, 48:60]), reads=[r_gsm[cb]], writes=[r_gsm[cb]]))
                        st_.append(lambda: P.add('dve', lambda e: e.tensor_tensor(out=o3(g2[cb][:]), in0=o3(osb[cb][:]), in1=b12(gsm[cb][:, 24:36]), op=ALU.subtract), reads=[r_osb[cb], r_gsm[cb]], writes=[r_g2[cb]]))
                        st_.append(lambda: P.add('dve', lambda e: e.tensor_tensor(out=o3(g2[cb][:]), in0=o3(g2[cb][:]), in1=b12(gsm[cb][:, 60:72]), op=ALU.mult), reads=[r_g2[cb], r_gsm[cb]], writes=[r_g2[cb]]))
                        st_.append(lambda: P.add('dve', lambda e: e.tensor_tensor(out=g2[cb][:], in0=g2[cb][:], in1=gtb[cb][:], op=ALU.mult), reads=[r_g2[cb], r_ld[cb]], writes=[r_g2[cb]]))
                        st_.append(lambda: P.add('dve', lambda e: e.tensor_tensor(out=g3t[cb][:], in0=g2[cb][:, 0:384], in1=g2[cb][:, 384:768], op=ALU.add), reads=[r_g2[cb]], writes=[r_g3[cb]]))
                        st_.append(lambda: P.add('dve', lambda e: e.tensor_tensor(out=rettok[cb][:], in0=g3t[cb][:], in1=gnw[:], op=ALU.mult), reads=[r_g3[cb], r_sgt], writes=[r_rettok[cb]]))
                        return st_

                    def sec_tr():
                        for pc in range(3):
                            P.add('pe', lambda e, pc=pc: e.transpose(pT[:, pc * 128:(pc + 1) * 128], natok[cb][:, pc * 128:(pc + 1) * 128], ident_b), reads=[r_natok[cb], r_cstbf], writes=[r_pT])
                        for pc in range(3):
                            P.add('pe', lambda e, pc=pc: e.transpose(pT[:, (3 + pc) * 128:(4 + pc) * 128], rettok[cb][:, pc * 128:(pc + 1) * 128], ident_b), reads=[r_rettok[cb], r_cstbf], writes=[r_pT])
                        P.add('act', lambda e: e.copy(out=catst[cb][:, 0:3, :], in_=pT[:, 0:384].rearrange('p (c t) -> p c t', c=3)), reads=[r_pT], writes=[r_catst[cb]])
                        P.add('act', lambda e: e.copy(out=catst[cb][:, 5:8, :], in_=pT[:, 384:768].rearrange('p (c t) -> p c t', c=3)), reads=[r_pT], writes=[r_catst[cb]])
                        P.add('pool', lambda e: e.dma_start(out=catT.rearrange('(kc p) t -> p kc t', p=128)[:, :, tok:tok + 128], in_=catst[cb][:]), reads=[r_catst[cb]], writes=[DR(id(catT), tl)], dma=True)

                    return sec_na, sec_sg, sec_ret, gn_steps, sec_tr

                chunks = list(range(NLC)) + ([] if last else [NLC, NLC + 1])
                pend_gn = []
                pend_tr = None
                for idx, c in enumerate(chunks):
                    na_, sg_, ret_, gn_, tr_ = do_chunk(c, idx)
                    na_(pend_gn)
                    sg_()
                    ret_()
                    if pend_tr is not None:
                        pend_tr()
                    pend_gn, pend_tr = gn_(), tr_
                while pend_gn:
                    pend_gn.pop(0)()
                pend_tr()
                P.emit_pass()

        for l_ in range(n_layers):
            conv_jobs.append([('w1_%d_0' % l_, w1_in[l_, 0]), ('w2_%d_0' % l_, w2_in[l_, 0])])
            conv_jobs.append([('win_%d' % l_, win_in[l_])])
            conv_jobs.append([('wo_%d' % l_, wout_in[l_])])
            conv_jobs.append([('w1_%d_1' % l_, w1_in[l_, 1]), ('w2_%d_1' % l_, w2_in[l_, 1])])
        pass_xpose_in(xTa)
        cur, oth = xTa, xTb
        for l in range(n_layers):
            last = (l == n_layers - 1)
            pass_mod(l)
            pass_ffn(l, 0, cur, oth)
            cur, oth = oth, cur
            if stop_after == 'ffn1':
                break
            pass_inproj(l, cur)
            if stop_after == 'inproj':
                break
            pass_mixer(l, last)
            if stop_after == 'mixer':
                break
            pass_outproj(l, cur, oth, skip_ctx=last)
            cur, oth = oth, cur
            if stop_after == 'outproj':
                break
            pass_ffn(l, 1, cur, oth, skip_ctx=last)
            cur, oth = oth, cur
        pass_xpose_out(cur)
    return nc


def prep_inputs(NT, b, inp, shared):
    m = dict(shared)
    m['x'] = np.ascontiguousarray(inp['x'][b])
    m['ctx'] = np.ascontiguousarray(inp['ctx'][b])
    cc = np.stack([inp['c'][b], inp['c_ctx']], axis=0)
    m['cT'] = np.ascontiguousarray(cc.reshape(2, KC, 128).transpose(2, 1, 0).reshape(128, 16))
    return m


def prep_shared(NT, inp):
    f = lambda a: np.ascontiguousarray(np.asarray(a, dtype=np.float32))
    NLC = NT // 128
    var_of, base_of, ridx, cidx, valid = na_tables(NLC)
    sh = {}
    sh['ada_w'] = f(inp['ada_w'])
    sh['ada_bT'] = f(inp['ada_b'].reshape(2, 72, 128).transpose(0, 2, 1))
    sh['norm_wT'] = f(inp['norm_w'].reshape(2, 3, KC, 128).transpose(3, 0, 1, 2).reshape(128, 48))
    sh['ffn_w1'] = f(inp['ffn_w1'])
    sh['ffn_w2'] = f(inp['ffn_w2'])
    w_in = np.asarray(inp['mix_w_in'], dtype=np.float32)
    perm = np.arange(384).reshape(6, 64)
    part = perm.copy()
    for h in range(6):
        for d in range(64):
            part[h, d] = h * 64 + (d + 16 if (d % 32) < 16 else d - 16)
    part = part.reshape(-1)
    sh['w_in_ext'] = f(np.concatenate([w_in, w_in[:, :, 1664 + part], w_in[:, :, 2048 + part]], axis=2))
    sh['w_out'] = f(inp['mix_w_out'])
    qk = np.zeros((128, 4), np.float32)
    for l in range(2):
        qk[:, 2 * l] = np.tile(inp['na_q_norm'][l], 2)
        qk[:, 2 * l + 1] = np.tile(inp['na_k_norm'][l], 2)
    sh['qknT'] = qk
    rpb = np.asarray(inp['na_rpb'], dtype=np.float32)
    g = rpb[:, :, ridx, cidx]
    g = np.where(valid[None, None], g, np.float32(-1e30)).astype(np.float32)
    sh['na_biasT'] = f(g.transpose(0, 2, 1, 4, 3, 5))
    sh['sg_wT'] = f(np.asarray(inp['sg_w']).transpose(0, 1, 3, 2))
    sgb = np.asarray(inp['sg_b'], dtype=np.float32)
    sh['sgb_rep'] = f(np.repeat(sgb.reshape(2, 2, 2, 1, 128), 64, axis=3).reshape(2, 2, 128, 128))
    sh['sg_lnw_rep'] = f(np.broadcast_to(np.asarray(inp['sg_ln_w']).reshape(1, 512), (128, 512)))
    sh['sg_lnb_rep'] = f(np.broadcast_to(np.asarray(inp['sg_ln_b']).reshape(1, 512), (128, 512)))
    sh['dlog_rep'] = f(np.broadcast_to(np.asarray(inp['ret_decay_logit']).reshape(1, 24), (128, 24)))
    sh['gnw_rep'] = f(np.broadcast_to(np.asarray(inp['ret_gn_w']).reshape(1, 768), (128, 768)))
    C, S = rope_tables(NT)
    sh['ropeC'] = C
    sh['ropeS'] = S
    sh['consts'] = const_table()
    return sh


_NC_CACHE = {}


def kernel(**inputs):
    inp = {k: np.asarray(v) for k, v in inputs.items()}
    B, NT, _ = inp['x'].shape
    if NT not in _NC_CACHE:
        _NC_CACHE[NT] = build(NT)
    nc = _NC_CACHE[NT]
    shared = prep_shared(NT, inp)
    in_maps = [prep_inputs(NT, b, inp, shared) for b in range(B)]
    res = run_bass_kernel_spmd(nc, in_maps, core_ids=list(range(B)))
    return np.stack([np.asarray(r['y']) for r in res.results], axis=0).astype(np.float32)
```

```python
import math
from contextlib import ExitStack

import numpy as np
import concourse.bass as bass
import concourse.mybir as mybir
from concourse.bass_utils import run_bass_kernel_spmd

F32 = mybir.dt.float32
BF16 = mybir.dt.bfloat16
AF = mybir.ActivationFunctionType
ALU = mybir.AluOpType
AX = mybir.AxisListType

ENGS = ['pe', 'act', 'dve', 'pool', 'sp']

D = 1024
KC = 8
DFF = 2816
FC = 22
NCTX = 256
GW = 64
NMOD = 9
INX = 4352
RMS_EPS = 1e-6
LN_EPS = 1e-5
LN8 = math.log(0.125)
import os
MIXDBG = os.environ.get('MIXDBG', 'scan,na,sg,ret,tr').split(',')


class Res:
    __slots__ = ('name', 'last_w', 'readers')

    def __init__(self, name=''):
        self.name = name
        self.last_w = None
        self.readers = []


class Op:
    __slots__ = ('eng', 'fn', 'deps', 'signal', 'seq', 'is_dma', 'dsem', 'dval', 'dprev')


class Prog:
    def __init__(self, nc, es, n_dma_sems=24):
        self.nc = nc
        self.n_dma_sems = n_dma_sems
        self.csem = {e: es.enter_context(nc.semaphore('c_' + e)) for e in ENGS}
        self.nsem = {'sp': 16, 'pool': 6}
        self.dsem = {e: [es.enter_context(nc.semaphore('d_%s_%d' % (e, i))) for i in range(self.nsem[e])]
                     for e in ('sp', 'pool')}
        self.dma_cnt = {e: 0 for e in ENGS}
        self.dma_use = {e: [0] * n_dma_sems for e in ENGS}
        self.seqc = {e: 0 for e in ENGS}
        self.ops = {e: [] for e in ENGS}
        self.waited = {e: {} for e in ENGS}

    def add(self, eng, fn, reads=(), writes=(), dma=False):
        op = Op()
        op.eng = eng
        op.fn = fn
        op.signal = False
        op.seq = 0
        op.is_dma = dma
        op.dsem = None
        op.dval = 0
        op.dprev = 0
        deps = []
        seen = set()

        def push(d):
            if d is None or id(d) in seen:
                return
            seen.add(id(d))
            if d.eng == 'pe' and eng == 'pe' and not d.is_dma and not dma:
                return
            deps.append(d)

        for r in reads:
            push(r.last_w)
        for w in writes:
            push(w.last_w)
            for rd in w.readers:
                push(rd)
        op.deps = deps
        for d in deps:
            d.signal = True
        for r in reads:
            r.readers.append(op)
        for w in writes:
            w.last_w = op
            w.readers = []
        if dma:
            assert eng in ('sp', 'pool')
            k = self.dma_cnt[eng] % self.nsem[eng]
            self.dma_cnt[eng] += 1
            op.dsem = k
            op.dprev = 16 * self.dma_use[eng][k]
            self.dma_use[eng][k] += 1
            op.dval = 16 * self.dma_use[eng][k]
        self.ops[eng].append(op)
        return op

    def emit_pass(self):
        nc = self.nc
        for e in ENGS:
            last = None
            for op in self.ops[e]:
                if not op.is_dma:
                    last = op
            if last is not None:
                last.signal = True
            for op in self.ops[e]:
                if op.signal and not op.is_dma:
                    self.seqc[e] += 1
                    op.seq = self.seqc[e]
        final_c = dict(self.seqc)
        final_d = {e: [16 * u for u in self.dma_use[e]] for e in ('sp', 'pool')}
        with nc.Block() as block:
            regs = {'pe': block.tensor, 'act': block.scalar, 'dve': block.vector,
                    'pool': block.gpsimd, 'sp': block.sync}
            for e in ENGS:
                ops = self.ops[e]

                def body(eng, e=e, ops=ops):
                    waited = self.waited[e]
                    for op in ops:
                        for d in op.deps:
                            if d.is_dma:
                                key = ('d', d.eng, d.dsem)
                                sem = self.dsem[d.eng][d.dsem]
                                val = d.dval
                            else:
                                key = ('c', d.eng)
                                sem = self.csem[d.eng]
                                val = d.seq
                            if waited.get(key, 0) >= val:
                                continue
                            waited[key] = val
                            eng.wait_ge(sem, val)
                        if op.is_dma:
                            key = ('d', e, op.dsem)
                            if op.dprev > 0 and waited.get(key, 0) < op.dprev:
                                waited[key] = op.dprev
                                eng.wait_ge(self.dsem[e][op.dsem], op.dprev)
                            ins = op.fn(eng)
                            ins.then_inc(self.dsem[e][op.dsem], 16)
                        else:
                            ins = op.fn(eng)
                            if op.signal:
                                ins.then_inc(self.csem[e], 1)
                    for e2 in ENGS:
                        if e2 != e and final_c[e2] > waited.get(('c', e2), 0):
                            waited[('c', e2)] = final_c[e2]
                            eng.wait_ge(self.csem[e2], final_c[e2])
                    for q in ('sp', 'pool'):
                        for k in range(self.nsem[q]):
                            if final_d[q][k] > waited.get(('d', q, k), 0):
                                waited[('d', q, k)] = final_d[q][k]
                                eng.wait_ge(self.dsem[q][k], final_d[q][k])

                regs[e](body)
        self.ops = {e: [] for e in ENGS}


def na_tables(nchunks):
    rows = 2 * nchunks
    kr = min(8, rows)
    sigs = {}
    var_of = []
    base_of = []
    tabs = []
    ir = np.arange(128) // 64
    cc = np.arange(128) % 64
    for j in range(nchunks):
        base = int(np.clip(j - 2, 0, nchunks - 5))
        qrow = 2 * j + ir[None, :]
        qcol = cc[None, :]
        rstart = np.clip(qrow - kr // 2, 0, rows - kr)
        cstart = np.clip(qcol - 8, 0, GW - 16)
        rid = np.zeros((5, 128, 128), np.int64)
        cid = np.zeros((5, 128, 128), np.int64)
        val = np.zeros((5, 128, 128), bool)
        for m in range(5):
            krow = 2 * (base + m) + ir[:, None]
            kcol = cc[:, None]
            v = (krow >= rstart) & (krow < rstart + kr) & (kcol >= cstart) & (kcol < cstart + 16)
            rid[m] = np.clip(krow - qrow + 7, 0, 14)
            cid[m] = np.clip(kcol - qcol + 15, 0, 30)
            val[m] = v
        sig = (base - j, val.tobytes())
        if sig not in sigs:
            sigs[sig] = len(tabs)
            tabs.append((rid, cid, val))
        var_of.append(sigs[sig])
        base_of.append(base)
    ridx = np.stack([t[0] for t in tabs])
    cidx = np.stack([t[1] for t in tabs])
    valid = np.stack([t[2] for t in tabs])
    return var_of, base_of, ridx, cidx, valid


def rope_tables(NT):
    NTOK = NT + NCTX
    t = np.arange(NT)
    inv = (10000.0 ** (-np.arange(16, dtype=np.float32) / 16)).astype(np.float32)
    ang_r = (t // GW).astype(np.float32)[:, None] * inv[None, :]
    ang_c = (t % GW).astype(np.float32)[:, None] * inv[None, :]
    C = np.ones((128, NTOK), np.float32)
    S = np.zeros((128, NTOK), np.float32)
    for p in range(128):
        d = p % 64
        ang = ang_r if d < 32 else ang_c
        f = d % 16
        first = (d % 32) < 16
        C[p, :NT] = np.cos(ang[:, f])
        S[p, :NT] = (-np.sin(ang[:, f])) if first else np.sin(ang[:, f])
    return C, S


def const_table():
    i = np.arange(128, dtype=np.float32)
    diffT = i[None, :] - i[:, None]
    mF = (diffT >= 0).astype(np.float32)
    mB = (diffT <= 0).astype(np.float32)
    ip1 = np.broadcast_to(i[None, :] + 1, (128, 128))
    rev = np.broadcast_to(128 - i[None, :], (128, 128))
    ident = np.eye(128, dtype=np.float32)
    bd = np.zeros((128, 128), np.float32)
    bd[:64, :64] = 1.0 / 64
    bd[64:, 64:] = 1.0 / 64
    pcol = i[:, None]
    prev = 127 - i[:, None]
    return np.ascontiguousarray(np.concatenate([diffT, mF, mB, ip1, rev, ident, bd, pcol, prev], axis=1).astype(np.float32))


C_DIFF, C_MF, C_MB, C_IP1, C_REV, C_ID, C_BD, C_PCOL, C_PREV = [k * 128 for k in range(7)] + [896, 897]
NCONST = 898


def build(NT, n_layers=2, stop_after=None, dbg=False):
    nc = bass.Bass('TRN2', target_bir_lowering=False)
    _uid = [0]

    def _sbuf_tensor(name, shape, dt):
        _uid[0] += 1
        return _orig_sb('%s_u%d' % (name, _uid[0]), shape, dt)

    def _psum_tensor(name, shape, dt):
        _uid[0] += 1
        return _orig_ps('%s_u%d' % (name, _uid[0]), shape, dt)

    _orig_sb = nc.sbuf_tensor
    _orig_ps = nc.psum_tensor
    NTOK = NT + NCTX
    NLC = NT // 128
    NCH = NLC + 2
    tiles = [(i * 512, 512, 0) for i in range(NT // 512)] + [(NT, 256, 1)]
    var_of, base_of, ridx, cidx, valid = na_tables(NLC)
    NV = ridx.shape[0]

    def din(name, shape, dt=F32):
        return nc.dram_tensor(name, list(shape), dt, kind='ExternalInput').ap()

    x_in = din('x', [NT, D])
    ctx_in = din('ctx', [NCTX, D])
    cT_in = din('cT', [128, 16])
    ada_w = din('ada_w', [2, D, NMOD * D])
    ada_bT = din('ada_bT', [2, 128, 72])
    norm_wT = din('norm_wT', [128, 48])
    w1_in = din('ffn_w1', [2, 2, D, 2 * DFF])
    w2_in = din('ffn_w2', [2, 2, DFF, D])
    win_in = din('w_in_ext', [2, D, INX])
    wout_in = din('w_out', [2, D, D])
    qkn_in = din('qknT', [128, 4])
    bias_in = din('na_biasT', [2, NV, 6, 128, 5, 128])
    sgw_in = din('sg_wT', [2, 4, 128, 128])
    sgb_in = din('sgb_rep', [2, 2, 128, 128])
    lnw_in = din('sg_lnw_rep', [128, 512])
    lnb_in = din('sg_lnb_rep', [128, 512])
    dlog_in = din('dlog_rep', [128, 24])
    gnw_in = din('gnw_rep', [128, 768])
    ropeC = din('ropeC', [128, NTOK])
    ropeS = din('ropeS', [128, NTOK])
    const_in = din('consts', [128, NCONST])
    y_out = nc.dram_tensor('y', [NT, D], F32, kind='ExternalOutput').ap()

    dbg_kind = 'ExternalOutput' if dbg else 'Internal'

    def dscr(name, shape, dt):
        return nc.dram_tensor(name, list(shape), dt, kind=dbg_kind).ap()

    xTa = dscr('xTa', [D, NTOK], F32)
    xTb = dscr('xTb', [D, NTOK], F32)
    qT_na = dscr('qT_na', [384, NTOK], BF16)
    kT_na = dscr('kT_na', [384, NTOK], BF16)
    v_na = dscr('v_na', [NTOK, 384], BF16)
    uT_sg = dscr('uT_sg', [256, NTOK], BF16)
    vn_sg = dscr('vn_sg', [NTOK, 256], BF16)
    rqT = dscr('rqT', [384, NTOK], BF16)
    rkT = dscr('rkT', [384, NTOK], BF16)
    rv_d = dscr('rv', [NTOK, 384], BF16)
    gates_d = dscr('gates', [NTOK, 768], BF16)
    catT = dscr('catT', [D, NTOK], BF16)

    dres = {}

    def DR(name, tile):
        k = (name, tile)
        if k not in dres:
            dres[k] = Res('%s_%s' % (name, tile))
        return dres[k]

    def tile_of_chunk(c):
        return c // 4 if c < NLC else NT // 512

    def chunk_tok(c):
        return c * 128

    with ExitStack() as ges:
        P = Prog(nc, ges)
        gsb = lambda name, shape, dt=F32: ges.enter_context(_sbuf_tensor(name, list(shape), dt))
        cst = gsb('cst', [128, NCONST])
        cst_bf = gsb('cst_bf', [128, 3 * 128], BF16)
        modT = gsb('modT', [128, 72, 2])
        mtab = gsb('mtab', [128, 9, 8, 2])
        r_cst = Res('cst')
        r_cstbf = Res('cst_bf')
        r_mtab = Res('mtab')
        lnb8 = gsb('lnb8', [128, 1])
        lnbe = gsb('lnbe', [128, 1])
        ident_f = cst[:, C_ID:C_ID + 128]
        ident_b = cst_bf[:, 0:128]
        bd_b = cst_bf[:, 128:256]
        ones_b = cst_bf[:, 256:384]

        P.add('sp', lambda e: e.dma_start(out=cst[:], in_=const_in), writes=[r_cst], dma=True)
        P.add('dve', lambda e: e.tensor_copy(out=cst_bf[:, 0:256], in_=cst[:, C_ID:C_ID + 256]), reads=[r_cst], writes=[r_cstbf])
        P.add('dve', lambda e: e.memset(cst_bf[:, 256:384], 1.0 / 1024), reads=[], writes=[r_cstbf])
        P.add('dve', lambda e: e.memset(lnb8[:], LN8), reads=[], writes=[r_cstbf])
        P.add('dve', lambda e: e.memset(lnbe[:], LN_EPS), reads=[], writes=[r_cstbf])
        P.emit_pass()

        def pass_xpose_in(dst):
            with ExitStack() as es:
                conv_issue()
                stg_in = [es.enter_context(_sbuf_tensor('xi%d' % i, [128, D], F32)) for i in range(3)]
                r_in = [Res() for _ in range(3)]
                stg = [es.enter_context(_sbuf_tensor('xo%d' % i, [128, KC, 512], F32)) for i in range(2)]
                r_stg = [Res() for _ in range(2)]
                pss = [es.enter_context(_psum_tensor('pxp%d' % i, [128, 512], F32)) for i in range(4)]
                r_ps = [Res() for _ in range(4)]
                ci = 0
                pi = 0
                for ti, (t0, T, j) in enumerate(tiles):
                    so = stg[ti % 2]
                    rso = r_stg[ti % 2]
                    for s in range(T // 128):
                        tok = t0 + s * 128
                        src = x_in[tok:tok + 128, :] if j == 0 else ctx_in[tok - NT:tok - NT + 128, :]
                        b = ci % 3
                        ci += 1
                        P.add('sp', lambda e, b=b, src=src: e.dma_start(out=stg_in[b][:], in_=src), writes=[r_in[b]], dma=True)
                        for half in range(2):
                            pb = pi % 4
                            pi += 1
                            for q in range(4):
                                kc = half * 4 + q
                                P.add('pe', lambda e, pb=pb, q=q, b=b, kc=kc: e.transpose(pss[pb][:, q * 128:(q + 1) * 128], stg_in[b][:, kc * 128:(kc + 1) * 128], ident_f),
                                      reads=[r_in[b], r_cst], writes=[r_ps[pb]])
                            eng = 'act' if half == 0 else 'dve'
                            dstv = so[:, half * 4:half * 4 + 4, s * 128:(s + 1) * 128]
                            srcv = pss[pb][:].rearrange('p (q t) -> p q t', q=4)
                            if eng == 'act':
                                P.add('act', lambda e, dstv=dstv, srcv=srcv: e.copy(out=dstv, in_=srcv), reads=[r_ps[pb]], writes=[rso])
                            else:
                                P.add('dve', lambda e, dstv=dstv, srcv=srcv: e.tensor_copy(out=dstv, in_=srcv), reads=[r_ps[pb]], writes=[rso])
                    dv = dst.rearrange('(kc p) t -> p kc t', p=128)[:, :, t0:t0 + T]
                    P.add('pool', lambda e, so=so, dv=dv, T=T: e.dma_start(out=dv, in_=so[:, :, 0:T]), reads=[rso], writes=[DR(id(dst), ti)], dma=True)
                P.emit_pass()

        def pass_xpose_out(src):
            with ExitStack() as es:
                stg_in = [es.enter_context(_sbuf_tensor('yi%d' % i, [128, KC, 512], F32)) for i in range(2)]
                r_in = [Res() for _ in range(2)]
                stg = [es.enter_context(_sbuf_tensor('yo%d' % i, [128, D], F32)) for i in range(3)]
                r_stg = [Res() for _ in range(3)]
                pss = [es.enter_context(_psum_tensor('pyp%d' % i, [128, 512], F32)) for i in range(4)]
                r_ps = [Res() for _ in range(4)]
                ci = 0
                pi = 0
                for ti, (t0, T, j) in enumerate(tiles):
                    if j == 1:
                        continue
                    si = stg_in[ti % 2]
                    rsi = r_in[ti % 2]
                    sv = src.rearrange('(kc p) t -> p kc t', p=128)[:, :, t0:t0 + T]
                    P.add('sp', lambda e, si=si, sv=sv: e.dma_start(out=si[:], in_=sv), reads=[DR(id(src), ti)], writes=[rsi], dma=True)
                    for s in range(T // 128):
                        b = ci % 3
                        ci += 1
                        for half in range(2):
                            pb = pi % 4
                            pi += 1
                            for q in range(4):
                                kc = half * 4 + q
                                P.add('pe', lambda e, pb=pb, q=q, si=si, kc=kc, s=s: e.transpose(pss[pb][:, q * 128:(q + 1) * 128], si[:, kc, s * 128:(s + 1) * 128], ident_f),
                                      reads=[rsi, r_cst], writes=[r_ps[pb]])
                            dstv = stg[b][:, half * 512:(half + 1) * 512]
                            if half == 0:
                                P.add('act', lambda e, dstv=dstv, pb=pb: e.copy(out=dstv, in_=pss[pb][:]), reads=[r_ps[pb]], writes=[r_stg[b]])
                            else:
                                P.add('dve', lambda e, dstv=dstv, pb=pb: e.tensor_copy(out=dstv, in_=pss[pb][:]), reads=[r_ps[pb]], writes=[r_stg[b]])
                        tok = t0 + s * 128
                        P.add('pool', lambda e, b=b, tok=tok: e.dma_start(out=y_out[tok:tok + 128, :], in_=stg[b][:]), reads=[r_stg[b]], writes=[DR('y', tok)], dma=True)
                P.emit_pass()

        def pass_mod(l):
            with ExitStack() as es:
                conv_issue()
                sb = lambda name, shape, dt=F32: es.enter_context(_sbuf_tensor(name, list(shape), dt))
                cT = sb('cT', [128, 16])
                sig = sb('csig', [128, 16])
                scT = sb('scT', [128, 16])
                abT = sb('abT', [128, 72])
                nwT = sb('nwT', [128, 48])
                wbuf = [sb('adaw%d' % i, [128, KC, 512]) for i in range(2)]
                r_w = [Res() for _ in range(2)]
                pm = es.enter_context(_psum_tensor('pmod', [128, 72, 2], F32))
                r_pm = Res()
                r_c = Res(); r_sc = Res(); r_ab = Res(); r_nw = Res(); r_mod = Res(); r_sig = Res()
                P.add('sp', lambda e: e.dma_start(out=cT[:], in_=cT_in), writes=[r_c], dma=True)
                P.add('sp', lambda e: e.dma_start(out=abT[:], in_=ada_bT[l]), writes=[r_ab], dma=True)
                P.add('sp', lambda e: e.dma_start(out=nwT[:], in_=norm_wT), writes=[r_nw], dma=True)
                P.add('act', lambda e: e.activation(out=sig[:], in_=cT[:], func=AF.Sigmoid), reads=[r_c], writes=[r_sig])
                P.add('dve', lambda e: e.tensor_tensor(out=scT[:], in0=cT[:], in1=sig[:], op=ALU.mult), reads=[r_c, r_sig], writes=[r_sc])
                wv = ada_w[l].rearrange('(kc p) n -> p kc n', p=128)
                for g in range(18):
                    b = g % 2
                    P.add('sp', lambda e, b=b, g=g: e.dma_start(out=wbuf[b][:], in_=wv[:, :, g * 512:(g + 1) * 512]), writes=[r_w[b]], dma=True)
                    for q in range(4):
                        oc = g * 4 + q
                        for kc in range(KC):
                            P.add('pe', lambda e, b=b, q=q, oc=oc, kc=kc: e.matmul(pm[:, oc, :], lhsT=wbuf[b][:, kc, q * 128:(q + 1) * 128], rhs=scT[:, 2 * kc:2 * kc + 2],
                                                                                  start=(kc == 0), stop=(kc == KC - 1)),
                                  reads=[r_w[b], r_sc], writes=[r_pm])
                P.add('dve', lambda e: e.tensor_tensor(out=modT[:], in0=pm[:], in1=abT[:].unsqueeze(2).broadcast_to([128, 72, 2]), op=ALU.add),
                      reads=[r_pm, r_ab], writes=[r_mod])
                for n in range(3):
                    sc = modT[:, (3 * n + 1) * 8:(3 * n + 2) * 8, :]
                    nw = nwT[:, (l * 3 + n) * 8:(l * 3 + n + 1) * 8].unsqueeze(2).broadcast_to([128, 8, 2])
                    P.add('dve', lambda e, n=n, sc=sc, nw=nw: e.scalar_tensor_tensor(out=mtab[:, n, :, :], in0=sc, scalar=1.0, in1=nw, op0=ALU.add, op1=ALU.mult),
                          reads=[r_mod, r_nw], writes=[r_mtab])
                    sh = modT[:, (3 * n) * 8:(3 * n + 1) * 8, :]
                    P.add('dve', lambda e, n=n, sh=sh: e.tensor_copy(out=mtab[:, 3 + n, :, :], in_=sh), reads=[r_mod], writes=[r_mtab])
                    gt = modT[:, (3 * n + 2) * 8:(3 * n + 3) * 8, :]
                    gm = 1.0 if n == 1 else 0.5
                    P.add('dve', lambda e, n=n, gt=gt, gm=gm: e.tensor_scalar(out=mtab[:, 6 + n, :, :], in0=gt, scalar1=gm, scalar2=None, op0=ALU.mult),
                          reads=[r_mod], writes=[r_mtab])
                P.emit_pass()

        def mt(kind, n, kc, j):
            return mtab[:, kind * 3 + n, kc, j:j + 1]

        wb_cache = {}

        def wb_tensor(key, src):
            if key not in wb_cache:
                rows, cols = src.shape
                wb_cache[key] = (dscr('wb_' + key, [rows, cols], BF16), src, Res('wb_' + key))
            return wb_cache[key]

        conv_jobs = []

        def conv_issue():
            if not conv_jobs:
                return
            for key, src in conv_jobs.pop(0):
                dstt, _, res = wb_tensor(key, src)
                rows, cols = src.shape
                r0 = 0
                while r0 < rows:
                    r1 = min(rows, r0 + 256)
                    c0 = 0
                    while c0 < cols:
                        c1 = min(cols, c0 + 2048)
                        P.add('pool', lambda e, r0=r0, r1=r1, c0=c0, c1=c1, dstt=dstt, src=src: e.dma_start(out=dstt[r0:r1, c0:c1], in_=src[r0:r1, c0:c1]),
                              writes=[res], dma=True)
                        c0 = c1
                    r0 = r1

        def load_w(dst, key, src, rows_chunks, ncols, res_list):
            dstt, _, res = wb_tensor(key, src)
            assert res.last_w is not None, key
            sv = dstt.rearrange('(kc p) n -> p kc n', p=128)
            for kc in range(rows_chunks):
                P.add('sp', lambda e, kc=kc: e.dma_start(out=dst[:, kc, :], in_=sv[:, kc, :]), reads=[res], writes=[res_list[kc]], dma=True)

        class Prep:
            def __init__(self, es, tag):
                sb = lambda name, shape, dt=F32: es.enter_context(_sbuf_tensor(tag + name, list(shape), dt))
                self.hT = [sb('hT%d' % i, [128, KC, 512], BF16) for i in range(2)]
                self.r_hT = [Res() for _ in range(2)]
                self.sq = [sb('sq%d' % i, [128, 512], BF16) for i in range(2)]
                self.r_sq = [Res() for _ in range(2)]
                self.ln = sb('ln', [128, 512])
                self.r_ln = Res()
                self.rstd = sb('rstd', [128, 512])
                self.r_rstd = Res()
                self.tmp = [sb('tmp%d' % i, [128, 512]) for i in range(2)]
                self.r_tmp = [Res() for _ in range(2)]
                self.pst = es.enter_context(_psum_tensor(tag + 'pst', [128, 512], F32))
                self.r_pst = Res()
                self.cnt = 0

            def chunk_a(self, slot, kc, T, xap, xres):
                b = kc % 2
                sq = self.sq[b]
                P.add('act', lambda e: e.activation(out=sq[:, 0:T], in_=xap, func=AF.Square), reads=[xres], writes=[self.r_sq[b]])
                hT = self.hT[slot]
                P.add('pool', lambda e: e.tensor_copy(out=hT[:, kc, 0:T], in_=xap), reads=[xres], writes=[self.r_hT[slot]])

            def chunk_b(self, kc, T):
                b = kc % 2
                sq = self.sq[b]
                P.add('pe', lambda e: e.matmul(self.pst[:, 0:T], lhsT=ones_b, rhs=sq[:, 0:T], start=(kc == 0), stop=(kc == KC - 1)),
                      reads=[self.r_sq[b], r_cstbf], writes=[self.r_pst])

            def chunk_in(self, slot, kc, T, xap, xres):
                self.chunk_a(slot, kc, T, xap, xres)
                self.chunk_b(kc, T)

            def finish_a(self, T):
                P.add('act', lambda e: e.activation(out=self.ln[:, 0:T], in_=self.pst[:, 0:T], func=AF.Ln, bias=RMS_EPS), reads=[self.r_pst], writes=[self.r_ln])
                P.add('act', lambda e: e.activation(out=self.rstd[:, 0:T], in_=self.ln[:, 0:T], func=AF.Exp, scale=-0.5), reads=[self.r_ln], writes=[self.r_rstd])

            def finish_kc(self, slot, kc, T, n, j):
                hT = self.hT[slot]
                b = kc % 2
                tmp = self.tmp[b]
                P.add('dve', lambda e: e.tensor_tensor(out=tmp[:, 0:T], in0=hT[:, kc, 0:T], in1=self.rstd[:, 0:T], op=ALU.mult),
                      reads=[self.r_hT[slot], self.r_rstd], writes=[self.r_tmp[b]])
                P.add('dve', lambda e: e.tensor_scalar(out=hT[:, kc, 0:T], in0=tmp[:, 0:T], scalar1=mt(0, n, kc, j), scalar2=mt(1, n, kc, j), op0=ALU.mult, op1=ALU.add),
                      reads=[self.r_tmp[b], r_mtab], writes=[self.r_hT[slot]])

            def finish(self, slot, T, n, j):
                self.finish_a(T)
                for kc in range(KC):
                    self.finish_kc(slot, kc, T, n, j)

            def steps(self, slot, T, n, j, load_chunk):
                def A(kc):
                    xap, xres = load_chunk(kc)
                    self.chunk_a(slot, kc, T, xap, xres)

                st = [lambda: A(0)]
                for kc in range(1, KC):
                    st.append(lambda kc=kc: (A(kc), self.chunk_b(kc - 1, T)))
                st.append(lambda: self.chunk_b(KC - 1, T))
                st.append(lambda: self.finish_a(T))
                for kc in range(KC):
                    st.append(lambda kc=kc: self.finish_kc(slot, kc, T, n, j))
                return st

        def pass_ffn(l, which, src, dst, skip_ctx=False):
            n = 0 if which == 0 else 2
            with ExitStack() as es:
                sb = lambda name, shape, dt=F32: es.enter_context(_sbuf_tensor(name, list(shape), dt))
                w1 = sb('w1', [128, KC, 2 * DFF], BF16)
                r_w1 = [Res() for _ in range(KC)]
                w2 = sb('w2', [128, FC, D], BF16)
                r_w2 = [Res() for _ in range(FC)]
                act = sb('actb', [128, FC, 512], BF16)
                r_act = [Res() for _ in range(FC)]
                xin = [sb('xin%d' % i, [128, 512]) for i in range(3)]
                r_xin = [Res() for _ in range(3)]
                xrs = [sb('xrs%d' % i, [128, 512]) for i in range(2)]
                r_xrs = [Res() for _ in range(2)]
                xo = [sb('xo%d' % i, [128, 512]) for i in range(2)]
                r_xo = [Res() for _ in range(2)]
                sg = [sb('sg%d' % i, [128, 512], BF16) for i in range(2)]
                r_sg = [Res() for _ in range(2)]
                prep = Prep(es, 'f')
                pg = [es.enter_context(_psum_tensor('pg%d' % i, [128, 512], F32)) for i in range(2)]
                pu = [es.enter_context(_psum_tensor('pu%d' % i, [128, 512], F32)) for i in range(2)]
                po = [es.enter_context(_psum_tensor('po%d' % i, [128, 512], F32)) for i in range(2)]
                r_pg = [Res() for _ in range(2)]
                r_pu = [Res() for _ in range(2)]
                r_po = [Res() for _ in range(2)]
                load_w(w1, 'w1_%d_%d' % (l, which), w1_in[l, which], KC, 2 * DFF, r_w1)
                load_w(w2, 'w2_%d_%d' % (l, which), w2_in[l, which], FC, D, r_w2)
                conv_issue()
                my_tiles = [(ti, t) for ti, t in enumerate(tiles) if not (skip_ctx and t[2] == 1)]
                srcv = src.rearrange('(kc p) t -> p kc t', p=128)
                dstv = dst.rearrange('(kc p) t -> p kc t', p=128)
                cnt = {'xin': 0, 'x2': 0}

                def prepare_steps(k):
                    ti, (t0, T, j) = my_tiles[k]
                    slot = k % 2

                    def load_chunk(kc):
                        b = cnt['xin'] % 3
                        cnt['xin'] += 1
                        P.add('sp', lambda e: e.dma_start(out=xin[b][:, 0:T], in_=srcv[:, kc, t0:t0 + T]), reads=[DR(id(src), ti)], writes=[r_xin[b]], dma=True)
                        return xin[b][:, 0:T], r_xin[b]

                    return prep.steps(slot, T, n, j, load_chunk)

                def prepare(k):
                    for st_ in prepare_steps(k):
                        st_()

                def do_tile(k):
                    ti, (t0, T, j) = my_tiles[k]
                    nsteps = prepare_steps(k + 1) if k + 1 < len(my_tiles) else []
                    slot = k % 2
                    hT = prep.hT[slot]
                    r_hT = prep.r_hT[slot]
                    for mo in range(FC):
                        pb = mo % 2
                        for kc in range(KC):
                            P.add('pe', lambda e, pb=pb, mo=mo, kc=kc: e.matmul(pg[pb][:, 0:T], lhsT=w1[:, kc, mo * 128:(mo + 1) * 128], rhs=hT[:, kc, 0:T],
                                                                               start=(kc == 0), stop=(kc == KC - 1)),
                                  reads=[r_w1[kc], r_hT], writes=[r_pg[pb]])
                        for kc in range(KC):
                            P.add('pe', lambda e, pb=pb, mo=mo, kc=kc: e.matmul(pu[pb][:, 0:T], lhsT=w1[:, kc, DFF + mo * 128:DFF + (mo + 1) * 128], rhs=hT[:, kc, 0:T],
                                                                               start=(kc == 0), stop=(kc == KC - 1)),
                                  reads=[r_w1[kc], r_hT], writes=[r_pu[pb]])
                        P.add('act', lambda e, pb=pb: e.activation(out=sg[pb][:, 0:T], in_=pg[pb][:, 0:T], func=AF.Silu), reads=[r_pg[pb]], writes=[r_sg[pb]])
                        P.add('dve', lambda e, pb=pb, mo=mo: e.tensor_tensor(out=act[:, mo, 0:T], in0=pu[pb][:, 0:T], in1=sg[pb][:, 0:T], op=ALU.mult),
                              reads=[r_pu[pb], r_sg[pb]], writes=[r_act[mo]])
                        if nsteps and mo >= 1:
                            nsteps.pop(0)()
                    while nsteps:
                        nsteps.pop(0)()
                    xsrc = src
                    xsv = xsrc.rearrange('(kc p) t -> p kc t', p=128)
                    def xrs_load(mo_):
                        pb_ = mo_ % 2
                        P.add('pool', lambda e: e.dma_start(out=xrs[pb_][:, 0:T], in_=xsv[:, mo_, t0:t0 + T]), reads=[DR(id(xsrc), ti)], writes=[r_xrs[pb_]], dma=True)

                    xrs_load(0)
                    xrs_load(1)
                    for mo in range(KC):
                        pb = mo % 2
                        for kc in range(FC):
                            P.add('pe', lambda e, pb=pb, mo=mo, kc=kc: e.matmul(po[pb][:, 0:T], lhsT=w2[:, kc, mo * 128:(mo + 1) * 128], rhs=act[:, kc, 0:T],
                                                                               start=(kc == 0), stop=(kc == FC - 1)),
                                  reads=[r_w2[kc], r_act[kc]], writes=[r_po[pb]])
                        P.add('dve', lambda e, pb=pb, mo=mo: e.scalar_tensor_tensor(out=xo[pb][:, 0:T], in0=po[pb][:, 0:T], scalar=mt(2, n, mo, j), in1=xrs[pb][:, 0:T],
                                                                                   op0=ALU.mult, op1=ALU.add),
                              reads=[r_po[pb], r_xrs[pb], r_mtab], writes=[r_xo[pb]])
                        P.add('pool', lambda e, pb=pb, mo=mo: e.dma_start(out=dstv[:, mo, t0:t0 + T], in_=xo[pb][:, 0:T]), reads=[r_xo[pb]], writes=[DR(id(dst), ti)], dma=True)
                        if mo + 2 < KC:
                            xrs_load(mo + 2)

                prepare(0)
                for k in range(len(my_tiles)):
                    do_tile(k)
                P.emit_pass()

        def pass_inproj(l, src):
            with ExitStack() as es:
                sb = lambda name, shape, dt=F32: es.enter_context(_sbuf_tensor(name, list(shape), dt))
                win = sb('win', [128, KC, INX], BF16)
                r_win = [Res() for _ in range(KC)]
                load_w(win, 'win_%d' % l, win_in[l], KC, INX, r_win)
                conv_issue()
                prep = Prep(es, 'i')
                xin = [sb('xin%d' % i, [128, 512]) for i in range(3)]
                r_xin = [Res() for _ in range(3)]
                rC = [sb('rC%d' % i, [128, 512]) for i in range(2)]
                rS = [sb('rS%d' % i, [128, 512]) for i in range(2)]
                r_rope = [Res() for _ in range(2)]
                qkw = sb('qkw', [128, 4])
                qkws = sb('qkws', [128, 2])
                r_qkw = Res()
                lnw = sb('lnw', [128, 256])
                lnb = sb('lnb', [128, 256])
                r_ln = Res()
                P.add('sp', lambda e: e.dma_start(out=qkw[:], in_=qkn_in), writes=[r_qkw], dma=True)
                P.add('dve', lambda e: e.tensor_scalar(out=qkws[:, 0:1], in0=qkw[:, 2 * l:2 * l + 1], scalar1=0.125, scalar2=None, op0=ALU.mult), reads=[r_qkw], writes=[r_qkw])
                P.add('dve', lambda e: e.tensor_copy(out=qkws[:, 1:2], in_=qkw[:, 2 * l + 1:2 * l + 2]), reads=[r_qkw], writes=[r_qkw])
                P.add('sp', lambda e: e.dma_start(out=lnw[:], in_=lnw_in[:, l * 256:(l + 1) * 256]), writes=[r_ln], dma=True)
                P.add('sp', lambda e: e.dma_start(out=lnb[:], in_=lnb_in[:, l * 256:(l + 1) * 256]), writes=[r_ln], dma=True)
                st_q = [sb('stq%d' % i, [128, 3, 512], BF16) for i in range(2)]
                st_k = [sb('stk%d' % i, [128, 3, 512], BF16) for i in range(2)]
                st_u = [sb('stu%d' % i, [128, 2, 512], BF16) for i in range(2)]
                st_rq = [sb('strq%d' % i, [128, 3, 512], BF16) for i in range(2)]
                st_rk = [sb('strk%d' % i, [128, 3, 512], BF16) for i in range(2)]
                r_stq = [Res() for _ in range(2)]; r_stk = [Res() for _ in range(2)]; r_stu = [Res() for _ in range(2)]
                r_strq = [Res() for _ in range(2)]; r_strk = [Res() for _ in range(2)]
                st_v = [sb('stv%d' % i, [128, 4, 384], BF16) for i in range(2)]
                st_rv = [sb('strv%d' % i, [128, 4, 384], BF16) for i in range(2)]
                st_g = [sb('stg%d' % i, [128, 4, 768], BF16) for i in range(2)]
                st_vn = [sb('stvn%d' % i, [128, 4, 256], BF16) for i in range(2)]
                r_stv = [[Res() for _ in range(4)] for _ in range(2)]; r_strv = [[Res() for _ in range(4)] for _ in range(2)]; r_stg = [[Res() for _ in range(4)] for _ in range(2)]; r_stvn = [[Res() for _ in range(4)] for _ in range(2)]
                sqb = [sb('qsq%d' % i, [128, 512], BF16) for i in range(2)]
                r_sqb = [Res() for _ in range(2)]
                lnq = [sb('lnq%d' % i, [128, 512]) for i in range(2)]
                r_lnq = [Res() for _ in range(2)]
                rsq = [sb('rsq%d' % i, [128, 512]) for i in range(2)]
                r_rsq = [Res() for _ in range(2)]
                t1 = [sb('t1%d' % i, [128, 512]) for i in range(2)]
                t2 = [sb('t2%d' % i, [128, 512]) for i in range(2)]
                r_t1 = [Res() for _ in range(2)]; r_t2 = [Res() for _ in range(2)]
                gv = [sb('gv%d' % i, [128, 256]) for i in range(2)]
                r_gv = [Res() for _ in range(2)]
                gsq = sb('gsq', [128, 256]); r_gsq = Res()
                cen = sb('cen', [128, 256]); r_cen = Res()
                sm = sb('sm', [128, 24]); r_sm = Res()
                pf = [es.enter_context(_psum_tensor('pf%d' % i, [128, 512], F32)) for i in range(4)]
                r_pf = [Res() for _ in range(4)]
                pt = [es.enter_context(_psum_tensor('pt%d' % i, [128, 512], F32)) for i in range(3)]
                r_pt = [Res() for _ in range(3)]
                srcv = src.rearrange('(kc p) t -> p kc t', p=128)
                cnt = {'xin': 0, 'pf': 0, 'pt': 0, 'w': 0}

                def prepare_steps(k):
                    ti, (t0, T, j) = k, tiles[k]
                    slot = k % 2

                    def load_chunk(kc):
                        b = cnt['xin'] % 3
                        cnt['xin'] += 1
                        P.add('sp', lambda e: e.dma_start(out=xin[b][:, 0:T], in_=srcv[:, kc, t0:t0 + T]), reads=[DR(id(src), ti)], writes=[r_xin[b]], dma=True)
                        return xin[b][:, 0:T], r_xin[b]

                    return prep.steps(slot, T, 1, j, load_chunk)

                def prepare(k):
                    for st_ in prepare_steps(k):
                        st_()

                nsteps = []

                def pop_step():
                    if nsteps:
                        nsteps.pop(0)()

                def fm_mm(colbase, c, T, hT, r_hT):
                    pb = cnt['pf'] % 4
                    cnt['pf'] += 1
                    c0 = colbase + c * 128
                    for kc in range(KC):
                        P.add('pe', lambda e, kc=kc: e.matmul(pf[pb][:, 0:T], lhsT=win[:, kc, c0:c0 + 128], rhs=hT[:, kc, 0:T], start=(kc == 0), stop=(kc == KC - 1)),
                              reads=[r_win[kc], r_hT], writes=[r_pf[pb]])
                    pop_step()
                    return pb

                def do_tile(k):
                    ti, (t0, T, j) = k, tiles[k]
                    slot = k % 2
                    hT = prep.hT[slot]
                    r_hT = prep.r_hT[slot]
                    sl = k % 2
                    P.add('sp', lambda e: e.dma_start(out=rC[sl][:, 0:T], in_=ropeC[:, t0:t0 + T]), writes=[r_rope[sl]], dma=True)
                    P.add('sp', lambda e: e.dma_start(out=rS[sl][:, 0:T], in_=ropeS[:, t0:t0 + T]), writes=[r_rope[sl]], dma=True)
                    def qk_a(colbase, c):
                        A = fm_mm(colbase, c, T, hT, r_hT)
                        w = cnt['w'] % 2
                        cnt['w'] += 1
                        P.add('act', lambda e: e.activation(out=sqb[w][:, 0:T], in_=pf[A][:, 0:T], func=AF.Square), reads=[r_pf[A]], writes=[r_sqb[w]])
                        return A, w

                    def qk_b(c, stg, r_stg_, wi, A, w):
                        B = cnt['pf'] % 4
                        cnt['pf'] += 1
                        P.add('pe', lambda e: e.matmul(pf[B][:, 0:T], lhsT=bd_b, rhs=sqb[w][:, 0:T], start=True, stop=True), reads=[r_sqb[w], r_cstbf], writes=[r_pf[B]])
                        P.add('act', lambda e: e.activation(out=lnq[w][:, 0:T], in_=pf[B][:, 0:T], func=AF.Ln, bias=RMS_EPS), reads=[r_pf[B]], writes=[r_lnq[w]])
                        P.add('act', lambda e: e.activation(out=rsq[w][:, 0:T], in_=lnq[w][:, 0:T], func=AF.Exp, scale=-0.5), reads=[r_lnq[w]], writes=[r_rsq[w]])
                        P.add('dve', lambda e: e.scalar_tensor_tensor(out=stg[:, c, 0:T], in0=pf[A][:, 0:T], scalar=qkws[:, wi:wi + 1], in1=rsq[w][:, 0:T], op0=ALU.mult, op1=ALU.mult),
                              reads=[r_pf[A], r_rsq[w], r_qkw], writes=[r_stg_])

                    items = [(0, c, st_q[sl], r_stq[sl], 0) for c in range(3)] + [(384, c, st_k[sl], r_stk[sl], 1) for c in range(3)]
                    prev = None
                    for (colbase, c, stg, r_stg_, wi) in items:
                        A, w = qk_a(colbase, c)
                        if prev is not None:
                            qk_b(*prev)
                        prev = (c, stg, r_stg_, wi, A, w)
                    qk_b(*prev)
                    def fm_store(stg, r_stg_, dten):
                        dv = dten.rearrange('(c p) t -> p c t', p=128)[:, :, t0:t0 + T]
                        P.add('pool', lambda e: e.dma_start(out=dv, in_=stg[:, :, 0:T]), reads=[r_stg_], writes=[DR(id(dten), ti)], dma=True)

                    fm_store(st_q[sl], r_stq[sl], qT_na)
                    fm_store(st_k[sl], r_stk[sl], kT_na)
                    for c in range(2):
                        A = fm_mm(1152, c, T, hT, r_hT)
                        P.add('act', lambda e, A=A, c=c: e.activation(out=st_u[sl][:, c, 0:T], in_=pf[A][:, 0:T], func=AF.Gelu_apprx_tanh), reads=[r_pf[A]], writes=[r_stu[sl]])
                    fm_store(st_u[sl], r_stu[sl], uT_sg)
                    for (colbase, pbase, stg, r_stg_) in ((1664, 3584, st_rq[sl], r_strq[sl]), (2048, 3968, st_rk[sl], r_strk[sl])):
                        for c in range(3):
                            A = fm_mm(colbase, c, T, hT, r_hT)
                            A2 = fm_mm(pbase, c, T, hT, r_hT)
                            w = cnt['w'] % 2
                            cnt['w'] += 1
                            P.add('dve', lambda e, A=A, w=w: e.tensor_tensor(out=t1[w][:, 0:T], in0=pf[A][:, 0:T], in1=rC[sl][:, 0:T], op=ALU.mult), reads=[r_pf[A], r_rope[sl]], writes=[r_t1[w]])
                            P.add('dve', lambda e, A2=A2, w=w: e.tensor_tensor(out=t2[w][:, 0:T], in0=pf[A2][:, 0:T], in1=rS[sl][:, 0:T], op=ALU.mult), reads=[r_pf[A2], r_rope[sl]], writes=[r_t2[w]])
                            P.add('pool', lambda e, w=w, c=c, stg=stg: e.tensor_tensor(out=stg[:, c, 0:T], in0=t1[w][:, 0:T], in1=t2[w][:, 0:T], op=ALU.add), reads=[r_t1[w], r_t2[w]], writes=[r_stg_])
                        fm_store(stg, r_stg_, rqT if colbase == 1664 else rkT)
                    def do_sub(s):
                        def tm_mm(c0, ncol):
                            pb = cnt['pt'] % 3
                            cnt['pt'] += 1
                            for kc in range(KC):
                                P.add('pe', lambda e, kc=kc: e.matmul(pt[pb][:, 0:ncol], lhsT=hT[:, kc, s * 128:(s + 1) * 128], rhs=win[:, kc, c0:c0 + ncol], start=(kc == 0), stop=(kc == KC - 1)),
                                      reads=[r_win[kc], r_hT], writes=[r_pt[pb]])
                            return pb
                        A = tm_mm(768, 384)
                        P.add('act', lambda e, A=A: e.copy(out=st_v[sl][:, s, :], in_=pt[A][:, 0:384]), reads=[r_pt[A]], writes=[r_stv[sl][s]])
                        A = tm_mm(2432, 384)
                        P.add('dve', lambda e, A=A: e.tensor_copy(out=st_rv[sl][:, s, :], in_=pt[A][:, 0:384]), reads=[r_pt[A]], writes=[r_strv[sl][s]])
                        A = tm_mm(2816, 384)
                        P.add('act', lambda e, A=A: e.activation(out=st_g[sl][:, s, 0:384], in_=pt[A][:, 0:384], func=AF.Silu), reads=[r_pt[A]], writes=[r_stg[sl][s]])
                        A = tm_mm(3200, 384)
                        P.add('act', lambda e, A=A: e.activation(out=st_g[sl][:, s, 384:768], in_=pt[A][:, 0:384], func=AF.Silu), reads=[r_pt[A]], writes=[r_stg[sl][s]])
                        A = tm_mm(1408, 256)
                        g = cnt['w'] % 2
                        cnt['w'] += 1
                        P.add('act', lambda e, A=A, g=g: e.activation(out=gv[g][:], in_=pt[A][:, 0:256], func=AF.Gelu_apprx_tanh), reads=[r_pt[A]], writes=[r_gv[g]])
                        g3 = lambda ap: ap.rearrange('p (g c) -> p g c', g=4)
                        bc = lambda ap: ap.unsqueeze(2).broadcast_to([128, 4, 64])
                        P.add('dve', lambda e, g=g: e.tensor_reduce(out=sm[:, 0:4], in_=g3(gv[g][:]), axis=AX.X, op=ALU.add), reads=[r_gv[g]], writes=[r_sm])
                        P.add('dve', lambda e, g=g: e.tensor_tensor(out=gsq[:], in0=gv[g][:], in1=gv[g][:], op=ALU.mult), reads=[r_gv[g]], writes=[r_gsq])
                        P.add('dve', lambda e: e.tensor_reduce(out=sm[:, 4:8], in_=g3(gsq[:]), axis=AX.X, op=ALU.add), reads=[r_gsq], writes=[r_sm])
                        P.add('dve', lambda e: e.tensor_scalar(out=sm[:, 8:12], in0=sm[:, 0:4], scalar1=1.0 / 64, scalar2=None, op0=ALU.mult), reads=[r_sm], writes=[r_sm])
                        P.add('dve', lambda e: e.tensor_tensor(out=sm[:, 12:16], in0=sm[:, 8:12], in1=sm[:, 8:12], op=ALU.mult), reads=[r_sm], writes=[r_sm])
                        P.add('dve', lambda e: e.scalar_tensor_tensor(out=sm[:, 16:20], in0=sm[:, 4:8], scalar=1.0 / 64, in1=sm[:, 12:16], op0=ALU.mult, op1=ALU.subtract), reads=[r_sm], writes=[r_sm])
                        P.add('act', lambda e: e.activation(out=sm[:, 16:20], in_=sm[:, 16:20], func=AF.Sqrt, bias=LN_EPS), reads=[r_sm], writes=[r_sm])
                        P.add('dve', lambda e: e.reciprocal(out=sm[:, 20:24], in_=sm[:, 16:20]), reads=[r_sm], writes=[r_sm])
                        P.add('dve', lambda e, g=g: e.tensor_tensor(out=g3(cen[:]), in0=g3(gv[g][:]), in1=bc(sm[:, 8:12]), op=ALU.subtract), reads=[r_gv[g], r_sm], writes=[r_cen])
                        P.add('dve', lambda e: e.tensor_tensor(out=g3(cen[:]), in0=g3(cen[:]), in1=bc(sm[:, 20:24]), op=ALU.mult), reads=[r_cen, r_sm], writes=[r_cen])
                        P.add('pool', lambda e: e.tensor_tensor(out=cen[:], in0=cen[:], in1=lnw[:], op=ALU.mult), reads=[r_cen, r_ln], writes=[r_cen])
                        P.add('pool', lambda e: e.tensor_tensor(out=st_vn[sl][:, s, :], in0=cen[:], in1=lnb[:], op=ALU.add), reads=[r_cen, r_ln], writes=[r_stvn[sl][s]])
                        for (stg, r_stg_, dten) in ((st_v[sl], r_stv[sl][s], v_na), (st_rv[sl], r_strv[sl][s], rv_d), (st_g[sl], r_stg[sl][s], gates_d), (st_vn[sl], r_stvn[sl][s], vn_sg)):
                            P.add('pool', lambda e, stg=stg, dten=dten: e.dma_start(out=dten[t0 + s * 128:t0 + (s + 1) * 128, :], in_=stg[:, s, :]), reads=[r_stg_], writes=[DR(id(dten), ti)], dma=True)
                    for s_ in range(T // 128):
                        do_sub(s_)

                prepare(0)
                for k in range(len(tiles)):
                    if k + 1 < len(tiles):
                        nsteps.extend(prepare_steps(k + 1))
                    do_tile(k)
                    while nsteps:
                        nsteps.pop(0)()
                P.emit_pass()

        def pass_outproj(l, src, dst, skip_ctx):
            with ExitStack() as es:
                sb = lambda name, shape, dt=F32: es.enter_context(_sbuf_tensor(name, list(shape), dt))
                wo = sb('wo', [128, KC, D], BF16)
                r_wo = [Res() for _ in range(KC)]
                load_w(wo, 'wo_%d' % l, wout_in[l], KC, D, r_wo)
                conv_issue()
                ct = [sb('ct%d' % i, [128, KC, 512], BF16) for i in range(2)]
                r_ct = [Res() for _ in range(2)]
                xin = [sb('xin%d' % i, [128, 512]) for i in range(3)]
                r_xin = [Res() for _ in range(3)]
                xo = [sb('xo%d' % i, [128, 512]) for i in range(3)]
                r_xo = [Res() for _ in range(3)]
                po = [es.enter_context(_psum_tensor('po%d' % i, [128, 512], F32)) for i in range(3)]
                r_po = [Res() for _ in range(3)]
                srcv = src.rearrange('(kc p) t -> p kc t', p=128)
                dstv = dst.rearrange('(kc p) t -> p kc t', p=128)
                catv = catT.rearrange('(kc p) t -> p kc t', p=128)
                cnt = {'i': 0}

                def do_tile(ti):
                    t0, T, j = tiles[ti]
                    sl = ti % 2
                    P.add('sp', lambda e: e.dma_start(out=ct[sl][:, :, 0:T], in_=catv[:, :, t0:t0 + T]), reads=[DR(id(catT), ti)], writes=[r_ct[sl]], dma=True)
                    for mo in range(KC):
                        b = cnt['i'] % 3
                        cnt['i'] += 1
                        P.add('sp', lambda e, b=b, mo=mo: e.dma_start(out=xin[b][:, 0:T], in_=srcv[:, mo, t0:t0 + T]), reads=[DR(id(src), ti)], writes=[r_xin[b]], dma=True)
                        for kc in range(KC):
                            P.add('pe', lambda e, b=b, mo=mo, kc=kc: e.matmul(po[b][:, 0:T], lhsT=wo[:, kc, mo * 128:(mo + 1) * 128], rhs=ct[sl][:, kc, 0:T], start=(kc == 0), stop=(kc == KC - 1)),
                                  reads=[r_wo[kc], r_ct[sl]], writes=[r_po[b]])
                        P.add('dve', lambda e, b=b, mo=mo: e.scalar_tensor_tensor(out=xo[b][:, 0:T], in0=po[b][:, 0:T], scalar=mt(2, 1, mo, j), in1=xin[b][:, 0:T], op0=ALU.mult, op1=ALU.add),
                              reads=[r_po[b], r_xin[b], r_mtab], writes=[r_xo[b]])
                        P.add('pool', lambda e, b=b, mo=mo: e.dma_start(out=dstv[:, mo, t0:t0 + T], in_=xo[b][:, 0:T]), reads=[r_xo[b]], writes=[DR(id(dst), ti)], dma=True)

                for ti in range(len(tiles)):
                    if skip_ctx and tiles[ti][2] == 1:
                        continue
                    do_tile(ti)
                P.emit_pass()

        def pass_mixer(l, last):
            with ExitStack() as es:
                conv_issue()
                sb = lambda name, shape, dt=F32: es.enter_context(_sbuf_tensor(name, list(shape), dt))
                E = sb('E', [128, NV, 6, 5, 128], BF16)
                r_E = Res()
                Sst = sb('Sst', [128, NCH, 2, 3, 64], BF16)
                r_Sst = [[Res(), Res()] for _ in range(NCH)]
                dl = sb('dl', [128, 12]); e1 = sb('e1', [128, 12]); lg = sb('lg', [128, 12]); nlg = sb('nlg', [128, 12])
                lgsel = sb('lgsel', [128, 2, 3]); g128 = sb('g128', [128, 2, 3]); te = sb('te', [128, 2, 6])
                fs = sb('fs', [128, 2, 3, 128]); fsm = sb('fsm', [128, 2, 6, 128]); Dm = sb('Dm', [128, 2, 6, 128])
                r_tab = Res()
                st = sb('st', [128, 2, 3, 64])
                r_st = [Res(), Res()]
                sgw = sb('sgw', [128, 4, 128], BF16); sgbt = sb('sgbt', [128, 2, 128]); gnw = sb('gnw', [128, 384])
                r_sgt = Res()
                pS = es.enter_context(_psum_tensor('pS', [128, 1024], F32)); r_pS = Res()
                pS2 = es.enter_context(_psum_tensor('pS2', [128, 1024], F32)); r_pS2 = Res()
                pO = es.enter_context(_psum_tensor('pO', [128, 512], F32)); r_pO = Res()
                pRo = es.enter_context(_psum_tensor('pRo', [128, 1024], F32)); r_pRo = Res()
                pT = es.enter_context(_psum_tensor('pT', [128, 1024], BF16)); r_pT = Res()
                pG = pRo[:, 768:1024]; r_pG = Res()
                pKV = pS2[:, 0:512]; r_pKV = r_pS2

                P.add('sp', lambda e: e.dma_start(out=dl[:], in_=dlog_in[:, l * 12:(l + 1) * 12]), writes=[r_tab], dma=True)
                P.add('act', lambda e: e.activation(out=e1[:], in_=dl[:], func=AF.Exp, scale=-1.0), reads=[r_tab], writes=[r_tab])
                P.add('act', lambda e: e.activation(out=nlg[:], in_=e1[:], func=AF.Ln, bias=1.0), reads=[r_tab], writes=[r_tab])
                P.add('dve', lambda e: e.tensor_scalar(out=lg[:], in0=nlg[:], scalar1=-1.0, scalar2=None, op0=ALU.mult), reads=[r_tab], writes=[r_tab])
                for d_ in range(2):
                    v2 = lg[:, d_ * 6:(d_ + 1) * 6].rearrange('p (r two) -> p r two', two=2)
                    P.add('dve', lambda e, d_=d_, v2=v2: e.tensor_copy(out=lgsel[0:64, d_, :], in_=v2[0:64, :, 0]), reads=[r_tab], writes=[r_tab])
                    P.add('dve', lambda e, d_=d_, v2=v2: e.tensor_copy(out=lgsel[64:128, d_, :], in_=v2[64:128, :, 1]), reads=[r_tab], writes=[r_tab])
                P.add('act', lambda e: e.activation(out=g128[:], in_=lgsel[:], func=AF.Exp, scale=128.0), reads=[r_tab], writes=[r_tab])
                P.add('act', lambda e: e.activation(out=te[:, 0, :], in_=lg[:, 0:6], func=AF.Exp, scale=cst[:, C_PREV:C_PREV + 1]), reads=[r_tab, r_cst], writes=[r_tab])
                P.add('act', lambda e: e.activation(out=te[:, 1, :], in_=lg[:, 6:12], func=AF.Exp, scale=cst[:, C_PCOL:C_PCOL + 1]), reads=[r_tab, r_cst], writes=[r_tab])
                for pr in range(3):
                    P.add('act', lambda e, pr=pr: e.activation(out=fs[:, 0, pr, :], in_=cst[:, C_IP1:C_IP1 + 128], func=AF.Exp, scale=lgsel[:, 0, pr:pr + 1], bias=lnb8[:, 0:1]),
                          reads=[r_tab, r_cst], writes=[r_tab])
                    P.add('act', lambda e, pr=pr: e.activation(out=fs[:, 1, pr, :], in_=cst[:, C_REV:C_REV + 128], func=AF.Exp, scale=lgsel[:, 1, pr:pr + 1], bias=lnb8[:, 0:1]),
                          reads=[r_tab, r_cst], writes=[r_tab])
                P.add('dve', lambda e: e.memset(fsm[:], 0.0), writes=[r_tab])
                for d_ in range(2):
                    for h in range(6):
                        r0_ = (h % 2) * 64
                        P.add('dve', lambda e, d_=d_, h=h, r0_=r0_: e.tensor_copy(out=fsm[r0_:r0_ + 64, d_, h, :], in_=fs[r0_:r0_ + 64, d_, h // 2, :]), reads=[r_tab], writes=[r_tab])
                for h in range(6):
                    P.add('act', lambda e, h=h: e.activation(out=Dm[:, 0, h, :], in_=cst[:, C_DIFF:C_DIFF + 128], func=AF.Exp, scale=lg[:, h:h + 1], bias=lnb8[:, 0:1]),
                          reads=[r_tab, r_cst], writes=[r_tab])
                    P.add('act', lambda e, h=h: e.activation(out=Dm[:, 1, h, :], in_=cst[:, C_DIFF:C_DIFF + 128], func=AF.Exp, scale=nlg[:, 6 + h:7 + h], bias=lnb8[:, 0:1]),
                          reads=[r_tab, r_cst], writes=[r_tab])
                mFb = cst[:, C_MF:C_MF + 128].unsqueeze(1).broadcast_to([128, 6, 128])
                mBb = cst[:, C_MB:C_MB + 128].unsqueeze(1).broadcast_to([128, 6, 128])
                P.add('dve', lambda e: e.tensor_tensor(out=Dm[:, 0, :, :], in0=Dm[:, 0, :, :], in1=mFb, op=ALU.mult), reads=[r_tab, r_cst], writes=[r_tab])
                P.add('dve', lambda e: e.tensor_tensor(out=Dm[:, 1, :, :], in0=Dm[:, 1, :, :], in1=mBb, op=ALU.mult), reads=[r_tab, r_cst], writes=[r_tab])
                bst = [sb('bst%d' % i, [128, 5, 128]) for i in range(2)]
                r_bst = [Res(), Res()]
                for v in range(NV):
                    for h in range(6):
                        b = (v * 6 + h) % 2
                        P.add('sp', lambda e, b=b, v=v, h=h: e.dma_start(out=bst[b][:], in_=bias_in[l, v, h]), writes=[r_bst[b]], dma=True)
                        P.add('act', lambda e, b=b, v=v, h=h: e.activation(out=E[:, v, h, :, :], in_=bst[b][:], func=AF.Exp), reads=[r_bst[b]], writes=[r_E])
                P.add('pool', lambda e: e.dma_start(out=sgw[:], in_=sgw_in[l].rearrange('g q p -> q g p')), writes=[r_sgt], dma=True)
                P.add('sp', lambda e: e.dma_start(out=sgbt[:], in_=sgb_in[l].rearrange('g p f -> p g f')), writes=[r_sgt], dma=True)
                P.add('sp', lambda e: e.dma_start(out=gnw[:], in_=gnw_in[:, l * 384:(l + 1) * 384]), writes=[r_sgt], dma=True)

                qv = lambda ten: ten.rearrange('(c p) t -> p c t', p=128)
                rkb = [sb('rkb%d' % i, [128, 3, 128], BF16) for i in range(2)]
                rvb = [sb('rvb%d' % i, [128, 384], BF16) for i in range(2)]
                r_rkb = [Res(), Res()]; r_rvb = [Res(), Res()]
                kte = [sb('kte%d' % i, [128, 384], BF16) for i in range(2)]
                r_kte = [Res(), Res()]
                P.add('dve', lambda e: e.memset(st[:], 0.0), writes=[r_st[0], r_st[1]])
                order_f = [NLC, NLC + 1] + list(range(NLC))
                order_b = [NLC + 1, NLC] + list(range(NLC - 1, -1, -1))
                sc = {'i': 0}

                def scan_step(dr, c):
                    i = sc['i'] % 2
                    sc['i'] += 1
                    tok = chunk_tok(c)
                    tl = tile_of_chunk(c)
                    P.add('act', lambda e: e.copy(out=Sst[:, c, dr, :, :], in_=st[:, dr, :, :]), reads=[r_st[dr]], writes=[r_Sst[c][dr]])
                    P.add('sp', lambda e: e.dma_start(out=rkb[i][:], in_=qv(rkT)[:, :, tok:tok + 128]), reads=[DR(id(rkT), tl)], writes=[r_rkb[i]], dma=True)
                    P.add('sp', lambda e: e.dma_start(out=rvb[i][:], in_=rv_d[tok:tok + 128, :]), reads=[DR(id(rv_d), tl)], writes=[r_rvb[i]], dma=True)
                    for pr in range(3):
                        P.add('pe', lambda e, pr=pr: e.transpose(pT[:, pr * 128:(pr + 1) * 128], rkb[i][:, pr, :], ident_b), reads=[r_rkb[i], r_cstbf], writes=[r_pT])
                    P.add('dve', lambda e: e.tensor_tensor(out=kte[i][:].rearrange('p (h d) -> p h d', h=6), in0=pT[:, 0:384].rearrange('p (h d) -> p h d', h=6),
                                                           in1=te[:, dr, :].unsqueeze(2).broadcast_to([128, 6, 64]), op=ALU.mult),
                          reads=[r_pT, r_tab], writes=[r_kte[i]])
                    for pr in range(3):
                        P.add('pe', lambda e, pr=pr: e.matmul(pS2[:, pr * 128:(pr + 1) * 128], lhsT=kte[i][:, pr * 128:(pr + 1) * 128], rhs=rvb[i][:, pr * 128:(pr + 1) * 128], start=True, stop=True),
                              reads=[r_kte[i], r_rvb[i]], writes=[r_pKV])
                    P.add('dve', lambda e: e.tensor_tensor(out=st[:, dr, :, :], in0=st[:, dr, :, :], in1=g128[:, dr, :].unsqueeze(2).broadcast_to([128, 3, 64]), op=ALU.mult),
                          reads=[r_st[dr], r_tab], writes=[r_st[dr]])
                    kv3 = pS2[:, 0:384].rearrange('p (r c) -> p r c', r=3)
                    P.add('dve', lambda e: e.tensor_tensor(out=st[0:64, dr, :, :], in0=st[0:64, dr, :, :], in1=kv3[0:64, :, 0:64], op=ALU.add), reads=[r_st[dr], r_pKV], writes=[r_st[dr]])
                    P.add('dve', lambda e: e.tensor_tensor(out=st[64:128, dr, :, :], in0=st[64:128, dr, :, :], in1=kv3[64:128, :, 64:128], op=ALU.add), reads=[r_st[dr], r_pKV], writes=[r_st[dr]])

                if 'scan' in MIXDBG:
                    for c in order_f:
                        scan_step(0, c)
                    for c in order_b:
                        scan_step(1, c)

                NSLOT = 8
                kslot = [sb('ks%d' % i, [128, 3, 128], BF16) for i in range(NSLOT + 2)]
                vslot = [sb('vs%d' % i, [128, 6, 65], BF16) for i in range(NSLOT + 2)]
                r_slot = [Res() for _ in range(NSLOT + 2)]
                slot_chunk = [None] * (NSLOT + 2)
                for i in range(NSLOT + 2):
                    P.add('pool', lambda e, i=i: e.memset(vslot[i][:], 1.0), writes=[r_slot[i]])
                dbl = lambda name, shape, dt=BF16: [sb(name + '%d' % i, shape, dt) for i in range(2)]
                qna = dbl('qna', [128, 3, 128]); uu = dbl('uu', [128, 2, 128]); vnb = dbl('vnb', [128, 256])
                rqb = dbl('rqb', [128, 3, 128]); rkc = dbl('rkc', [128, 3, 128]); rvc = dbl('rvc', [128, 384]); gtb = dbl('gtb', [128, 768])
                r_ld = [Res(), Res()]
                expS = dbl('expS', [128, 7, 128]); r_expS = [Res(), Res()]
                qfs = dbl('qfs', [128, 2, 6, 128]); r_qfs = [Res(), Res()]
                SD = dbl('SD', [128, 2, 6, 128]); r_SD = [Res(), Res()]
                natok = dbl('natok', [128, 384]); r_natok = [Res(), Res()]
                rettok = dbl('rettok', [128, 384]); r_rettok = [Res(), Res()]
                catst = dbl('catst', [128, 8, 128]); r_catst = [Res(), Res()]
                rec = dbl('rec', [128, 6], F32); r_rec = [Res(), Res()]
                sgt = dbl('sgtmp', [128, 2, 128], F32); r_sgtmp = [Res(), Res()]
                osb = dbl('osb', [128, 768], F32); r_osb = [Res(), Res()]
                osq = dbl('osq', [128, 768], F32); r_osq = [Res(), Res()]
                gsm = dbl('gsm', [128, 72], F32); r_gsm = [Res(), Res()]
                g2 = dbl('g2', [128, 768], F32); r_g2 = [Res(), Res()]
                g3t = dbl('g3t', [128, 384], F32); r_g3 = [Res(), Res()]

                def ensure_slot(kc_):
                    if kc_ >= NLC:
                        s = NSLOT + (kc_ - NLC)
                    else:
                        s = kc_ % NSLOT
                    if slot_chunk[s] != kc_:
                        slot_chunk[s] = kc_
                        tok = chunk_tok(kc_)
                        tl = tile_of_chunk(kc_)
                        P.add('sp', lambda e: e.dma_start(out=kslot[s][:], in_=qv(kT_na)[:, :, tok:tok + 128]), reads=[DR(id(kT_na), tl)], writes=[r_slot[s]], dma=True)
                        P.add('sp', lambda e: e.dma_start(out=vslot[s][:, :, 0:64], in_=v_na[tok:tok + 128, :].rearrange('p (h d) -> p h d', h=6)), reads=[DR(id(v_na), tl)], writes=[r_slot[s]], dma=True)
                    return s

                def do_chunk(c, idx):
                    cb = idx % 2
                    tok = chunk_tok(c)
                    tl = tile_of_chunk(c)
                    is_ctx = c >= NLC
                    for (dst_, ten, fm) in ((qna[cb], qT_na, True), (uu[cb], uT_sg, True), (rqb[cb], rqT, True), (rkc[cb], rkT, True)):
                        P.add('sp', lambda e, dst_=dst_, ten=ten: e.dma_start(out=dst_[:], in_=qv(ten)[:, :, tok:tok + 128]), reads=[DR(id(ten), tl)], writes=[r_ld[cb]], dma=True)
                    for (dst_, ten) in ((vnb[cb], vn_sg), (rvc[cb], rv_d), (gtb[cb], gates_d)):
                        P.add('sp', lambda e, dst_=dst_, ten=ten: e.dma_start(out=dst_[:], in_=ten[tok:tok + 128, :]), reads=[DR(id(ten), tl)], writes=[r_ld[cb]], dma=True)
                    if is_ctx:
                        kchunks = []
                    else:
                        kchunks = [base_of[c] + m for m in range(5)]
                    kchunks = kchunks + [NLC, NLC + 1]
                    slots = [ensure_slot(kc_) for kc_ in kchunks]
                    nb = len(slots)
                    var = None if is_ctx else var_of[c]
                    def sec_na(pend):
                        pSb = [pS, pS2]
                        r_pSb = [r_pS, r_pS2]

                        def scores(h):
                            buf, pc, r0 = h % 2, h // 2, (h % 2) * 64
                            for bi, s in enumerate(slots):
                                P.add('pe', lambda e, bi=bi, s=s: e.matmul(pSb[buf][:, bi * 128:(bi + 1) * 128], lhsT=kslot[s][r0:r0 + 64, pc, :], rhs=qna[cb][r0:r0 + 64, pc, :], start=True, stop=True),
                                      reads=[r_slot[s], r_ld[cb]], writes=[r_pSb[buf]])

                        def soft(h):
                            buf = eb = h % 2
                            P.add('act', lambda e: e.activation(out=expS[eb][:, 0:nb, :], in_=pSb[buf][:, 0:nb * 128].rearrange('p (b q) -> p b q', b=nb), func=AF.Exp),
                                  reads=[r_pSb[buf]], writes=[r_expS[eb]])
                            if not is_ctx:
                                P.add('dve', lambda e: e.tensor_tensor(out=expS[eb][:, 0:5, :], in0=expS[eb][:, 0:5, :], in1=E[:, var, h, :, :], op=ALU.mult),
                                      reads=[r_expS[eb], r_E], writes=[r_expS[eb]])

                        def pv(h):
                            eb = h % 2
                            for bi, s in enumerate(slots):
                                P.add('pe', lambda e, bi=bi, s=s: e.matmul(pO[:, h * 65:(h + 1) * 65], lhsT=expS[eb][:, bi, :], rhs=vslot[s][:, h, :], start=(bi == 0), stop=(bi == nb - 1)),
                                      reads=[r_expS[eb], r_slot[s]], writes=[r_pO])

                        scores(0)
                        for h in range(6):
                            if h + 1 < 6:
                                scores(h + 1)
                            soft(h)
                            pv(h)
                            for _ in range(3):
                                if pend:
                                    pend.pop(0)()
                        while pend:
                            pend.pop(0)()
                        po3 = pO[:, 0:390].rearrange('p (h d) -> p h d', h=6)
                        P.add('dve', lambda e: e.reciprocal(out=rec[cb][:], in_=po3[:, :, 64]), reads=[r_pO], writes=[r_rec[cb]])
                        P.add('dve', lambda e: e.tensor_tensor(out=natok[cb][:].rearrange('p (h d) -> p h d', h=6), in0=po3[:, :, 0:64], in1=rec[cb][:].unsqueeze(2).broadcast_to([128, 6, 64]), op=ALU.mult),
                              reads=[r_pO, r_rec[cb]], writes=[r_natok[cb]])

                    def sec_sg():
                        for gp in range(2):
                            for half in range(2):
                                g = 2 * gp + half
                                P.add('pe', lambda e, gp=gp, half=half, g=g: e.matmul(pRo[half * 64:(half + 1) * 64, 768 + gp * 128:768 + (gp + 1) * 128], lhsT=vnb[cb][:, g * 64:(g + 1) * 64], rhs=sgw[:, g, :], start=True, stop=True),
                                      reads=[r_ld[cb], r_sgt], writes=[r_pG])
                        P.add('dve', lambda e: e.tensor_tensor(out=sgt[cb][:], in0=pRo[:, 768:1024].rearrange('p (g q) -> p g q', g=2), in1=sgbt[:], op=ALU.add), reads=[r_pG, r_sgt], writes=[r_sgtmp[cb]])
                        P.add('dve', lambda e: e.tensor_tensor(out=catst[cb][:, 3:5, :], in0=sgt[cb][:], in1=uu[cb][:], op=ALU.mult), reads=[r_sgtmp[cb], r_ld[cb]], writes=[r_catst[cb]])

                    def sec_ret():
                        for dr in range(2):
                            P.add('dve', lambda e, dr=dr: e.tensor_tensor(out=qfs[cb][:, dr, :, :].rearrange('p (j two) q -> p j two q', two=2),
                                                                          in0=rqb[cb][:].unsqueeze(2).broadcast_to([128, 3, 2, 128]),
                                                                          in1=fsm[:, dr, :, :].rearrange('p (j two) q -> p j two q', two=2), op=ALU.mult),
                                  reads=[r_ld[cb], r_tab], writes=[r_qfs[cb]])
                        for h in range(6):
                            pc, half = h // 2, h % 2
                            r0 = half * 64
                            P.add('pe', lambda e, h=h, pc=pc, r0=r0, half=half: e.matmul(pS[:, half * 512 + pc * 128:half * 512 + (pc + 1) * 128], lhsT=rkc[cb][r0:r0 + 64, pc, :], rhs=rqb[cb][r0:r0 + 64, pc, :], start=True, stop=True),
                                  reads=[r_ld[cb]], writes=[r_pS])
                        for dr in range(2):
                            for par in range(2):
                                P.add('dve', lambda e, dr=dr, par=par: e.tensor_tensor(out=SD[cb][:, dr, :, :].rearrange('p (j two) q -> p j two q', two=2)[:, :, par, :],
                                                                                      in0=pS[:, par * 512:par * 512 + 384].rearrange('p (j q) -> p j q', j=3),
                                                                                      in1=Dm[:, dr, :, :].rearrange('p (j two) q -> p j two q', two=2)[:, :, par, :], op=ALU.mult),
                                      reads=[r_pS, r_tab], writes=[r_SD[cb]])
                        for dr in range(2):
                            for h in range(6):
                                pc, half = h // 2, h % 2
                                r0 = half * 64
                                ob = (dr * 6 + h) * 64
                                P.add('pe', lambda e, dr=dr, h=h, ob=ob: e.matmul(pRo[:, ob:ob + 64], lhsT=SD[cb][:, dr, h, :], rhs=rvc[cb][:, h * 64:(h + 1) * 64], start=True, stop=False),
                                      reads=[r_SD[cb], r_ld[cb]], writes=[r_pRo])
                                P.add('pe', lambda e, dr=dr, h=h, ob=ob, pc=pc, r0=r0: e.matmul(pRo[:, ob:ob + 64], lhsT=qfs[cb][:, dr, h, :], rhs=Sst[:, c, dr, pc, :], start=False, stop=True),
                                      reads=[r_qfs[cb], r_Sst[c][dr]], writes=[r_pRo])

                    def gn_steps():
                        st_ = []
                        o3 = lambda ap: ap.rearrange('p (g e) -> p g e', g=12)
                        b12 = lambda ap: ap.unsqueeze(2).broadcast_to([128, 12, 64])
                        st_.append(lambda: P.add('dve', lambda e: e.tensor_copy(out=osb[cb][:], in_=pRo[:, 0:768]), reads=[r_pRo], writes=[r_osb[cb]]))
                        st_.append(lambda: P.add('dve', lambda e: e.tensor_reduce(out=gsm[cb][:, 0:12], in_=o3(osb[cb][:]), axis=AX.X, op=ALU.add), reads=[r_osb[cb]], writes=[r_gsm[cb]]))
                        st_.append(lambda: P.add('dve', lambda e: e.tensor_tensor(out=osq[cb][:], in0=osb[cb][:], in1=osb[cb][:], op=ALU.mult), reads=[r_osb[cb]], writes=[r_osq[cb]]))
                        st_.append(lambda: P.add('dve', lambda e: e.tensor_reduce(out=gsm[cb][:, 12:24], in_=o3(osq[cb][:]), axis=AX.X, op=ALU.add), reads=[r_osq[cb]], writes=[r_gsm[cb]]))
                        st_.append(lambda: P.add('dve', lambda e: e.tensor_scalar(out=gsm[cb][:, 24:36], in0=gsm[cb][:, 0:12], scalar1=1.0 / 64, scalar2=None, op0=ALU.mult), reads=[r_gsm[cb]], writes=[r_gsm[cb]]))
                        st_.append(lambda: P.add('dve', lambda e: e.tensor_tensor(out=gsm[cb][:, 36:48], in0=gsm[cb][:, 24:36], in1=gsm[cb][:, 24:36], op=ALU.mult), reads=[r_gsm[cb]], writes=[r_gsm[cb]]))
                        st_.append(lambda: P.add('dve', lambda e: e.scalar_tensor_tensor(out=gsm[cb][:, 48:60], in0=gsm[cb][:, 12:24], scalar=1.0 / 64, in1=gsm[cb][:, 36:48], op0=ALU.mult, op1=ALU.subtract), reads=[r_gsm[cb]], writes=[r_gsm[cb]]))
                        st_.append(lambda: P.add('act', lambda e: e.activation(out=gsm[cb][:, 48:60], in_=gsm[cb][:, 48:60], func=AF.Ln, bias=lnbe[:, 0:1]), reads=[r_gsm[cb]], writes=[r_gsm[cb]]))
                        st_.append(lambda: P.add('act', lambda e: e.activation(out=gsm[cb][:, 60:72], in_=gsm[cb][:, 48:60], func=AF.Exp, scale=-0.5), reads=[r_gsm[cb]], writes=[r_gsm[cb]]))
                        st_.append(lambda: P.add('dve', lambda e: e.tensor_tensor(out=o3(g2[cb][:]), in0=o3(osb[cb][:]), in1=b12(gsm[cb][:, 24:36]), op=ALU.subtract), reads=[r_osb[cb], r_gsm[cb]], writes=[r_g2[cb]]))
                        st_.append(lambda: P.add('dve', lambda e: e.tensor_tensor(out=o3(g2[cb][:]), in0=o3(g2[cb][:]), in1=b12(gsm[cb][:, 60:72]), op=ALU.mult), reads=[r_g2[cb], r_gsm[cb]], writes=[r_g2[cb]]))
                        st_.append(lambda: P.add('dve', lambda e: e.tensor_tensor(out=g2[cb][:], in0=g2[cb][:], in1=gtb[cb][:], op=ALU.mult), reads=[r_g2[cb], r_ld[cb]], writes=[r_g2[cb]]))
                        st_.append(lambda: P.add('dve', lambda e: e.tensor_tensor(out=g3t[cb][:], in0=g2[cb][:, 0:384], in1=g2[cb][:, 384:768], op=ALU.add), reads=[r_g2[cb]], writes=[r_g3[cb]]))
                        st_.append(lambda: P.add('dve', lambda e: e.tensor_tensor(out=rettok[cb][:], in0=g3t[cb][:], in1=gnw[:], op=ALU.mult), reads=[r_g3[cb], r_sgt], writes=[r_rettok[cb]]))
                        return st_

                    def sec_tr():
                        for pc in range(3):
                            P.add('pe', lambda e, pc=pc: e.transpose(pT[:, pc * 128:(pc + 1) * 128], natok[cb][:, pc * 128:(pc + 1) * 128], ident_b), reads=[r_natok[cb], r_cstbf], writes=[r_pT])
                        for pc in range(3):
                            P.add('pe', lambda e, pc=pc: e.transpose(pT[:, (3 + pc) * 128:(4 + pc) * 128], rettok[cb][:, pc * 128:(pc + 1) * 128], ident_b), reads=[r_rettok[cb], r_cstbf], writes=[r_pT])
                        P.add('act', lambda e: e.copy(out=catst[cb][:, 0:3, :], in_=pT[:, 0:384].rearrange('p (c t) -> p c t', c=3)), reads=[r_pT], writes=[r_catst[cb]])
                        P.add('act', lambda e: e.copy(out=catst[cb][:, 5:8, :], in_=pT[:, 384:768].rearrange('p (c t) -> p c t', c=3)), reads=[r_pT], writes=[r_catst[cb]])
                        P.add('pool', lambda e: e.dma_start(out=catT.rearrange('(kc p) t -> p kc t', p=128)[:, :, tok:tok + 128], in_=catst[cb][:]), reads=[r_catst[cb]], writes=[DR(id(catT), tl)], dma=True)

                    return sec_na, sec_sg, sec_ret, gn_steps, sec_tr

                chunks = list(range(NLC)) + ([] if last else [NLC, NLC + 1])
                pend_gn = []
                pend_tr = None
                for idx, c in enumerate(chunks):
                    na_, sg_, ret_, gn_, tr_ = do_chunk(c, idx)
                    na_(pend_gn)
                    sg_()
                    ret_()
                    if pend_tr is not None:
                        pend_tr()
                    pend_gn, pend_tr = gn_(), tr_
                while pend_gn:
                    pend_gn.pop(0)()
                pend_tr()
                P.emit_pass()

        for l_ in range(n_layers):
            conv_jobs.append([('w1_%d_0' % l_, w1_in[l_, 0]), ('w2_%d_0' % l_, w2_in[l_, 0])])
            conv_jobs.append([('win_%d' % l_, win_in[l_])])
            conv_jobs.append([('wo_%d' % l_, wout_in[l_])])
            conv_jobs.append([('w1_%d_1' % l_, w1_in[l_, 1]), ('w2_%d_1' % l_, w2_in[l_, 1])])
        pass_xpose_in(xTa)
        cur, oth = xTa, xTb
        for l in range(n_layers):
            last = (l == n_layers - 1)
            pass_mod(l)
            pass_ffn(l, 0, cur, oth)
            cur, oth = oth, cur
            if stop_after == 'ffn1':
                break
            pass_inproj(l, cur)
            if stop_after == 'inproj':
                break
            pass_mixer(l, last)
            if stop_after == 'mixer':
                break
            pass_outproj(l, cur, oth, skip_ctx=last)
            cur, oth = oth, cur
            if stop_after == 'outproj':
                break
            pass_ffn(l, 1, cur, oth, skip_ctx=last)
            cur, oth = oth, cur
        pass_xpose_out(cur)
    return nc


def prep_inputs(NT, b, inp, shared):
    m = dict(shared)
    m['x'] = np.ascontiguousarray(inp['x'][b])
    m['ctx'] = np.ascontiguousarray(inp['ctx'][b])
    cc = np.stack([inp['c'][b], inp['c_ctx']], axis=0)
    m['cT'] = np.ascontiguousarray(cc.reshape(2, KC, 128).transpose(2, 1, 0).reshape(128, 16))
    return m


def prep_shared(NT, inp):
    f = lambda a: np.ascontiguousarray(np.asarray(a, dtype=np.float32))
    NLC = NT // 128
    var_of, base_of, ridx, cidx, valid = na_tables(NLC)
    sh = {}
    sh['ada_w'] = f(inp['ada_w'])
    sh['ada_bT'] = f(inp['ada_b'].reshape(2, 72, 128).transpose(0, 2, 1))
    sh['norm_wT'] = f(inp['norm_w'].reshape(2, 3, KC, 128).transpose(3, 0, 1, 2).reshape(128, 48))
    sh['ffn_w1'] = f(inp['ffn_w1'])
    sh['ffn_w2'] = f(inp['ffn_w2'])
    w_in = np.asarray(inp['mix_w_in'], dtype=np.float32)
    perm = np.arange(384).reshape(6, 64)
    part = perm.copy()
    for h in range(6):
        for d in range(64):
            part[h, d] = h * 64 + (d + 16 if (d % 32) < 16 else d - 16)
    part = part.reshape(-1)
    sh['w_in_ext'] = f(np.concatenate([w_in, w_in[:, :, 1664 + part], w_in[:, :, 2048 + part]], axis=2))
    sh['w_out'] = f(inp['mix_w_out'])
    qk = np.zeros((128, 4), np.float32)
    for l in range(2):
        qk[:, 2 * l] = np.tile(inp['na_q_norm'][l], 2)
        qk[:, 2 * l + 1] = np.tile(inp['na_k_norm'][l], 2)
    sh['qknT'] = qk
    rpb = np.asarray(inp['na_rpb'], dtype=np.float32)
    g = rpb[:, :, ridx, cidx]
    g = np.where(valid[None, None], g, np.float32(-1e30)).astype(np.float32)
    sh['na_biasT'] = f(g.transpose(0, 2, 1, 4, 3, 5))
    sh['sg_wT'] = f(np.asarray(inp['sg_w']).transpose(0, 1, 3, 2))
    sgb = np.asarray(inp['sg_b'], dtype=np.float32)
    sh['sgb_rep'] = f(np.repeat(sgb.reshape(2, 2, 2, 1, 128), 64, axis=3).reshape(2, 2, 128, 128))
    sh['sg_lnw_rep'] = f(np.broadcast_to(np.asarray(inp['sg_ln_w']).reshape(1, 512), (128, 512)))
    sh['sg_lnb_rep'] = f(np.broadcast_to(np.asarray(inp['sg_ln_b']).reshape(1, 512), (128, 512)))
    sh['dlog_rep'] = f(np.broadcast_to(np.asarray(inp['ret_decay_logit']).reshape(1, 24), (128, 24)))
    sh['gnw_rep'] = f(np.broadcast_to(np.asarray(inp['ret_gn_w']).reshape(1, 768), (128, 768)))
    C, S = rope_tables(NT)
    sh['ropeC'] = C
    sh['ropeS'] = S
    sh['consts'] = const_table()
    return sh


_NC_CACHE = {}


def kernel(**inputs):
    inp = {k: np.asarray(v) for k, v in inputs.items()}
    B, NT, _ = inp['x'].shape
    if NT not in _NC_CACHE:
        _NC_CACHE[NT] = build(NT)
    nc = _NC_CACHE[NT]
    shared = prep_shared(NT, inp)
    in_maps = [prep_inputs(NT, b, inp, shared) for b in range(B)]
    res = run_bass_kernel_spmd(nc, in_maps, core_ids=list(range(B)))
    return np.stack([np.asarray(r['y']) for r in res.results], axis=0).astype(np.float32)
```
